# Optimizing a Trainium2 kernel written in Bass

```python
import math
import jax, jax.numpy as jnp
from jax import lax
import numpy as np

D_MODEL = 2048
BATCH = 4
SEQ = 4096
DEPTH = 1

HEAD_DIM = 128
N_HEADS_A = 8
N_HEADS_B = 8
D_A = N_HEADS_A * HEAD_DIM
D_B = N_HEADS_B * HEAD_DIM
D_MIX = D_A + D_B
Q_RANK = 512
IDX_HEADS = 16
IDX_DIM = 64
TOPK_MAX = 256
N_BUCKETS = 32
MAX_DISTANCE = 128
Q_BLOCK = 128
N_GROUPS = 4
EXPERTS_PER_GROUP = 8
N_EXPERTS = N_GROUPS * EXPERTS_PER_GROUP
EXPERT_TOPK = 2
D_EXPERT = 512
MOE_BLOCK = 128
N_MOD = 6
EPS = 1e-6
IN_COLS = (Q_RANK, HEAD_DIM, HEAD_DIM, IDX_DIM, IDX_HEADS, D_B, D_B, D_B)
D_IN = sum(IN_COLS)

kernel_name = "hymba_dsa_stickbreak_hmoe_layer"


def rms_norm(x, g):
    xf = x.astype(jnp.float32)
    y = xf * lax.rsqrt(jnp.mean(xf * xf, axis=-1, keepdims=True) + EPS)
    return (y * g.astype(jnp.float32)).astype(x.dtype)


def t5_bucket(dist):
    max_exact = N_BUCKETS // 2
    d = jnp.maximum(dist, 1).astype(jnp.float32)
    large = max_exact + (jnp.log(d / max_exact) / math.log(MAX_DISTANCE / max_exact)
                         * (N_BUCKETS - max_exact)).astype(jnp.int32)
    large = jnp.minimum(large, N_BUCKETS - 1)
    return jnp.where(dist < max_exact, dist, large)


def dsa_block(q_blk, qi_blk, wi_blk, t_blk, k, v, k_idx, rel_bias, topk):
    s_pos = jnp.arange(k.shape[1], dtype=jnp.int32)
    causal = s_pos[None, :] <= t_blk[:, None]
    sc = jnp.einsum('bqhd,bsd->bqhs', qi_blk, k_idx)
    score = jnp.einsum('bqhs,bqh->bqs', jax.nn.relu(sc), wi_blk).astype(jnp.float32)
    score = jnp.where(causal[None], score, -jnp.inf)
    _, sel = lax.top_k(score, topk)
    valid = sel <= t_blk[None, :, None]
    k_sel = jax.vmap(lambda kk, ii: kk[ii])(k, sel)
    v_sel = jax.vmap(lambda vv, ii: vv[ii])(v, sel)
    logits = jnp.einsum('bqhd,bqkd->bqhk', q_blk, k_sel).astype(jnp.float32)
    bias = rel_bias[t5_bucket(jnp.maximum(t_blk[None, :, None] - sel, 0))]
    logits = logits + jnp.moveaxis(bias, -1, 2).astype(jnp.float32)
    logits = jnp.where(valid[:, :, None, :], logits, -jnp.inf)
    p = jax.nn.softmax(logits, axis=-1)
    return jnp.einsum('bqhk,bqkd->bqhd', p.astype(v.dtype), v_sel)


def stick_breaking_block(q_blk, t_blk, k, v):
    s_pos = jnp.arange(k.shape[1], dtype=jnp.int32)
    strict = (s_pos[None, :] < t_blk[:, None])[None, None]
    z = jnp.einsum('bqhd,bshd->bhqs', q_blk, k).astype(jnp.float32)
    log_1m = jnp.where(strict, jax.nn.log_sigmoid(-z), 0.0)
    tail = lax.cumsum(log_1m, axis=3, reverse=True) - log_1m
    a = jnp.where(strict, jnp.exp(jax.nn.log_sigmoid(z) + tail), 0.0)
    return jnp.einsum('bhqs,bshd->bqhd', a.astype(v.dtype), v)


def hybrid_mixer(h, w_in, q_norm_g, w_q_up, q_gain, k_gain, rel_bias, gn_a, gn_b, w_out):
    B, S, _ = h.shape
    splits = list(np.cumsum(IN_COLS)[:-1])
    c_q, k_a, v_a, k_idx, w_idx, q_b, k_b, v_b = jnp.split(h @ w_in, splits, axis=-1)
    q_up = rms_norm(c_q, q_norm_g) @ w_q_up
    q_a = q_up[..., :D_A].reshape(B, S, N_HEADS_A, HEAD_DIM)
    q_a = rms_norm(q_a, q_gain) * (HEAD_DIM ** -0.5)
    k_a = rms_norm(k_a, k_gain)
    q_idx = q_up[..., D_A:].reshape(B, S, IDX_HEADS, IDX_DIM) * (IDX_DIM ** -0.5)
    w_idx = w_idx * (IDX_HEADS ** -0.5)
    q_b = q_b.reshape(B, S, N_HEADS_B, HEAD_DIM) * (HEAD_DIM ** -0.5)
    k_b = k_b.reshape(B, S, N_HEADS_B, HEAD_DIM)
    v_b = v_b.reshape(B, S, N_HEADS_B, HEAD_DIM)

    topk = min(TOPK_MAX, S // 4)
    nb = S // Q_BLOCK

    def to_blocks(a):
        return jnp.moveaxis(a.reshape(B, nb, Q_BLOCK, *a.shape[2:]), 1, 0)

    t_all = jnp.arange(S, dtype=jnp.int32).reshape(nb, Q_BLOCK)

    def per_block(args):
        qa, qi, wi, qb, t = args
        oa = dsa_block(qa, qi, wi, t, k_a, v_a, k_idx, rel_bias, topk)
        ob = stick_breaking_block(qb, t, k_b, v_b)
        return oa, ob

    o_a, o_b = lax.map(per_block, (to_blocks(q_a), to_blocks(q_idx), to_blocks(w_idx),
                                   to_blocks(q_b), t_all))
    o_a = jnp.moveaxis(o_a, 0, 1).reshape(B, S, D_A)
    o_b = jnp.moveaxis(o_b, 0, 1).reshape(B, S, D_B)
    o = jnp.concatenate([rms_norm(o_a, gn_a), rms_norm(o_b, gn_b)], axis=-1)
    return o @ w_out


def hierarchical_moe(h, router_g, router_e, w_gate, w_up, w_down):
    n, d = h.shape
    pg = jax.nn.softmax((h @ router_g).astype(jnp.float32), axis=-1)
    g_idx = jnp.argmax(pg, axis=-1).astype(jnp.int32)
    g_w = jnp.max(pg, axis=-1)
    le = (h @ router_e).astype(jnp.float32).reshape(n, N_GROUPS, EXPERTS_PER_GROUP)
    le = jnp.take_along_axis(le, g_idx[:, None, None], axis=1)[:, 0]
    top_p, top_i = lax.top_k(jax.nn.softmax(le, axis=-1), EXPERT_TOPK)
    gate = g_w[:, None] * top_p / jnp.sum(top_p, axis=-1, keepdims=True)
    expert = g_idx[:, None] * EXPERTS_PER_GROUP + top_i.astype(jnp.int32)

    a = n * EXPERT_TOPK
    flat_e = expert.reshape(a)
    flat_w = gate.reshape(a)
    flat_tok = jnp.repeat(jnp.arange(n, dtype=jnp.int32), EXPERT_TOPK)
    order = jnp.argsort(flat_e)
    se = flat_e[order]
    counts = jnp.bincount(flat_e, length=N_EXPERTS).astype(jnp.int32)
    padded = (counts + MOE_BLOCK - 1) // MOE_BLOCK * MOE_BLOCK
    pad_end = jnp.cumsum(padded)
    pad_start = pad_end - padded
    start = jnp.cumsum(counts) - counts
    dest = pad_start[se] + jnp.arange(a, dtype=jnp.int32) - start[se]
    p = a + N_EXPERTS * MOE_BLOCK
    nb = p // MOE_BLOCK
    buf_tok = jnp.zeros((p,), jnp.int32).at[dest].set(flat_tok[order])
    buf_w = jnp.zeros((p,), jnp.float32).at[dest].set(flat_w[order])
    blk_expert = jnp.minimum(
        jnp.searchsorted(pad_end, jnp.arange(nb, dtype=jnp.int32) * MOE_BLOCK, side='right'),
        N_EXPERTS - 1)
    xb = h[buf_tok].reshape(nb, MOE_BLOCK, d)

    def expert_block(args):
        xe, e = args
        u = jax.nn.silu(xe @ w_gate[e]) * (xe @ w_up[e])
        return u @ w_down[e]

    yb = lax.map(expert_block, (xb, blk_expert)).reshape(p, d)
    return jnp.zeros_like(h).at[buf_tok].add(yb * buf_w[:, None].astype(h.dtype))


def setup_inputs(seed: int = 0) -> dict:
    key = jax.random.key(seed)
    ks = jax.random.split(key, 24)
    f = jnp.float32
    nrm = lambda k, shape, s: jax.random.normal(k, shape, f) * s
    L, D = DEPTH, D_MODEL
    return {
        "x": nrm(ks[0], (BATCH, SEQ, D), 1.0),
        "c": nrm(ks[1], (BATCH, D), 1.0),
        "w_mod": nrm(ks[2], (L, D, N_MOD * D), 0.5 * D ** -0.5),
        "b_mod": nrm(ks[3], (L, N_MOD * D), 0.02),
        "ln1_g": 1.0 + nrm(ks[4], (L, D), 0.02),
        "w_in": nrm(ks[5], (L, D, D_IN), D ** -0.5),
        "q_norm_g": 1.0 + nrm(ks[6], (L, Q_RANK), 0.02),
        "w_q_up": nrm(ks[7], (L, Q_RANK, D_A + IDX_HEADS * IDX_DIM), Q_RANK ** -0.5),
        "q_gain": 1.0 + nrm(ks[8], (L, HEAD_DIM), 0.02),
        "k_gain": 1.0 + nrm(ks[9], (L, HEAD_DIM), 0.02),
        "rel_bias": nrm(ks[10], (N_BUCKETS, N_HEADS_A), 0.5),
        "gn_a": 1.0 + nrm(ks[11], (L, D_A), 0.02),
        "gn_b": 1.0 + nrm(ks[12], (L, D_B), 0.02),
        "w_out": nrm(ks[13], (L, D_MIX, D), D_MIX ** -0.5),
        "ln2_g": 1.0 + nrm(ks[14], (L, D), 0.02),
        "router_g": nrm(ks[15], (L, D, N_GROUPS), D ** -0.5),
        "router_e": nrm(ks[16], (L, D, N_EXPERTS), D ** -0.5),
        "w_gate": nrm(ks[17], (L, N_EXPERTS, D, D_EXPERT), D ** -0.5),
        "w_up": nrm(ks[18], (L, N_EXPERTS, D, D_EXPERT), D ** -0.5),
        "w_down": nrm(ks[19], (L, N_EXPERTS, D_EXPERT, D), D_EXPERT ** -0.5),
    }


def reference(x, c, w_mod, b_mod, ln1_g, w_in, q_norm_g, w_q_up, q_gain, k_gain, rel_bias,
              gn_a, gn_b, w_out, ln2_g, router_g, router_e, w_gate, w_up, w_down):
    B, S, D = x.shape
    for l in range(DEPTH):
        mod = jax.nn.silu(c) @ w_mod[l] + b_mod[l]
        sh1, sc1, g1, sh2, sc2, g2 = [m[:, None, :] for m in jnp.split(mod, N_MOD, axis=-1)]
        h = rms_norm(x, ln1_g[l]) * (1.0 + sc1) + sh1
        x = x + g1 * hybrid_mixer(h, w_in[l], q_norm_g[l], w_q_up[l], q_gain[l], k_gain[l],
                                  rel_bias, gn_a[l], gn_b[l], w_out[l])
        h = rms_norm(x, ln2_g[l]) * (1.0 + sc2) + sh2
        y = hierarchical_moe(h.reshape(B * S, D), router_g[l], router_e[l],
                             w_gate[l], w_up[l], w_down[l])
        x = x + g2 * y.reshape(B, S, D)
    return x
```

```python
import contextlib
import numpy as np
import concourse.bass as bass
import concourse.mybir as mybir
from concourse.bass_utils import run_bass_kernel_spmd

F32 = mybir.dt.float32
BF16 = mybir.dt.bfloat16
AF = mybir.ActivationFunctionType
ALU = mybir.AluOpType
AX = mybir.AxisListType

D = 2048
KC = 16
SA = 4096
NOWN = 2048
HD = 128
NE = 32
DE = 512
EPS = 1e-6
BIG = 1.0e30
TOPK = 256
NBIS = 14
IN_COLS = (512, 128, 128, 64, 16, 1024, 1024, 1024)
D_IN = sum(IN_COLS)
DEBUG = None


class Sched:
    ENG = ("pe", "act", "dve", "pool", "sp")
    BLK = {"pe": "tensor", "act": "scalar", "dve": "vector", "pool": "gpsimd", "sp": "sync"}

    def __init__(self, nc, stack):
        self.nc = nc
        self.stack = stack
        self.sem = {e: stack.enter_context(nc.semaphore("sem_" + e)) for e in self.ENG}
        self.cnt = {e: 0 for e in self.ENG}
        self.ops = {e: [] for e in self.ENG}
        self.waited = {e: {} for e in self.ENG}
        self.lastw = {}
        self.readers = {}
        self.touched = set()

    def _semfor(self, slot):
        name = "d_" + str(slot)
        if name not in self.sem:
            self.sem[name] = self.stack.enter_context(self.nc.semaphore(name))
            self.cnt[name] = 0
        return name

    def _deps(self, eng, reads, writes):
        need = {}

        def add(tok):
            if tok is None:
                return
            s, v = tok
            if need.get(s, 0) < v:
                need[s] = v
        for k in reads:
            add(self.lastw.get(k))
            if k.startswith("ps"):
                for t in self.readers.get(k, ()):
                    add(t)
        for k in writes:
            add(self.lastw.get(k))
            for t in self.readers.get(k, ()):
                add(t)
        out = []
        for s, v in need.items():
            if s == eng and eng == "pe":
                continue
            if self.waited[eng].get(s, 0) >= v:
                continue
            self.waited[eng][s] = v
            out.append((s, v))
        return out

    def _record(self, tok, reads, writes):
        for k in reads:
            self.readers.setdefault(k, []).append(tok)
        for k in writes:
            self.lastw[k] = tok
            self.readers[k] = []

    def op(self, eng, fn, reads=(), writes=()):
        waits = self._deps(eng, reads, writes)
        self.cnt[eng] += 1
        tok = (eng, self.cnt[eng])
        self.ops[eng].append((waits, fn, eng, 1))
        self._record(tok, reads, writes)
        return tok

    def dma(self, q, fn, reads=(), writes=(), slot=None, n=1):
        if slot is None:
            slot = writes[0]
        name = self._semfor(slot)
        waits = self._deps(q, reads, writes)
        self.cnt[name] += 16 * n
        tok = (name, self.cnt[name])
        self.ops[q].append((waits, fn, name, 16))
        self._record(tok, reads, writes)
        self.touched.add(name)
        return tok

    def drain(self):
        waits = []
        for name in sorted(self.touched):
            v = self.cnt[name]
            if self.waited["sp"].get(name, 0) < v:
                self.waited["sp"][name] = v
                waits.append((name, v))
        self.touched = set()
        if waits:
            self.ops["sp"].append((waits, None, None, 0))

    def emit(self):
        self.drain()
        with self.nc.Block() as block:
            for eng in self.ENG:
                ops = self.ops[eng]
                if not ops:
                    continue

                def body(e, ops=ops):
                    for waits, fn, semname, inc in ops:
                        for s, v in waits:
                            e.wait_ge(self.sem[s], v)
                        if fn is None:
                            continue
                        r = fn(e)
                        if isinstance(r, (list, tuple)):
                            for ins in r:
                                ins.then_inc(self.sem[semname], inc)
                        else:
                            r.then_inc(self.sem[semname], inc)
                getattr(block, self.BLK[eng])(body)
        self.ops = {e: [] for e in self.ENG}


def build_nc(stop=6):
    nc = bass.Bass("TRN2", target_bir_lowering=False)
    dbg = stop < 6

    def din(name, shape, dt=F32):
        return nc.dram_tensor(name, list(shape), dt, kind="ExternalInput").ap()

    xk = din("xk", [SA, D])
    kvalid_d = din("kvalid", [128, 1])
    c_col_d = din("c_col", [128, KC])
    w_mod = din("w_mod", [D, 6 * D])
    b_mod = din("b_mod", [1, 6 * D])
    ln1g_d = din("ln1g", [128, KC])
    ln2g_d = din("ln2g", [128, KC])
    w_in = din("w_in", [D, D_IN])
    qng_d = din("qng", [128, 4])
    w_qup = din("w_qup", [512, 2048])
    qgain_d = din("qgain", [128, 1])
    kgain_d = din("kgain", [128, 1])
    gna_d = din("gna", [128, 8])
    gnb_d = din("gnb", [128, 8])
    w_out = din("w_out", [D, D])
    router_d = din("router", [D, 36])
    if not dbg:
        w_gate = din("w_gate", [NE, D, DE])
        w_up = din("w_up", [NE, D, DE])
        w_down = din("w_down", [NE, DE, D])
    tb_d = din("tbias", [8, 128, 1024])
    rb31_d = din("rb31", [128, 8])
    out_d = nc.dram_tensor("out", [NOWN, D], F32, kind="ExternalOutput").ap()

    def dscr(name, shape, dt):
        return nc.dram_tensor(name, list(shape), dt, kind="ExternalOutput").ap()

    dbg_outs = {}

    def finish(S, items):
        for name, t, shape, dt in items:
            d_ = nc.dram_tensor("dbg_" + name, list(shape), dt, kind="ExternalOutput").ap()
            S.dma("sp", lambda e, t=t, d_=d_: e.dma_start(out=d_, in_=t[:]), reads=[name], writes=["dbg_" + name])
        if items:
            S.emit()

    kbT_s = dscr("kbT_s", [8, 128, SA], BF16)
    vb_s = dscr("vb_s", [SA, 1024], BF16)
    qbT_s = dscr("qbT_s", [8, 128, NOWN], BF16)
    qaT_s = dscr("qaT_s", [4, 128, 8, 512], BF16)
    qiT_s = dscr("qiT_s", [4, 128, 8, 512], BF16)
    x1_s = dscr("x1_s", [NOWN, D], F32)

    with contextlib.ExitStack() as gs:
        S = Sched(nc, gs)

        def sb(stack, name, shape, dt):
            return stack.enter_context(nc.sbuf_tensor(name, list(shape), dt))

        ps = [gs.enter_context(nc.psum_tensor("ps%d" % i, [128, 512], F32)) for i in range(8)]
        PK = ["ps%d" % i for i in range(8)]

        ident = sb(gs, "ident", [128, 128], F32)
        identb = sb(gs, "identb", [128, 128], BF16)
        onesb = sb(gs, "onesb", [128, 128], BF16)
        negtri = sb(gs, "negtri", [128, 128], BF16)
        negones = sb(gs, "negones", [128, 128], BF16)
        ones_row = sb(gs, "ones_row", [1, 128], F32)
        modT = sb(gs, "modT", [128, 96], F32)
        G1 = sb(gs, "G1", [128, KC], F32)
        G2 = sb(gs, "G2", [128, KC], F32)
        g2_bc = sb(gs, "g2_bc", [128, D], F32)
        kvalid = sb(gs, "kvalid_t", [128, 1], F32)
        qng = sb(gs, "qng_t", [128, 4], F32)
        qgain = sb(gs, "qgain_t", [128, 1], F32)
        kgain = sb(gs, "kgain_t", [128, 1], F32)
        gna = sb(gs, "gna_t", [128, 8], F32)
        gnb = sb(gs, "gnb_t", [128, 8], F32)
        ats = contextlib.ExitStack()
        g1_bc = sb(ats, "g1_bc", [128, D], F32)
        with contextlib.ExitStack() as p0:
            c_col = sb(p0, "c_col_t", [128, KC], F32)
            sc_col = sb(p0, "sc_col", [128, KC], F32)
            mrow = [sb(p0, "mrow%d" % i, [1, 512], F32) for i in range(2)]
            bmc = [sb(p0, "bmc%d" % i, [1, 512], F32) for i in range(2)]
            ln1g = sb(p0, "ln1g_t", [128, KC], F32)
            ln2g = sb(p0, "ln2g_t", [128, KC], F32)
            tmpc = sb(p0, "tmpc", [128, KC], F32)
            one11 = sb(p0, "one11", [1, 1], F32)
            onesf0 = sb(p0, "onesf0", [128, 128], F32)
            wm = [sb(p0, "wm%d" % i, [128, KC, 512], F32) for i in range(3)]

            small = [(c_col, c_col_d, "c_col"), (ln1g, ln1g_d, "ln1g"), (ln2g, ln2g_d, "ln2g"), (qng, qng_d, "qng"),
                     (qgain, qgain_d, "qgain"), (kgain, kgain_d, "kgain"), (gna, gna_d, "gna"), (gnb, gnb_d, "gnb"),
                     (kvalid, kvalid_d, "kvalid")]
            for t, d_, k in small:
                S.dma("sp", lambda e, t=t, d_=d_: e.dma_start(out=t[:], in_=d_), writes=[k])

            S.op("pool", lambda e: e.memset(onesf0[:], 1.0), writes=["onesf0"])
            S.op("pool", lambda e: e.memset(ones_row[:], 1.0), writes=["ones_row"])
            S.op("pool", lambda e: e.memset(one11[:], 1.0), writes=["one11"])
            S.op("pool", lambda e: e.memset(onesb[:], 1.0), writes=["onesb"])
            S.op("pool", lambda e: e.memset(negones[:], -1.0), writes=["negones"])
            S.op("pool", lambda e: e.affine_select(out=ident[:], in_=onesf0[:], pattern=[[-1, 128]],
                                                   compare_op=ALU.is_equal, fill=0.0, base=0, channel_multiplier=1),
                 reads=["onesf0"], writes=["ident"])
            S.op("pool", lambda e: e.tensor_copy(out=identb[:], in_=ident[:]), reads=["ident"], writes=["identb"])
            S.op("pool", lambda e: e.affine_select(out=negtri[:], in_=negones[:], pattern=[[-1, 128]],
                                                   compare_op=ALU.is_ge, fill=0.0, base=0, channel_multiplier=1),
                 reads=["negones"], writes=["negtri"])
            S.op("act", lambda e: e.activation(out=sc_col[:], in_=c_col[:], func=AF.Silu), reads=["c_col"],
                 writes=["sc_col"])
            for g in range(24):
                wt = wm[g % 3]
                wk = "wm%d" % (g % 3)
                mi = g % 2
                S.dma("sp", lambda e, wt=wt, g=g: e.dma_start(
                    out=wt[:], in_=w_mod[:, g * 512:(g + 1) * 512].rearrange("(c p) n -> p c n", p=128)), writes=[wk])
                S.dma("sp", lambda e, mi=mi, g=g: e.dma_start(out=bmc[mi][:], in_=b_mod[:, g * 512:(g + 1) * 512]),
                      writes=["bmc%d" % mi])
                pk = g % 2

                def mm(e, wt=wt, pk=pk):
                    r = None
                    for k in range(KC):
                        r = e.matmul(ps[pk][0:1, :], sc_col[:, k:k + 1], wt[:, k, :], start=(k == 0), stop=(k == KC - 1))
                    return r
                S.op("pe", mm, reads=[wk, "sc_col"], writes=[PK[pk]])
                S.op("dve", lambda e, mi=mi, pk=pk: e.tensor_tensor(out=mrow[mi][:], in0=ps[pk][0:1, :], in1=bmc[mi][:], op=ALU.add),
                     reads=[PK[pk], "bmc%d" % mi], writes=["mrow%d" % mi])

                def mmT(e, mi=mi, g=g):
                    r = None
                    for q in range(4):
                        j = g * 4 + q
                        r = e.matmul(ps[2][:, j:j + 1], mrow[mi][0:1, q * 128:(q + 1) * 128], one11[0:1, 0:1], start=True, stop=True)
                    return r
                S.op("pe", mmT, reads=["mrow%d" % mi, "one11"], writes=[PK[2]])
                if 8 <= g < 12 or 20 <= g < 24:
                    dst, dk, q = (g1_bc, "g1_bc", g - 8) if g < 12 else (g2_bc, "g2_bc", g - 20)
                    pb = 3 + g % 2
                    S.op("pe", lambda e, pb=pb, mi=mi: e.matmul(ps[pb][:, :], ones_row[0:1, :], mrow[mi][0:1, :], start=True, stop=True),
                         reads=["mrow%d" % mi, "ones_row"], writes=[PK[pb]])
                    S.op("dve", lambda e, dst=dst, pb=pb, q=q: e.tensor_copy(out=dst[:, q * 512:(q + 1) * 512], in_=ps[pb][:, :]),
                         reads=[PK[pb]], writes=[dk])
            S.op("dve", lambda e: e.tensor_copy(out=modT[:], in_=ps[2][:, 0:96]), reads=[PK[2]], writes=["modT"])
            S.op("dve", lambda e: e.tensor_scalar(out=tmpc[:], in0=modT[:, 16:32], scalar1=1.0, scalar2=None, op0=ALU.add),
                 reads=["modT"], writes=["tmpc"])
            S.op("dve", lambda e: e.tensor_tensor(out=G1[:], in0=tmpc[:], in1=ln1g[:], op=ALU.mult),
                 reads=["tmpc", "ln1g"], writes=["G1"])
            S.op("dve", lambda e: e.tensor_scalar(out=tmpc[:], in0=modT[:, 64:80], scalar1=1.0, scalar2=None, op0=ALU.add),
                 reads=["modT", "G1"], writes=["tmpc"])
            S.op("dve", lambda e: e.tensor_tensor(out=G2[:], in0=tmpc[:], in1=ln2g[:], op=ALU.mult),
                 reads=["tmpc", "ln2g"], writes=["G2"])
            S.emit()


        maskV = sb(ats, "maskV", [128, 512], BF16)
        maskS = sb(ats, "maskS", [128, 4, 512], BF16)
        maskI = sb(ats, "maskI", [128, 4, 512], BF16)
        kaT = sb(ats, "kaT", [128, SA], BF16)
        va = sb(ats, "va", [128, 32, 128], BF16)
        kiT = sb(ats, "kiT", [128, SA], BF16)
        widx = sb(ats, "widx", [128, 16, 16], F32)
        c2 = contextlib.ExitStack()
        cqT = sb(c2, "cqT", [128, 4, NOWN], BF16)

        evq = [0]

        def evac_scale(dst, src, s1, s2, reads, writes):
            evq[0] += 1
            if evq[0] % 2 == 0:
                if s2 is None:
                    S.op("dve", lambda e: e.tensor_scalar(out=dst, in0=src, scalar1=s1, scalar2=None, op0=ALU.mult),
                         reads=reads, writes=writes)
                else:
                    S.op("dve", lambda e: e.tensor_scalar(out=dst, in0=src, scalar1=s1, scalar2=s2, op0=ALU.mult,
                                                          op1=ALU.add), reads=reads, writes=writes)
            else:
                if s2 is None:
                    S.op("act", lambda e: e.activation(out=dst, in_=src, func=AF.Copy if isinstance(s1, float) else AF.Identity,
                                                       scale=s1), reads=reads, writes=writes)
                else:
                    S.op("act", lambda e: e.activation(out=dst, in_=src, func=AF.Identity, scale=s1, bias=s2),
                         reads=reads, writes=writes)

        def rstd_from(ss_ap, dim, key_in, key_out, out_ap):
            S.op("act", lambda e: e.activation(out=out_ap, in_=ss_ap, func=AF.Sqrt, scale=1.0 / dim, bias=EPS),
                 reads=[key_in], writes=[key_out])
            S.op("dve", lambda e: e.reciprocal(out=out_ap, in_=out_ap), reads=[key_out], writes=[key_out])

        psrot = [0]

        def nextps():
            i = psrot[0] % 8
            psrot[0] += 1
            return i

        if stop == 0:
            finish(S, [("modT", modT, [128, 96], F32), ("G1", G1, [128, KC], F32), ("G2", G2, [128, KC], F32),
                       ("g1_bc", g1_bc, [128, D], F32), ("g2_bc", g2_bc, [128, D], F32), ("ident", ident, [128, 128], F32),
                       ("negtri", negtri, [128, 128], BF16)])
            c2.close()
            ats.close()
            return nc
        with contextlib.ExitStack() as p1:
            hT = sb(p1, "hT", [128, KC, 1024], BF16)
            xs = sb(p1, "xs", [128, 4, D], F32)
            junk = sb(p1, "junk", [128, D], BF16)
            ssq = sb(p1, "ssq", [128, 8], F32)
            rstd = sb(p1, "rstd", [128, 8], F32)
            wbuf = [sb(p1, "wbuf%d" % i, [128, KC, 512], BF16) for i in range(2)]
            stg = [sb(p1, "stg%d" % i, [128, 512], BF16) for i in range(3)]
            stgv = [sb(p1, "stgv%d" % i, [128, 1024], BF16) for i in range(2)]
            tm32 = [sb(p1, "tm32_%d" % i, [128, 512], F32) for i in range(2)]
            sm = sb(p1, "sm", [128, 8], F32)
            onesf = sb(p1, "onesf", [128, 512], F32)
            S.op("pool", lambda e: e.memset(onesf[:], 1.0), writes=["onesf"])
            for i in range(4):
                S.op("pool", lambda e, i=i: e.affine_select(out=maskI[:, i, :], in_=onesf[:], pattern=[[1, 512]],
                                                            compare_op=ALU.is_ge, fill=0.0, base=-128 * i,
                                                            channel_multiplier=-1),
                     reads=["onesf"], writes=["maskI"])
                S.op("pool", lambda e, i=i: e.affine_select(out=maskS[:, i, :], in_=onesf[:], pattern=[[1, 512]],
                                                            compare_op=ALU.is_ge, fill=0.0, base=-128 * i - 1,
                                                            channel_multiplier=-1),
                     reads=["onesf"], writes=["maskS"])
            S.op("dve", lambda e: e.tensor_scalar(out=maskV[:], in0=onesf[:], scalar1=kvalid[:, 0:1], scalar2=None,
                                                  op0=ALU.mult), reads=["onesf", "kvalid"], writes=["maskV"])

            wload_i = [0]

            def load_w(col0, ncol, dup=False):
                i = wload_i[0] % 2
                wload_i[0] += 1
                wt, wk = wbuf[i], "wbuf%d" % i
                if dup:
                    def f(e, wt=wt):
                        a_ = e.dma_start(out=wt[:, :, 0:64], in_=w_in[:, col0:col0 + 64].rearrange("(c p) n -> p c n", p=128))
                        b_ = e.dma_start(out=wt[:, :, 64:128], in_=w_in[:, col0:col0 + 64].rearrange("(c p) n -> p c n", p=128))
                        return [a_, b_]
                    S.dma("pool", f, writes=[wk], n=2)
                else:
                    S.dma("pool", lambda e, wt=wt: e.dma_start(
                        out=wt[:, :, 0:ncol], in_=w_in[:, col0:col0 + ncol].rearrange("(c p) n -> p c n", p=128)), writes=[wk])
                return wt, wk

            def proj_tok(wt, wk, ncol, lb, pi):
                hk = "hT%d" % (lb // 4)

                def mm(e):
                    r = None
                    for kc in range(KC):
                        r = e.matmul(ps[pi][:, 0:ncol], hT[:, kc, lb * 128:(lb + 1) * 128], wt[:, kc, 0:ncol],
                                     start=(kc == 0), stop=(kc == KC - 1))
                    return r
                S.op("pe", mm, reads=[hk, wk], writes=[PK[pi]])

            def proj_feat(wt, wk, c0, hb, pi):
                def mm(e):
                    r = None
                    for kc in range(KC):
                        r = e.matmul(ps[pi][:, :], wt[:, kc, c0:c0 + 128], hT[:, kc, hb * 512:(hb + 1) * 512],
                                     start=(kc == 0), stop=(kc == KC - 1))
                    return r
                S.op("pe", mm, reads=["hT%d" % hb, wk], writes=[PK[pi]])

            def stage_xdma(at):
                S.dma("sp", lambda e: e.dma_start(
                    out=xs[:], in_=xk[at * 512:(at + 1) * 512, :].rearrange("(b p) n -> p b n", p=128)), writes=["xs"])

            def stage_xnorm():
                for b in range(4):
                    S.op("act", lambda e, b=b: e.activation(out=junk[:], in_=xs[:, b, :], func=AF.Square,
                                                            accum_out=ssq[:, b:b + 1]),
                         reads=["xs"], writes=["junk", "ssq"])
                rstd_from(ssq[:, 0:4], D, "ssq", "rstd", rstd[:, 0:4])
                for b in range(4):
                    S.op("dve", lambda e, b=b: e.tensor_scalar(
                        out=xs[:, b, :], in0=xs[:, b, :], scalar1=rstd[:, b:b + 1], scalar2=None, op0=ALU.mult),
                        reads=["rstd", "xs"], writes=["xs"])

            def stage_T(hb):
                for kc in range(KC):
                    pi = nextps()

                    def tr(e, kc=kc, pi=pi):
                        r = None
                        for b in range(4):
                            r = e.transpose(ps[pi][:, b * 128:(b + 1) * 128], xs[:, b, kc * 128:(kc + 1) * 128], ident[:])
                        return r
                    S.op("pe", tr, reads=["xs", "ident"], writes=[PK[pi]])
                    evac_scale(hT[:, kc, hb * 512:(hb + 1) * 512], ps[pi][:, :], G1[:, kc:kc + 1], modT[:, kc:kc + 1],
                               [PK[pi], "G1", "modT"], ["hT%d" % hb])

            def stage_P(at, hb, nxt):
                own = (at % 2 == 1)
                oj = at // 2
                if own:
                    wt, wk = load_w(0, 512)
                    for b in range(4):
                        lb = hb * 4 + b
                        tb = oj * 4 + b
                        pi = nextps()
                        proj_tok(wt, wk, 512, lb, pi)
                        t32, tk = tm32[tb % 2], "tm32_%d" % (tb % 2)
                        S.op("act", lambda e, pi=pi: e.activation(out=junk[:, 0:512], in_=ps[pi][:, :], func=AF.Square,
                                                                  accum_out=sm[:, 0:1]),
                             reads=[PK[pi]], writes=["junk", "sm"])
                        rstd_from(sm[:, 0:1], 512.0, "sm", "sm1", sm[:, 1:2])
                        S.op("dve", lambda e, pi=pi, t32=t32: e.tensor_scalar(out=t32[:], in0=ps[pi][:, :], scalar1=sm[:, 1:2],
                                                                             scalar2=None, op0=ALU.mult),
                             reads=[PK[pi], "sm1"], writes=[tk])
                        pj = nextps()

                        def tr(e, t32=t32, pj=pj):
                            r = None
                            for c4 in range(4):
                                r = e.transpose(ps[pj][:, c4 * 128:(c4 + 1) * 128], t32[:, c4 * 128:(c4 + 1) * 128], ident[:])
                            return r
                        S.op("pe", tr, reads=[tk, "ident"], writes=[PK[pj]])
                        for c4 in range(4):
                            evac_scale(cqT[:, c4, tb * 128:(tb + 1) * 128], ps[pj][:, c4 * 128:(c4 + 1) * 128],
                                       qng[:, c4:c4 + 1], None, [PK[pj], "qng"], ["cqT"])
                wt, wk = load_w(512, 336)
                for b in range(4):
                    lb = hb * 4 + b
                    tb = oj * 4 + b
                    ab = at * 4 + b
                    pi = nextps()
                    proj_tok(wt, wk, 336, lb, pi)
                    t32, tk = tm32[lb % 2], "tm32_%d" % (lb % 2)
                    S.op("act", lambda e, pi=pi: e.activation(out=junk[:, 0:128], in_=ps[pi][:, 0:128], func=AF.Square,
                                                              accum_out=sm[:, 2:3]),
                         reads=[PK[pi]], writes=["junk", "sm2"])
                    rstd_from(sm[:, 2:3], 128.0, "sm2", "sm3", sm[:, 3:4])
                    S.op("dve", lambda e, pi=pi, t32=t32: e.tensor_scalar(out=t32[:, 0:128], in0=ps[pi][:, 0:128], scalar1=sm[:, 3:4],
                                                                         scalar2=None, op0=ALU.mult),
                         reads=[PK[pi], "sm3"], writes=[tk])
                    S.op("act", lambda e, pi=pi, ab=ab: e.activation(out=va[:, ab, :], in_=ps[pi][:, 128:256], func=AF.Copy),
                         reads=[PK[pi]], writes=["va"])
                    if own:
                        S.op("dve", lambda e, pi=pi, tb=tb: e.tensor_scalar(out=widx[:, tb, :], in0=ps[pi][:, 320:336],
                                                                           scalar1=1.0 / 32.0, scalar2=None, op0=ALU.mult),
                             reads=[PK[pi]], writes=["widx"])
                    pj = nextps()
                    S.op("pe", lambda e, t32=t32, pj=pj: e.transpose(ps[pj][:, 0:128], t32[:, 0:128], ident[:]),
                         reads=[tk, "ident"], writes=[PK[pj]])
                    evac_scale(kaT[:, ab * 128:(ab + 1) * 128], ps[pj][:, 0:128], kgain[:, 0:1], None,
                               [PK[pj], "kgain"], ["kaT"])
                wt, wk = load_w(768, 64, dup=True)
                pi = nextps()
                proj_feat(wt, wk, 0, hb, pi)
                evac_scale(kiT[:, at * 512:(at + 1) * 512], ps[pi][:, :], 1.0, None, [PK[pi]], ["kiT"])
                if nxt is not None:
                    stage_xnorm()
                fm = ([("q", 848)] if own else []) + [("k", 1872)]
                for kind, col0 in fm:
                    for half in range(2):
                        wt, wk = load_w(col0 + half * 512, 512)
                        for hh in range(4):
                            hd = half * 4 + hh
                            pi = nextps()
                            proj_feat(wt, wk, hh * 128, hb, pi)
                            si = evq[0] % 3
                            st, sk = stg[si], "stg%d" % si
                            evac_scale(st[:], ps[pi][:, :], (float(HD) ** -0.5) if kind == "q" else 1.0, None, [PK[pi]], [sk])
                            if kind == "q":
                                S.dma("sp", lambda e, st=st, hd=hd: e.dma_start(
                                    out=qbT_s[hd, :, oj * 512:(oj + 1) * 512], in_=st[:]), reads=[sk], writes=["qbT_s"])
                            else:
                                S.dma("sp", lambda e, st=st, hd=hd: e.dma_start(
                                    out=kbT_s[hd, :, at * 512:(at + 1) * 512], in_=st[:]), reads=[sk], writes=["kbT_s"])
                for half in range(2):
                    wt, wk = load_w(2896 + half * 512, 512)
                    for b in range(4):
                        lb = hb * 4 + b
                        ab = at * 4 + b
                        pi = nextps()
                        proj_tok(wt, wk, 512, lb, pi)
                        si = evq[0] % 3
                        st, sk = stg[si], "stg%d" % si
                        evac_scale(st[:], ps[pi][:, :], 1.0, None, [PK[pi]], [sk])
                        S.dma("sp", lambda e, st=st, ab=ab, half=half: e.dma_start(
                            out=vb_s[ab * 128:(ab + 1) * 128, half * 512:(half + 1) * 512], in_=st[:]),
                            reads=[sk], writes=["vb_s"])

            tile_seq = [1, 3, 5, 7, 0, 2, 4, 6]
            stage_xdma(tile_seq[0])
            stage_xnorm()
            stage_T(0)
            for idx, at in enumerate(tile_seq):
                hb = idx % 2
                nxt = tile_seq[idx + 1] if idx + 1 < len(tile_seq) else None
                if nxt is not None:
                    stage_xdma(nxt)
                stage_P(at, hb, nxt)
                if nxt is not None:
                    stage_T(1 - hb)
            S.emit()

        if stop == 1:
            finish(S, [("kaT", kaT, [128, SA], BF16), ("va", va, [128, 32, 128], BF16), ("kiT", kiT, [128, SA], BF16),
                       ("widx", widx, [128, 16, 16], F32), ("cqT", cqT, [128, 4, NOWN], BF16), ("maskS", maskS, [128, 4, 512], BF16),
                       ("maskI", maskI, [128, 4, 512], BF16), ("maskV", maskV, [128, 512], BF16)])
            c2.close()
            ats.close()
            return nc
        with contextlib.ExitStack() as p2:
            wqu = sb(p2, "wqu", [128, 4, 2048], BF16)
            qa32 = sb(p2, "qa32", [128, 4, 1024], F32)
            sqb = [sb(p2, "sqb%d" % i, [128, 512], BF16) for i in range(2)]
            rsb = [sb(p2, "rsb%d" % i, [128, 512], F32) for i in range(2)]
            qsc = sb(p2, "qsc", [128, 1], F32)
            stg2 = [sb(p2, "stg2_%d" % i, [128, 512], BF16) for i in range(3)]
            S.op("dve", lambda e: e.tensor_scalar(out=qsc[:], in0=qgain[:], scalar1=float(HD) ** -0.5, scalar2=None,
                                                  op0=ALU.mult), reads=["qgain"], writes=["qsc"])

            def ldq(e):
                return [e.dma_start(out=wqu[:, :, hf * 1024:(hf + 1) * 1024],
                                    in_=w_qup[:, hf * 1024:(hf + 1) * 1024].rearrange("(c p) n -> p c n", p=128)) for hf in range(2)]
            S.dma("pool", ldq, writes=["wqu"], n=2)
            q2 = [0]
            for li in range(4):
                for b in range(4):
                    tb = li * 4 + b
                    for half in range(2):
                        pi = nextps()

                        def mm(e, tb=tb, half=half, pi=pi):
                            r = None
                            for kc in range(4):
                                r = e.matmul(ps[pi][:, :], cqT[:, kc, tb * 128:(tb + 1) * 128],
                                             wqu[:, kc, half * 512:(half + 1) * 512], start=(kc == 0), stop=(kc == 3))
                            return r
                        S.op("pe", mm, reads=["cqT", "wqu"], writes=[PK[pi]])
                        if (b + half) % 2 == 0:
                            S.op("dve", lambda e, pi=pi, b=b, half=half: e.tensor_copy(
                                out=qa32[:, b, half * 512:(half + 1) * 512], in_=ps[pi][:, :]), reads=[PK[pi]], writes=["qa32"])
                        else:
                            S.op("act", lambda e, pi=pi, b=b, half=half: e.activation(
                                out=qa32[:, b, half * 512:(half + 1) * 512], in_=ps[pi][:, :], func=AF.Copy), reads=[PK[pi]], writes=["qa32"])
                for hd in range(8):
                    pi = nextps()
                    k2 = q2[0] % 2
                    q2[0] += 1

                    def tr(e, hd=hd, pi=pi):
                        r = None
                        for b in range(4):
                            r = e.transpose(ps[pi][:, b * 128:(b + 1) * 128], qa32[:, b, hd * 128:(hd + 1) * 128], ident[:])
                        return r
                    S.op("pe", tr, reads=["qa32", "ident"], writes=[PK[pi]])
                    S.op("act", lambda e, pi=pi, k2=k2: e.activation(out=sqb[k2][:], in_=ps[pi][:, :], func=AF.Square),
                         reads=[PK[pi]], writes=["sqb%d" % k2])
                    pj = nextps()
                    S.op("pe", lambda e, pj=pj, k2=k2: e.matmul(ps[pj][:, :], onesb[:], sqb[k2][:], start=True, stop=True),
                         reads=["sqb%d" % k2, "onesb"], writes=[PK[pj]])
                    S.op("act", lambda e, pj=pj, k2=k2: e.activation(out=rsb[k2][:], in_=ps[pj][:, :], func=AF.Sqrt, scale=1.0 / 128.0, bias=EPS),
                         reads=[PK[pj]], writes=["rsb%d" % k2])
                    S.op("dve", lambda e, k2=k2: e.reciprocal(out=rsb[k2][:], in_=rsb[k2][:]), reads=["rsb%d" % k2], writes=["rsb%d" % k2])
                    si = q2[0] % 3
                    st, sk = stg2[si], "stg2_%d" % si
                    S.op("dve", lambda e, pi=pi, k2=k2, st=st: e.scalar_tensor_tensor(out=st[:], in0=ps[pi][:, :], scalar=qsc[:, 0:1],
                                                                                    in1=rsb[k2][:], op0=ALU.mult, op1=ALU.mult),
                         reads=[PK[pi], "qsc", "rsb%d" % k2], writes=[sk])
                    S.dma("sp", lambda e, st=st, hd=hd, li=li: e.dma_start(out=qaT_s[li, :, hd, :], in_=st[:]),
                          reads=[sk], writes=["qaT_s"])
                for pr in range(8):
                    pi = nextps()

                    def mm(e, pr=pr, li=li, pi=pi):
                        r = None
                        for kc in range(4):
                            r = e.matmul(ps[pi][:, :], wqu[:, kc, 1024 + pr * 128: 1024 + (pr + 1) * 128],
                                         cqT[:, kc, li * 512:(li + 1) * 512], start=(kc == 0), stop=(kc == 3))
                        return r
                    S.op("pe", mm, reads=["cqT", "wqu"], writes=[PK[pi]])
                    q2[0] += 1
                    si = q2[0] % 3
                    st, sk = stg2[si], "stg2_%d" % si
                    evac_scale(st[:], ps[pi][:, :], 1.0, None, [PK[pi]], [sk])
                    S.dma("sp", lambda e, st=st, pr=pr, li=li: e.dma_start(out=qiT_s[li, :, pr, :], in_=st[:]),
                          reads=[sk], writes=["qiT_s"])
            S.emit()
        c2.close()
        if stop == 2:
            finish(S, [])
            ats.close()
            return nc
        oaT = sb(ats, "oaT", [128, 8, NOWN], BF16)

        with contextlib.ExitStack() as p3:
            IT = sb(p3, "IT", [128, 32, 512], BF16)
            Wh = sb(p3, "Wh", [128, 8, 1024], BF16)
            tb32 = [sb(p3, "tb32_%d" % i, [128, 1024], F32) for i in range(1)]
            rb31 = sb(p3, "rb31_t", [128, 8], F32)
            nrb31 = sb(p3, "nrb31", [128, 8], F32)
            Wdiag = sb(p3, "Wdiag", [128, 4, 16, 128], BF16)
            qi = [sb(p3, "qi%d" % i, [128, 8, 512], BF16) for i in range(1)]
            qa = [sb(p3, "qa%d" % i, [128, 8, 512], BF16) for i in range(1)]
            rl = [sb(p3, "rl%d" % i, [128, 512], BF16) for i in range(4)]
            pt = [sb(p3, "pt%d" % i, [128, 512], BF16) for i in range(4)]
            pm = [sb(p3, "pm%d" % i, [128, 512], BF16) for i in range(4)]
            cmpb = [sb(p3, "cmp%d" % i, [128, 512], BF16) for i in range(4)]
            lo = sb(p3, "lo", [128, 512], F32)
            stp = sb(p3, "stp", [128, 512], F32)
            midb = sb(p3, "midb", [128, 512], BF16)
            rden = sb(p3, "rden", [128, 512], F32)
            penV = sb(p3, "penV", [128, 512], BF16)
            penI = sb(p3, "penI", [128, 4, 512], BF16)

            S.dma("sp", lambda e: e.dma_start(out=rb31[:], in_=rb31_d), writes=["rb31"])
            S.op("dve", lambda e: e.tensor_scalar(out=nrb31[:], in0=rb31[:], scalar1=-1.0, scalar2=None, op0=ALU.mult),
                 reads=["rb31"], writes=["nrb31"])
            S.op("dve", lambda e: e.tensor_scalar(out=penV[:], in0=maskV[:], scalar1=-1.0, scalar2=BIG, op0=ALU.add, op1=ALU.mult),
                 reads=["maskV"], writes=["penV"])
            for i in range(4):
                S.op("dve", lambda e, i=i: e.tensor_scalar(out=penI[:, i, :], in0=maskI[:, i, :], scalar1=-1.0, scalar2=BIG,
                                                           op0=ALU.add, op1=ALU.mult), reads=["maskI"], writes=["penI"])
            for h in range(8):
                tt_, tk = tb32[0], "tb32_0"
                S.dma("sp", lambda e, tt_=tt_, h=h: e.dma_start(out=tt_[:], in_=tb_d[h]), writes=[tk])
                S.op("act", lambda e, tt_=tt_, h=h: e.activation(out=Wh[:, h, :], in_=tt_[:], func=AF.Exp, bias=nrb31[:, h:h + 1]),
                     reads=[tk, "nrb31"], writes=["Wh"])
                S.op("pool", lambda e, h=h: e.affine_select(out=Wh[:, h, :], in_=Wh[:, h, :], pattern=[[1, 1024]],
                                                            compare_op=ALU.is_ge, fill=0.0, base=-384, channel_multiplier=-1),
                     reads=["Wh"], writes=["Wh"])

            W0 = 16.0
            rot = [0]
            SCB = [0, 1, 7]

            def dsa_tile(j):
                nkt = 2 * j + 2
                nkb = 4 * nkt
                qit, qik = qi[0], "qi0"
                qat, qak = qa[0], "qa0"
                S.dma("sp", lambda e: e.dma_start(out=qit[:], in_=qiT_s[j]), reads=["qiT_s"], writes=[qik])
                S.dma("sp", lambda e: e.dma_start(out=qat[:], in_=qaT_s[j]), reads=["qaT_s"], writes=[qak])
                for qb in range(4):
                    for h in range(16):
                        S.op("dve", lambda e, qb=qb, h=h: e.tensor_scalar(
                            out=Wdiag[:, qb, h, :], in0=identb[:], scalar1=widx[:, j * 4 + qb, h:h + 1], scalar2=None, op0=ALU.mult),
                            reads=["identb", "widx"], writes=["Wdiag"])
                its = [(kt, qb, h) for kt in range(nkt) for qb in range(4) for h in range(16)]
                stt = {}

                def stageSC(i):
                    kt, qb, h = its[i]
                    g = rot[0]
                    rot[0] += 1
                    scb, rli = SCB[g % 3], g % 4
                    p0_ = 64 * (h % 2)
                    stt[i] = (scb, rli)
                    S.op("pe", lambda e: e.matmul(ps[scb][:, :], qit[p0_:p0_ + 64, h // 2, qb * 128:(qb + 1) * 128],
                                                  kiT[p0_:p0_ + 64, kt * 512:(kt + 1) * 512], start=True, stop=True),
                         reads=[qik, "kiT"], writes=[PK[scb]])
                    rlt, rlk = rl[rli], "rl%d" % rli
                    if g % 2 == 0:
                        S.op("act", lambda e: e.activation(out=rlt[:], in_=ps[scb][:, :], func=AF.Relu), reads=[PK[scb]], writes=[rlk])
                    else:
                        S.op("dve", lambda e: e.tensor_scalar(out=rlt[:], in0=ps[scb][:, :], scalar1=0.0, scalar2=None, op0=ALU.max),
                             reads=[PK[scb]], writes=[rlk])

                def stageACC(i):
                    kt, qb, h = its[i]
                    scb, rli = stt[i]
                    rlt, rlk = rl[rli], "rl%d" % rli

                    def acc(e):
                        r = None
                        for sbk in range(4):
                            r = e.matmul(ps[2 + sbk][:, qb * 128:(qb + 1) * 128], rlt[:, sbk * 128:(sbk + 1) * 128],
                                         Wdiag[:, qb, h, :], start=(h == 0), stop=(h == 15))
                        return r
                    S.op("pe", acc, reads=[rlk, "Wdiag"], writes=[PK[2], PK[3], PK[4], PK[5]])
                    if qb == 3 and h == 15:
                        for sbk in range(4):
                            kb = kt * 4 + sbk
                            if kt == 0 or kt == nkt - 1:
                                mk = maskV[:] if kt == 0 else maskI[:, sbk, :]
                                pn = penV[:] if kt == 0 else penI[:, sbk, :]
                                S.op("dve", lambda e, kb=kb, sbk=sbk, mk=mk: e.tensor_tensor(out=IT[:, kb, :], in0=ps[2 + sbk][:, :], in1=mk,
                                                                                              op=ALU.mult),
                                     reads=[PK[2 + sbk], "maskV", "maskI"], writes=["IT"])
                                S.op("dve", lambda e, kb=kb, pn=pn: e.tensor_tensor(out=IT[:, kb, :], in0=IT[:, kb, :], in1=pn, op=ALU.add),
                                     reads=["IT", "penV", "penI"], writes=["IT"])
                            else:
                                S.op("act", lambda e, kb=kb, sbk=sbk: e.activation(out=IT[:, kb, :], in_=ps[2 + sbk][:, :], func=AF.Copy),
                                     reads=[PK[2 + sbk]], writes=["IT"])
                for i in range(len(its) + 1):
                    if i < len(its):
                        stageSC(i)
                    if i >= 1:
                        stageACC(i - 1)
                S.op("pool", lambda e: e.memset(lo[:], -W0), writes=["lo"])
                for it in range(NBIS + 1):
                    hk = W0 / (2.0 ** it)
                    final = (it == NBIS)
                    S.op("dve", lambda e, hk=hk, final=final: e.tensor_scalar(out=midb[:], in0=lo[:], scalar1=(0.0 if final else hk),
                                                                              scalar2=None, op0=ALU.add),
                         reads=["lo"], writes=["midb"])
                    if final:
                        for kb in range(nkb):
                            S.op("dve", lambda e, kb=kb: e.tensor_tensor(out=IT[:, kb, :], in0=IT[:, kb, :], in1=midb[:], op=ALU.is_ge),
                                 reads=["IT", "midb"], writes=["IT"])
                        break
                    for kb in range(nkb):
                        ci = kb % 4
                        S.op("dve", lambda e, kb=kb, ci=ci: e.tensor_tensor(out=cmpb[ci][:], in0=IT[:, kb, :], in1=midb[:], op=ALU.is_ge),
                             reads=["IT", "midb"], writes=["cmp%d" % ci])
                        S.op("pe", lambda e, kb=kb, ci=ci: e.matmul(ps[6][:, :], onesb[:], cmpb[ci][:], start=(kb == 0), stop=(kb == nkb - 1)),
                             reads=["cmp%d" % ci, "onesb"], writes=[PK[6]])
                    S.op("dve", lambda e, hk=hk: e.tensor_scalar(out=stp[:], in0=ps[6][:, :], scalar1=TOPK - 0.5, scalar2=hk,
                                                                 op0=ALU.is_ge, op1=ALU.mult), reads=[PK[6]], writes=["stp"])
                    S.op("dve", lambda e: e.tensor_tensor(out=lo[:], in0=lo[:], in1=stp[:], op=ALU.add), reads=["lo", "stp"], writes=["lo"])
                for h in range(8):
                    dsa_head(j, h, nkb, qat, qak)

            def dsa_head(j, h, nkb, qat, qak):
                ob, db = 2 + 2 * (h % 2), 3 + 2 * (h % 2)
                sst = {}

                def stageLG(kb):
                    g = rot[0]
                    rot[0] += 1
                    lgb, pi_ = SCB[g % 3], g % 4
                    sst[kb] = pi_
                    S.op("pe", lambda e: e.matmul(ps[lgb][:, :], kaT[:, kb * 128:(kb + 1) * 128], qat[:, h, :], start=True, stop=True),
                         reads=["kaT", qak], writes=[PK[lgb]])
                    S.op("act", lambda e: e.activation(out=pt[pi_][:], in_=ps[lgb][:, :], func=AF.Exp), reads=[PK[lgb]], writes=["pt%d" % pi_])
                    S.op("dve", lambda e: e.tensor_tensor(out=pm[pi_][:], in0=pt[pi_][:], in1=IT[:, kb, :], op=ALU.mult),
                         reads=["pt%d" % pi_, "IT"], writes=["pm%d" % pi_])
                    r_ = kb - (nkb - 4)
                    if r_ >= -1:
                        u0 = 384 - 128 * r_
                        S.op("dve", lambda e: e.tensor_tensor(out=pm[pi_][:], in0=pm[pi_][:], in1=Wh[:, h, u0:u0 + 512], op=ALU.mult),
                             reads=["pm%d" % pi_, "Wh"], writes=["pm%d" % pi_])

                def stagePV(kb):
                    pi_ = sst[kb]

                    def pv(e):
                        e.matmul(ps[ob][:, :], va[:, kb, :], pm[pi_][:], start=(kb == 0), stop=(kb == nkb - 1))
                        return e.matmul(ps[db][:, :], onesb[:], pm[pi_][:], start=(kb == 0), stop=(kb == nkb - 1))
                    S.op("pe", pv, reads=["va", "pm%d" % pi_, "onesb"], writes=[PK[ob], PK[db]])
                for step in range(nkb + 2):
                    if step < nkb:
                        stageLG(step)
                    if 0 <= step - 2 < nkb:
                        stagePV(step - 2)
                S.op("dve", lambda e: e.reciprocal(out=rden[:], in_=ps[db][:, :]), reads=[PK[db]], writes=["rden"])
                S.op("dve", lambda e: e.tensor_tensor(out=oaT[:, h, j * 512:(j + 1) * 512], in0=ps[ob][:, :], in1=rden[:], op=ALU.mult),
                     reads=[PK[ob], "rden"], writes=["oaT"])

            for j in range(4):
                dsa_tile(j)
            S.emit()

        if stop == 3:
            finish(S, [("oaT", oaT, [128, 8, NOWN], BF16)])
            ats.close()
            return nc
        obT = sb(ats, "obT", [128, 8, NOWN], BF16)

        def oTc(kc, sl):
            return (oaT if kc < 8 else obT)[:, kc % 8, sl]

        with contextlib.ExitStack() as p4:
            kbt = [sb(p4, "kbt%d" % i, [128, SA], BF16) for i in range(2)]
            vbt = [sb(p4, "vbt%d" % i, [128, 32, 128], BF16) for i in range(2)]
            qbt = [sb(p4, "qbt%d" % i, [128, NOWN], BF16) for i in range(2)]
            NZ, NL, NC_, NA = 2, 4, 3, 4
            ez = [sb(p4, "ez%d" % i, [128, 512], F32) for i in range(NZ)]
            Lp = [sb(p4, "Lp%d" % i, [128, 512], BF16) for i in range(NL)]
            Cc = [sb(p4, "Cc%d" % i, [128, 512], BF16) for i in range(NC_)]
            at_ = [sb(p4, "at%d" % i, [128, 512], BF16) for i in range(NA)]
            zero_c = sb(p4, "zero_c", [128, 512], BF16)
            S.op("pool", lambda e: e.memset(zero_c[:], 0.0), writes=["zero_c"])
            gi = [0]
            cci = [0]
            for h in range(8):
                b_ = h % 2
                S.dma("sp", lambda e, b_=b_, h=h: e.dma_start(out=kbt[b_][:], in_=kbT_s[h]), reads=["kbT_s"], writes=["kbt%d" % b_])
                S.dma("sp", lambda e, b_=b_, h=h: e.dma_start(out=qbt[b_][:], in_=qbT_s[h]), reads=["qbT_s"], writes=["qbt%d" % b_])
                S.dma("sp", lambda e, b_=b_, h=h: e.dma_start(
                    out=vbt[b_][:], in_=vb_s[:, h * 128:(h + 1) * 128].rearrange("(b p) d -> p b d", p=128)),
                    reads=["vb_s"], writes=["vbt%d" % b_])
                kk, qk, vk = "kbt%d" % b_, "qbt%d" % b_, "vbt%d" % b_
                def sb_tile(h, j, b_, kk, qk, vk):
                    nkb = 8 * (j + 1)
                    obk = 4 + (h * 4 + j) % 2
                    order = list(reversed(range(nkb)))
                    n = len(order)
                    qslc = qbt[b_][:, j * 512:(j + 1) * 512]
                    st = {}

                    def maskof(kb):
                        if kb < 4:
                            return maskV[:]
                        if kb >= nkb - 4:
                            return maskS[:, kb - (nkb - 4), :]
                        return None

                    def stageZ(i):
                        kb = order[i]
                        g = gi[0]
                        gi[0] += 1
                        zb, li_ = g % NZ, g % NL
                        kslc = kbt[b_][:, kb * 128:(kb + 1) * 128]
                        st[i] = dict(kb=kb, zb=zb, li=li_, ai=g % NA, eb=2 + g % 2, kslc=kslc, mk=maskof(kb))
                        S.op("pe", lambda e: e.matmul(ps[zb][:, :], kslc, qslc, start=True, stop=True), reads=[kk, qk], writes=[PK[zb]])
                        S.op("act", lambda e: e.activation(out=ez[zb][:], in_=ps[zb][:, :], func=AF.Exp), reads=[PK[zb]], writes=["ez%d" % zb])
                        S.op("act", lambda e: e.activation(out=Lp[li_][:], in_=ez[zb][:], func=AF.Ln, bias=1.0),
                             reads=["ez%d" % zb], writes=["Lp%d" % li_])
                        mk = st[i]["mk"]
                        if mk is not None:
                            S.op("dve", lambda e: e.tensor_tensor(out=Lp[li_][:], in0=Lp[li_][:], in1=mk, op=ALU.mult),
                                 reads=["Lp%d" % li_, "maskV", "maskS"], writes=["Lp%d" % li_])
                        if i == 0:
                            st[i]["cc"] = None
                        else:
                            prev = st[i - 1]
                            c_new = cci[0] % NC_
                            cci[0] += 1
                            pl = prev["li"]
                            if prev["cc"] is None:
                                S.op("pool", lambda e: e.tensor_copy(out=Cc[c_new][:], in_=Lp[pl][:]), reads=["Lp%d" % pl], writes=["Cc%d" % c_new])
                            else:
                                pc = prev["cc"]
                                S.op("pool", lambda e: e.tensor_tensor(out=Cc[c_new][:], in0=Cc[pc][:], in1=Lp[pl][:], op=ALU.add),
                                     reads=["Cc%d" % pc, "Lp%d" % pl], writes=["Cc%d" % c_new])
                            st[i]["cc"] = c_new

                    def stageE(i):
                        s_ = st[i]
                        eb, kslc, li_, cc, ai = s_["eb"], s_["kslc"], s_["li"], s_["cc"], s_["ai"]
                        ccap = zero_c[:] if cc is None else Cc[cc][:]
                        cck = "zero_c" if cc is None else "Cc%d" % cc

                        def em(e):
                            e.matmul(ps[eb][:, :], kslc, qslc, start=True, stop=False)
                            e.matmul(ps[eb][:, :], negtri[:], Lp[li_][:], start=False, stop=False)
                            return e.matmul(ps[eb][:, :], negones[:], ccap, start=False, stop=True)
                        S.op("pe", em, reads=[kk, qk, "negtri", "negones", "Lp%d" % li_, cck], writes=[PK[eb]])
                        S.op("act", lambda e: e.activation(out=at_[ai][:], in_=ps[eb][:, :], func=AF.Exp), reads=[PK[eb]], writes=["at%d" % ai])
                        mk = s_["mk"]
                        if mk is not None:
                            S.op("dve", lambda e: e.tensor_tensor(out=at_[ai][:], in0=at_[ai][:], in1=mk, op=ALU.mult),
                                 reads=["at%d" % ai, "maskV", "maskS"], writes=["at%d" % ai])

                    def stageV(i):
                        s_ = st[i]
                        kb, ai = s_["kb"], s_["ai"]
                        S.op("pe", lambda e: e.matmul(ps[obk][:, :], vbt[b_][:, kb, :], at_[ai][:], start=(i == 0), stop=(i == n - 1)),
                             reads=[vk, "at%d" % ai], writes=[PK[obk]])

                    for step in range(n + 2):
                        if step < n:
                            stageZ(step)
                        if 0 <= step - 1 < n:
                            stageE(step - 1)
                        if 0 <= step - 2 < n:
                            stageV(step - 2)
                    S.op("act", lambda e, obk=obk, h=h, j=j: e.activation(out=obT[:, h, j * 512:(j + 1) * 512], in_=ps[obk][:, :],
                                                                         func=AF.Copy), reads=[PK[obk]], writes=["obT"])
                for j in range(4):
                    sb_tile(h, j, b_, kk, qk, vk)
            S.emit()
        if stop == 4:
            finish(S, [("oaT", oaT, [128, 8, NOWN], BF16), ("obT", obT, [128, 8, NOWN], BF16)])
            ats.close()
            return nc
        with contextlib.ExitStack() as p5:
            wo = sb(p5, "wo", [128, KC, D], BF16)
            sq = [sb(p5, "sq%d" % i, [128, 512], BF16) for i in range(2)]
            rs = sb(p5, "rs", [128, 512], F32)
            xs5 = [sb(p5, "xs5_%d" % i, [128, D], F32) for i in range(1)]
            t5 = [sb(p5, "t5_%d" % i, [128, D], F32) for i in range(1)]
            for cg in range(4):
                S.dma("pool", lambda e, cg=cg: e.dma_start(out=wo[:, :, cg * 512:(cg + 1) * 512],
                                                           in_=w_out[:, cg * 512:(cg + 1) * 512].rearrange("(c p) n -> p c n", p=128)),
                      writes=["wo"])
            r5 = [0]
            for j in range(4):
                for g in range(2):
                    gn = gna if g == 0 else gnb
                    gk = "gna" if g == 0 else "gnb"
                    pb = r5[0] % 2
                    r5[0] += 1
                    for c_ in range(8):
                        si = c_ % 2
                        S.op("act", lambda e, si=si, g=g, c_=c_, j=j: e.activation(out=sq[si][:], in_=oTc(g * 8 + c_, slice(j * 512, (j + 1) * 512)),
                                                                                 func=AF.Square), reads=["oaT", "obT"], writes=["sq%d" % si])
                        S.op("pe", lambda e, pb=pb, si=si, c_=c_: e.matmul(ps[pb][:, :], onesb[:], sq[si][:], start=(c_ == 0), stop=(c_ == 7)),
                             reads=["sq%d" % si, "onesb"], writes=[PK[pb]])
                    S.op("act", lambda e, pb=pb: e.activation(out=rs[:], in_=ps[pb][:, :], func=AF.Sqrt, scale=1.0 / 1024.0, bias=EPS),
                         reads=[PK[pb]], writes=["rs"])
                    S.op("dve", lambda e: e.reciprocal(out=rs[:], in_=rs[:]), reads=["rs"], writes=["rs"])
                    for c_ in range(8):
                        S.op("dve", lambda e, g=g, c_=c_, j=j, gn=gn: e.scalar_tensor_tensor(
                            out=oTc(g * 8 + c_, slice(j * 512, (j + 1) * 512)), in0=oTc(g * 8 + c_, slice(j * 512, (j + 1) * 512)),
                            scalar=gn[:, c_:c_ + 1], in1=rs[:], op0=ALU.mult, op1=ALU.mult),
                            reads=["oaT", "obT", "rs", gk], writes=["oaT" if g == 0 else "obT"])
            for tb in range(16):
                at = 2 * (tb // 4) + 1
                r0 = at * 512 + (tb % 4) * 128
                xb, xbk = xs5[0], "xs5_0"
                tt_, ttk = t5[0], "t5_0"
                S.dma("sp", lambda e, xb=xb, r0=r0: e.dma_start(out=xb[:], in_=xk[r0:r0 + 128, :]), writes=[xbk])
                for cg in range(4):
                    pb = 2 + r5[0] % 4
                    r5[0] += 1

                    def mm(e, pb=pb, tb=tb, cg=cg):
                        r = None
                        for kc in range(KC):
                            r = e.matmul(ps[pb][:, :], oTc(kc, slice(tb * 128, (tb + 1) * 128)), wo[:, kc, cg * 512:(cg + 1) * 512],
                                         start=(kc == 0), stop=(kc == KC - 1))
                        return r
                    S.op("pe", mm, reads=["oaT", "obT", "wo"], writes=[PK[pb]])
                    S.op("dve", lambda e, pb=pb, tt_=tt_, cg=cg: e.tensor_tensor(out=tt_[:, cg * 512:(cg + 1) * 512], in0=ps[pb][:, :],
                                                                                in1=g1_bc[:, cg * 512:(cg + 1) * 512], op=ALU.mult),
                         reads=[PK[pb], "g1_bc"], writes=[ttk])
                S.op("pool", lambda e, tt_=tt_, xb=xb: e.tensor_tensor(out=tt_[:], in0=tt_[:], in1=xb[:], op=ALU.add),
                     reads=[ttk, xbk], writes=[ttk])
                S.dma("sp", lambda e, tt_=tt_, tb=tb: e.dma_start(out=x1_s[tb * 128:(tb + 1) * 128, :], in_=tt_[:]),
                      reads=[ttk], writes=["x1_s"])
            S.emit()
        if stop == 5:
            finish(S, [("oaT", oaT, [128, 8, NOWN], BF16), ("obT", obT, [128, 8, NOWN], BF16)])
            ats.close()
            return nc
        ats.close()

        with contextlib.ExitStack() as p6:
            x1t = sb(p6, "x1t", [128, 4, D], F32)
            h2T = sb(p6, "h2T", [128, KC, 512], BF16)
            acc = sb(p6, "acc", [128, 4, D], F32)
            h2T32 = acc[:].rearrange("p b (q n) -> p (b q) n", q=4)
            rt = sb(p6, "rt", [128, KC, 36], F32)
            wg = [sb(p6, "wg%d" % i, [128, KC, DE], BF16) for i in range(2)]
            wu = [sb(p6, "wu%d" % i, [128, KC, DE], BF16) for i in range(2)]
            wd = [sb(p6, "wd%d" % i, [128, 4, D], BF16) for i in range(2)]
            sg = [sb(p6, "sg%d" % i, [128, 512], BF16) for i in range(2)]
            uT = [sb(p6, "uT%d" % i, [128, 4, 512], BF16) for i in range(2)]
            rsm = sb(p6, "rsm", [128, 16], F32)
            lgB = sb(p6, "lgB", [128, 4, 36], F32)
            lemB = sb(p6, "lemB", [128, 4, 32], F32)
            m8B = sb(p6, "m8B", [128, 4, 32], F32)
            rsmB = sb(p6, "rsmB", [128, 4, 16], F32)
            import os
            P6CUT = int(os.environ.get("P6CUT", "9"))
            is1B = sb(p6, "is1B", [128, 4, 32], F32)
            is2B = sb(p6, "is2B", [128, 4, 32], F32)
            Gt = sb(p6, "Gt", [128, 4, 32], F32)
            S.dma("sp", lambda e: e.dma_start(out=rt[:], in_=router_d.rearrange("(c p) n -> p c n", p=128)), writes=["rt"])
            r6 = [0]
            ei = [0]
            def route_block(b):
                ops = []
                lg, rr = lgB[:, b, :], rsmB[:, b, :]
                i1, i2, lm, mm8 = is1B[:, b, :], is2B[:, b, :], lemB[:, b, :], m8B[:, b, :]

                def kx(keys):
                    return [k if k == "Gt" else "%s_%d" % (k, b) for k in keys]

                def dv(fn, reads, writes):
                    ops.append(("dve", fn, kx(reads), kx(writes)))

                def ac(fn, reads, writes):
                    ops.append(("act", fn, kx(reads), kx(writes)))
                TT = lambda o, a_, b_, op: (lambda e: e.tensor_tensor(out=o, in0=a_, in1=b_, op=op))
                dv(TT(rr[:, 0:2], lg[:, 0:2], lg[:, 2:4], ALU.max), ["lg"], ["rA"])
                dv(TT(rr[:, 8:9], rr[:, 0:1], rr[:, 1:2], ALU.max), ["rA"], ["r8"])
                dv(lambda e: e.tensor_scalar(out=rr[:, 9:10], in0=rr[:, 8:9], scalar1=-1.0, scalar2=None, op0=ALU.mult), ["r8"], ["r9"])
                ac(lambda e: e.activation(out=i1[:, 0:4], in_=lg[:, 0:4], func=AF.Exp, bias=rr[:, 9:10]), ["lg", "r9"], ["is1"])
                dv(TT(rr[:, 0:2], i1[:, 0:2], i1[:, 2:4], ALU.add), ["is1", "rA"], ["rA"])
                dv(TT(rr[:, 10:11], rr[:, 0:1], rr[:, 1:2], ALU.add), ["rA"], ["r10"])
                dv(lambda e: e.reciprocal(out=rr[:, 11:12], in_=rr[:, 10:11]), ["r10"], ["r11"])
                dv(lambda e: e.tensor_scalar(out=i2[:, 0:4], in0=lg[:, 0:4], scalar1=rr[:, 8:9], scalar2=None, op0=ALU.is_ge),
                   ["lg", "r8"], ["is2"])
                dv(lambda e: e.tensor_scalar(out=i2[:, 0:4], in0=i2[:, 0:4], scalar1=-1.0, scalar2=BIG, op0=ALU.add, op1=ALU.mult),
                   ["is2"], ["is2"])
                for g in range(4):
                    dv(lambda e, g=g: e.tensor_scalar(out=lm[:, g * 8:(g + 1) * 8], in0=lg[:, 4 + g * 8:12 + g * 8],
                                                      scalar1=i2[:, g:g + 1], scalar2=None, op0=ALU.add), ["lg", "is2"], ["lem"])

                def tree_max(outcol, okey):
                    dv(TT(mm8[:, 0:16], lm[:, 0:16], lm[:, 16:32], ALU.max), ["lem", "m8"], ["m8"])
                    dv(TT(mm8[:, 16:24], mm8[:, 0:8], mm8[:, 8:16], ALU.max), ["m8"], ["m8"])
                    dv(TT(mm8[:, 24:28], mm8[:, 16:20], mm8[:, 20:24], ALU.max), ["m8"], ["m8"])
                    dv(TT(mm8[:, 28:30], mm8[:, 24:26], mm8[:, 26:28], ALU.max), ["m8"], ["m8"])
                    dv(TT(outcol, mm8[:, 28:29], mm8[:, 29:30], ALU.max), ["m8"], [okey])
                tree_max(rr[:, 2:3], "l1")
                dv(lambda e: e.tensor_scalar(out=i1, in0=lm, scalar1=rr[:, 2:3], scalar2=None, op0=ALU.is_ge), ["lem", "l1"], ["is1"])
                dv(lambda e: e.scalar_tensor_tensor(out=lm, in0=i1, scalar=-BIG, in1=lm, op0=ALU.mult, op1=ALU.add),
                   ["is1", "lem"], ["lem"])
                tree_max(rr[:, 3:4], "l2")
                dv(lambda e: e.tensor_scalar(out=i2, in0=lm, scalar1=rr[:, 3:4], scalar2=None, op0=ALU.is_ge), ["lem", "l2"], ["is2"])
                dv(lambda e: e.tensor_scalar(out=rr[:, 12:13], in0=rr[:, 2:3], scalar1=-1.0, scalar2=None, op0=ALU.mult), ["l1"], ["r12"])
                ac(lambda e: e.activation(out=rr[:, 13:14], in_=rr[:, 3:4], func=AF.Exp, bias=rr[:, 12:13]), ["l2", "r12"], ["r13"])
                dv(lambda e: e.tensor_scalar(out=rr[:, 14:15], in0=rr[:, 13:14], scalar1=1.0, scalar2=None, op0=ALU.add), ["r13"], ["r14"])
                dv(lambda e: e.reciprocal(out=rr[:, 14:15], in_=rr[:, 14:15]), ["r14"], ["r14"])
                dv(TT(rr[:, 14:15], rr[:, 14:15], rr[:, 11:12], ALU.mult), ["r14", "r11"], ["r14"])
                dv(TT(rr[:, 15:16], rr[:, 14:15], rr[:, 13:14], ALU.mult), ["r14", "r13"], ["r15"])
                dv(lambda e: e.tensor_scalar(out=Gt[:, b, :], in0=i1, scalar1=rr[:, 14:15], scalar2=None, op0=ALU.mult), ["is1", "r14"], ["Gt"])
                dv(lambda e: e.scalar_tensor_tensor(out=Gt[:, b, :], in0=i2, scalar=rr[:, 15:16], in1=Gt[:, b, :],
                                                    op0=ALU.mult, op1=ALU.add), ["is2", "r15", "Gt"], ["Gt"])
                return ops

            for tt in range(4):
                S.dma("sp", lambda e, tt=tt: e.dma_start(out=x1t[:], in_=x1_s[tt * 512:(tt + 1) * 512, :].rearrange("(b p) n -> p b n", p=128)),
                      reads=["x1_s"], writes=["x1t"])
                P6SKIP = os.environ.get("P6SKIP", "")
                for b in range(0 if "sq" in P6SKIP else 4):
                    S.op("act", lambda e, b=b: e.activation(out=acc[:, 0, :], in_=x1t[:, b, :], func=AF.Square, accum_out=rsm[:, b:b + 1]),
                         reads=["x1t"], writes=["acc", "rsm"])
                S.op("act", lambda e: e.activation(out=rsm[:, 4:8], in_=rsm[:, 0:4], func=AF.Sqrt, scale=1.0 / D, bias=EPS),
                     reads=["rsm"], writes=["rsm4"])
                S.op("dve", lambda e: e.reciprocal(out=rsm[:, 4:8], in_=rsm[:, 4:8]), reads=["rsm4"], writes=["rsm4"])
                for b in range(4):
                    S.op("dve", lambda e, b=b: e.tensor_scalar(out=x1t[:, b, :], in0=x1t[:, b, :], scalar1=rsm[:, 4 + b:5 + b], scalar2=None,
                                                               op0=ALU.mult), reads=["x1t", "rsm4"], writes=["x1t"])
                for kc in range(0 if "tr" in P6SKIP else KC):
                    pi = 4 + r6[0] % 4
                    r6[0] += 1

                    def tr(e, kc=kc, pi=pi):
                        r = None
                        for b in range(4):
                            r = e.transpose(ps[pi][:, b * 128:(b + 1) * 128], x1t[:, b, kc * 128:(kc + 1) * 128], ident[:])
                        return r
                    S.op("pe", tr, reads=["x1t", "ident"], writes=[PK[pi]])
                    S.op("dve", lambda e, kc=kc, pi=pi: e.tensor_scalar(out=h2T[:, kc, :], in0=ps[pi][:, :], scalar1=G2[:, kc:kc + 1],
                                                                       scalar2=modT[:, 48 + kc:49 + kc], op0=ALU.mult, op1=ALU.add),
                         reads=[PK[pi], "G2", "modT"], writes=["h2T"])
                    S.op("act", lambda e, kc=kc, pi=pi: e.activation(out=h2T32[:, kc, :], in_=ps[pi][:, :], func=AF.Identity,
                                                                    scale=G2[:, kc:kc + 1], bias=modT[:, 48 + kc:49 + kc]),
                         reads=[PK[pi], "G2", "modT"], writes=["acc"])
                for b in range(4 if P6CUT >= 2 else 0):
                    pi = 4 + r6[0] % 4
                    r6[0] += 1

                    def rmm(e, b=b, pi=pi):
                        r = None
                        for kc in range(KC):
                            r = e.matmul(ps[pi][:, 0:36], h2T32[:, kc, b * 128:(b + 1) * 128], rt[:, kc, :], start=(kc == 0), stop=(kc == KC - 1))
                        return r
                    S.op("pe", rmm, reads=["acc", "rt"], writes=[PK[pi]])
                    S.op("dve", lambda e, pi=pi, b=b: e.tensor_copy(out=lgB[:, b, :], in_=ps[pi][:, 0:36]), reads=[PK[pi]], writes=["lg_%d" % b])
                pend = [route_block(b) for b in range(4)]
                for k in range(len(pend[0])):
                    for b in range(4):
                        eng_, fn_, rd_, wr_ = pend[b][k]
                        S.op(eng_, fn_, reads=rd_, writes=wr_)
                if "ms" not in P6SKIP:
                    S.dma("sp", lambda e, tt=tt: e.dma_start(out=acc[:], in_=x1_s[tt * 512:(tt + 1) * 512, :].rearrange("(b p) n -> p b n", p=128)),
                          reads=["x1_s"], writes=["acc"])
                for ex in range(NE if P6CUT >= 5 else (2 if P6CUT >= 4 else 0)):
                    wi = ei[0] % 2
                    ei[0] += 1
                    if not (os.environ.get("P6NODMA") and (tt > 0 or ex >= 2)):
                      S.dma("pool", lambda e, wi=wi, ex=ex: e.dma_start(out=wg[wi][:], in_=w_gate[ex].rearrange("(c p) n -> p c n", p=128)),
                          writes=["wg%d" % wi])
                      S.dma("pool", lambda e, wi=wi, ex=ex: e.dma_start(out=wu[wi][:], in_=w_up[ex].rearrange("(c p) n -> p c n", p=128)),
                          writes=["wu%d" % wi])
                      S.dma("pool", lambda e, wi=wi, ex=ex: [e.dma_start(
                        out=wd[wi][:, :, hf * 1024:(hf + 1) * 1024],
                        in_=w_down[ex][:, hf * 1024:(hf + 1) * 1024].rearrange("(c p) n -> p c n", p=128)) for hf in range(2)],
                        writes=["wd%d" % wi], n=2)
                    for fc in range(4):
                        S.op("dve", lambda e, wi=wi, fc=fc: e.tensor_tensor(out=wd[wi][:, fc, :], in0=wd[wi][:, fc, :], in1=g2_bc[:], op=ALU.mult),
                             reads=["wd%d" % wi, "g2_bc"], writes=["wd%d" % wi])
                    ut, utk = uT[wi], "uT%d" % wi
                    for fc in range(4):
                        ga, ua = 2 * (fc % 2), 1 + 2 * (fc % 2)

                        def gm(e, wi=wi, fc=fc, ga=ga, ua=ua):
                            for kc in range(KC):
                                e.matmul(ps[ga][:, :], wg[wi][:, kc, fc * 128:(fc + 1) * 128], h2T[:, kc, :], start=(kc == 0), stop=(kc == KC - 1))
                            r = None
                            for kc in range(KC):
                                r = e.matmul(ps[ua][:, :], wu[wi][:, kc, fc * 128:(fc + 1) * 128], h2T[:, kc, :], start=(kc == 0), stop=(kc == KC - 1))
                            return r
                        S.op("pe", gm, reads=["wg%d" % wi, "wu%d" % wi, "h2T"], writes=[PK[ga], PK[ua]])
                        S.op("act", lambda e, fc=fc, ga=ga: e.activation(out=sg[fc % 2][:], in_=ps[ga][:, :], func=AF.Silu),
                             reads=[PK[ga]], writes=["sg%d" % (fc % 2)])
                        S.op("dve", lambda e, fc=fc, ua=ua, ut=ut: e.tensor_tensor(out=ut[:, fc, :], in0=sg[fc % 2][:], in1=ps[ua][:, :], op=ALU.mult),
                             reads=["sg%d" % (fc % 2), PK[ua]], writes=[utk])
                    for b in range(4):
                        for dg in range(4):
                            yb = 4 + r6[0] % 4
                            r6[0] += 1

                            def dm(e, yb=yb, b=b, dg=dg, wi=wi, ut=ut):
                                r = None
                                for fc in range(4):
                                    r = e.matmul(ps[yb][:, :], ut[:, fc, b * 128:(b + 1) * 128], wd[wi][:, fc, dg * 512:(dg + 1) * 512],
                                                 start=(fc == 0), stop=(fc == 3))
                                return r
                            S.op("pe", dm, reads=[utk, "wd%d" % wi], writes=[PK[yb]])
                            S.op("dve", lambda e, yb=yb, b=b, dg=dg, ex=ex: e.scalar_tensor_tensor(
                                out=acc[:, b, dg * 512:(dg + 1) * 512], in0=ps[yb][:, :], scalar=Gt[:, b, ex:ex + 1],
                                in1=acc[:, b, dg * 512:(dg + 1) * 512], op0=ALU.mult, op1=ALU.add),
                                reads=[PK[yb], "Gt", "acc"], writes=["acc"])
                S.dma("sp", lambda e, tt=tt: e.dma_start(out=out_d[tt * 512:(tt + 1) * 512, :].rearrange("(b p) n -> p b n", p=128), in_=acc[:]),
                      reads=["acc"], writes=["out_d"])
            S.emit()
    return nc


def _t5_bucket(dist):
    max_exact = 16
    d = np.maximum(dist, 1).astype(np.float32)
    large = max_exact + (np.log(d / max_exact) / np.log(128 / max_exact) * (32 - max_exact)).astype(np.int32)
    large = np.minimum(large, 31)
    return np.where(dist < max_exact, dist, large)


_NC_CACHE = {}


def kernel(x, c, w_mod, b_mod, ln1_g, w_in, q_norm_g, w_q_up, q_gain, k_gain, rel_bias, gn_a, gn_b, w_out, ln2_g,
           router_g, router_e, w_gate, w_up, w_down):
    f32 = np.float32
    x = np.asarray(x, f32)
    c = np.asarray(c, f32)

    def col(v, n):
        return np.ascontiguousarray(np.asarray(v, f32).reshape(n, 128).T)

    s_ = np.arange(128)[:, None]
    u_ = np.arange(1024)[None, :]
    delta = np.clip(u_ - 384 - s_, 0, 4095)
    bidx = _t5_bucket(delta)
    rb = np.asarray(rel_bias, f32)
    tbias = np.ascontiguousarray(np.transpose(rb[bidx], (2, 0, 1)))
    rb31 = np.ascontiguousarray(np.broadcast_to(rb[31][None, :], (128, 8)))
    shared = {
        "w_mod": np.ascontiguousarray(np.asarray(w_mod, f32)[0]),
        "b_mod": np.ascontiguousarray(np.asarray(b_mod, f32)[0][None, :]),
        "ln1g": col(ln1_g[0], 16), "ln2g": col(ln2_g[0], 16),
        "w_in": np.ascontiguousarray(np.asarray(w_in, f32)[0]),
        "qng": col(q_norm_g[0], 4),
        "w_qup": np.ascontiguousarray(np.asarray(w_q_up, f32)[0]),
        "qgain": col(q_gain[0], 1), "kgain": col(k_gain[0], 1),
        "gna": col(gn_a[0], 8), "gnb": col(gn_b[0], 8),
        "w_out": np.ascontiguousarray(np.asarray(w_out, f32)[0]),
        "router": np.ascontiguousarray(np.concatenate([np.asarray(router_g, f32)[0], np.asarray(router_e, f32)[0]], axis=1)),
        "w_gate": np.ascontiguousarray(np.asarray(w_gate, f32)[0]),
        "w_up": np.ascontiguousarray(np.asarray(w_up, f32)[0]),
        "w_down": np.ascontiguousarray(np.asarray(w_down, f32)[0]),
        "tbias": tbias, "rb31": rb31,
    }
    in_maps = []
    for core in range(8):
        b, h = core // 2, core % 2
        tiles = x[b].reshape(8, 512, D)
        if h == 1:
            xk = x[b]
        else:
            xk = np.concatenate([tiles[0:1], tiles[0:7]], axis=0).reshape(SA, D)
        m = dict(shared)
        m["xk"] = np.ascontiguousarray(xk)
        m["kvalid"] = np.full((128, 1), float(h), f32)
        m["c_col"] = col(c[b], 16)
        in_maps.append(m)
    if "nc" not in _NC_CACHE:
        _NC_CACHE["nc"] = build_nc()
    res = run_bass_kernel_spmd(_NC_CACHE["nc"], in_maps, core_ids=list(range(8)))
    out = np.empty((4, 4096, D), f32)
    for core in range(8):
        b, h = core // 2, core % 2
        o = np.asarray(res.results[core]["out"], f32).reshape(4, 512, D)
        for j in range(4):
            g = 2 * j + h
            out[b, g * 512:(g + 1) * 512] = o[j]
    return out
```

```python
import contextlib
import numpy as np
import concourse.bass as bass
import concourse.mybir as mybir
from concourse.bass_utils import run_bass_kernel_spmd

F32 = mybir.dt.float32
BF16 = mybir.dt.bfloat16
AF = mybir.ActivationFunctionType
ALU = mybir.AluOpType
AX = mybir.AxisListType

D = 2048
KC = 16
SA = 4096
NOWN = 2048
HD = 128
NE = 32
DE = 512
EPS = 1e-6
BIG = 1.0e30
TOPK = 256
NBIS = 14
IN_COLS = (512, 128, 128, 64, 16, 1024, 1024, 1024)
D_IN = sum(IN_COLS)
DEBUG = None


class Sched:
    ENG = ("pe", "act", "dve", "pool", "sp")
    BLK = {"pe": "tensor", "act": "scalar", "dve": "vector", "pool": "gpsimd", "sp": "sync"}

    def __init__(self, nc, stack):
        self.nc = nc
        self.stack = stack
        self.sem = {e: stack.enter_context(nc.semaphore("sem_" + e)) for e in self.ENG}
        self.cnt = {e: 0 for e in self.ENG}
        self.ops = {e: [] for e in self.ENG}
        self.waited = {e: {} for e in self.ENG}
        self.lastw = {}
        self.readers = {}
        self.touched = set()

    def _semfor(self, slot):
        name = "d_" + str(slot)
        if name not in self.sem:
            self.sem[name] = self.stack.enter_context(self.nc.semaphore(name))
            self.cnt[name] = 0
        return name

    def _deps(self, eng, reads, writes):
        need = {}

        def add(tok):
            if tok is None:
                return
            s, v = tok
            if need.get(s, 0) < v:
                need[s] = v
        for k in reads:
            add(self.lastw.get(k))
            if k.startswith("ps"):
                for t in self.readers.get(k, ()):
                    add(t)
        for k in writes:
            add(self.lastw.get(k))
            for t in self.readers.get(k, ()):
                add(t)
        out = []
        for s, v in need.items():
            if s == eng and eng == "pe":
                continue
            if self.waited[eng].get(s, 0) >= v:
                continue
            self.waited[eng][s] = v
            out.append((s, v))
        return out

    def _record(self, tok, reads, writes):
        for k in reads:
            self.readers.setdefault(k, []).append(tok)
        for k in writes:
            self.lastw[k] = tok
            self.readers[k] = []

    def op(self, eng, fn, reads=(), writes=()):
        waits = self._deps(eng, reads, writes)
        self.cnt[eng] += 1
        tok = (eng, self.cnt[eng])
        self.ops[eng].append((waits, fn, eng, 1))
        self._record(tok, reads, writes)
        return tok

    def dma(self, q, fn, reads=(), writes=(), slot=None, n=1):
        if slot is None:
            slot = writes[0]
        name = self._semfor(slot)
        waits = self._deps(q, reads, writes)
        self.cnt[name] += 16 * n
        tok = (name, self.cnt[name])
        self.ops[q].append((waits, fn, name, 16))
        self._record(tok, reads, writes)
        self.touched.add(name)
        return tok

    def drain(self):
        waits = []
        for name in sorted(self.touched):
            v = self.cnt[name]
            if self.waited["sp"].get(name, 0) < v:
                self.waited["sp"][name] = v
                waits.append((name, v))
        self.touched = set()
        if waits:
            self.ops["sp"].append((waits, None, None, 0))

    def emit(self):
        self.drain()
        with self.nc.Block() as block:
            for eng in self.ENG:
                ops = self.ops[eng]
                if not ops:
                    continue

                def body(e, ops=ops):
                    for waits, fn, semname, inc in ops:
                        for s, v in waits:
                            e.wait_ge(self.sem[s], v)
                        if fn is None:
                            continue
                        r = fn(e)
                        if isinstance(r, (list, tuple)):
                            for ins in r:
                                ins.then_inc(self.sem[semname], inc)
                        else:
                            r.then_inc(self.sem[semname], inc)
                getattr(block, self.BLK[eng])(body)
        self.ops = {e: [] for e in self.ENG}


def build_nc(stop=6):
    nc = bass.Bass("TRN2", target_bir_lowering=False)
    dbg = stop < 6

    def din(name, shape, dt=F32):
        return nc.dram_tensor(name, list(shape), dt, kind="ExternalInput").ap()

    xk = din("xk", [SA, D])
    kvalid_d = din("kvalid", [128, 1])
    c_col_d = din("c_col", [128, KC])
    w_mod = din("w_mod", [D, 6 * D])
    b_mod = din("b_mod", [1, 6 * D])
    ln1g_d = din("ln1g", [128, KC])
    ln2g_d = din("ln2g", [128, KC])
    w_in = din("w_in", [D, D_IN])
    qng_d = din("qng", [128, 4])
    w_qup = din("w_qup", [512, 2048])
    qgain_d = din("qgain", [128, 1])
    kgain_d = din("kgain", [128, 1])
    gna_d = din("gna", [128, 8])
    gnb_d = din("gnb", [128, 8])
    w_out = din("w_out", [D, D])
    router_d = din("router", [D, 36])
    if not dbg:
        w_gate = din("w_gate", [NE, D, DE])
        w_up = din("w_up", [NE, D, DE])
        w_down = din("w_down", [NE, DE, D])
    tb_d = din("tbias", [8, 128, 1024])
    rb31_d = din("rb31", [128, 8])
    out_d = nc.dram_tensor("out", [NOWN, D], F32, kind="ExternalOutput").ap()

    def dscr(name, shape, dt):
        return nc.dram_tensor(name, list(shape), dt, kind="ExternalOutput").ap()

    dbg_outs = {}

    def finish(S, items):
        for name, t, shape, dt in items:
            d_ = nc.dram_tensor("dbg_" + name, list(shape), dt, kind="ExternalOutput").ap()
            S.dma("sp", lambda e, t=t, d_=d_: e.dma_start(out=d_, in_=t[:]), reads=[name], writes=["dbg_" + name])
        if items:
            S.emit()

    kbT_s = dscr("kbT_s", [8, 128, SA], BF16)
    vb_s = dscr("vb_s", [SA, 1024], BF16)
    qbT_s = dscr("qbT_s", [8, 128, NOWN], BF16)
    qaT_s = dscr("qaT_s", [4, 128, 8, 512], BF16)
    qiT_s = dscr("qiT_s", [4, 128, 8, 512], BF16)
    x1_s = dscr("x1_s", [NOWN, D], F32)

    with contextlib.ExitStack() as gs:
        S = Sched(nc, gs)

        def sb(stack, name, shape, dt):
            return stack.enter_context(nc.sbuf_tensor(name, list(shape), dt))

        ps = [gs.enter_context(nc.psum_tensor("ps%d" % i, [128, 512], F32)) for i in range(8)]
        PK = ["ps%d" % i for i in range(8)]

        ident = sb(gs, "ident", [128, 128], F32)
        identb = sb(gs, "identb", [128, 128], BF16)
        onesb = sb(gs, "onesb", [128, 128], BF16)
        negtri = sb(gs, "negtri", [128, 128], BF16)
        negones = sb(gs, "negones", [128, 128], BF16)
        ones_row = sb(gs, "ones_row", [1, 128], F32)
        modT = sb(gs, "modT", [128, 96], F32)
        G1 = sb(gs, "G1", [128, KC], F32)
        G2 = sb(gs, "G2", [128, KC], F32)
        g2_bc = sb(gs, "g2_bc", [128, D], F32)
        kvalid = sb(gs, "kvalid_t", [128, 1], F32)
        qng = sb(gs, "qng_t", [128, 4], F32)
        qgain = sb(gs, "qgain_t", [128, 1], F32)
        kgain = sb(gs, "kgain_t", [128, 1], F32)
        gna = sb(gs, "gna_t", [128, 8], F32)
        gnb = sb(gs, "gnb_t", [128, 8], F32)
        ats = contextlib.ExitStack()
        g1_bc = sb(ats, "g1_bc", [128, D], F32)
        with contextlib.ExitStack() as p0:
            c_col = sb(p0, "c_col_t", [128, KC], F32)
            sc_col = sb(p0, "sc_col", [128, KC], F32)
            mrow = [sb(p0, "mrow%d" % i, [1, 512], F32) for i in range(2)]
            bmc = [sb(p0, "bmc%d" % i, [1, 512], F32) for i in range(2)]
            ln1g = sb(p0, "ln1g_t", [128, KC], F32)
            ln2g = sb(p0, "ln2g_t", [128, KC], F32)
            tmpc = sb(p0, "tmpc", [128, KC], F32)
            one11 = sb(p0, "one11", [1, 1], F32)
            onesf0 = sb(p0, "onesf0", [128, 128], F32)
            wm = [sb(p0, "wm%d" % i, [128, KC, 512], F32) for i in range(3)]

            small = [(c_col, c_col_d, "c_col"), (ln1g, ln1g_d, "ln1g"), (ln2g, ln2g_d, "ln2g"), (qng, qng_d, "qng"),
                     (qgain, qgain_d, "qgain"), (kgain, kgain_d, "kgain"), (gna, gna_d, "gna"), (gnb, gnb_d, "gnb"),
                     (kvalid, kvalid_d, "kvalid")]
            for t, d_, k in small:
                S.dma("sp", lambda e, t=t, d_=d_: e.dma_start(out=t[:], in_=d_), writes=[k])

            S.op("pool", lambda e: e.memset(onesf0[:], 1.0), writes=["onesf0"])
            S.op("pool", lambda e: e.memset(ones_row[:], 1.0), writes=["ones_row"])
            S.op("pool", lambda e: e.memset(one11[:], 1.0), writes=["one11"])
            S.op("pool", lambda e: e.memset(onesb[:], 1.0), writes=["onesb"])
            S.op("pool", lambda e: e.memset(negones[:], -1.0), writes=["negones"])
            S.op("pool", lambda e: e.affine_select(out=ident[:], in_=onesf0[:], pattern=[[-1, 128]],
                                                   compare_op=ALU.is_equal, fill=0.0, base=0, channel_multiplier=1),
                 reads=["onesf0"], writes=["ident"])
            S.op("pool", lambda e: e.tensor_copy(out=identb[:], in_=ident[:]), reads=["ident"], writes=["identb"])
            S.op("pool", lambda e: e.affine_select(out=negtri[:], in_=negones[:], pattern=[[-1, 128]],
                                                   compare_op=ALU.is_ge, fill=0.0, base=0, channel_multiplier=1),
                 reads=["negones"], writes=["negtri"])
            S.op("act", lambda e: e.activation(out=sc_col[:], in_=c_col[:], func=AF.Silu), reads=["c_col"],
                 writes=["sc_col"])
            for g in range(24):
                wt = wm[g % 3]
                wk = "wm%d" % (g % 3)
                mi = g % 2
                S.dma("sp", lambda e, wt=wt, g=g: e.dma_start(
                    out=wt[:], in_=w_mod[:, g * 512:(g + 1) * 512].rearrange("(c p) n -> p c n", p=128)), writes=[wk])
                S.dma("sp", lambda e, mi=mi, g=g: e.dma_start(out=bmc[mi][:], in_=b_mod[:, g * 512:(g + 1) * 512]),
                      writes=["bmc%d" % mi])
                pk = g % 2

                def mm(e, wt=wt, pk=pk):
                    r = None
                    for k in range(KC):
                        r = e.matmul(ps[pk][0:1, :], sc_col[:, k:k + 1], wt[:, k, :], start=(k == 0), stop=(k == KC - 1))
                    return r
                S.op("pe", mm, reads=[wk, "sc_col"], writes=[PK[pk]])
                S.op("dve", lambda e, mi=mi, pk=pk: e.tensor_tensor(out=mrow[mi][:], in0=ps[pk][0:1, :], in1=bmc[mi][:], op=ALU.add),
                     reads=[PK[pk], "bmc%d" % mi], writes=["mrow%d" % mi])

                def mmT(e, mi=mi, g=g):
                    r = None
                    for q in range(4):
                        j = g * 4 + q
                        r = e.matmul(ps[2][:, j:j + 1], mrow[mi][0:1, q * 128:(q + 1) * 128], one11[0:1, 0:1], start=True, stop=True)
                    return r
                S.op("pe", mmT, reads=["mrow%d" % mi, "one11"], writes=[PK[2]])
                if 8 <= g < 12 or 20 <= g < 24:
                    dst, dk, q = (g1_bc, "g1_bc", g - 8) if g < 12 else (g2_bc, "g2_bc", g - 20)
                    pb = 3 + g % 2
                    S.op("pe", lambda e, pb=pb, mi=mi: e.matmul(ps[pb][:, :], ones_row[0:1, :], mrow[mi][0:1, :], start=True, stop=True),
                         reads=["mrow%d" % mi, "ones_row"], writes=[PK[pb]])
                    S.op("dve", lambda e, dst=dst, pb=pb, q=q: e.tensor_copy(out=dst[:, q * 512:(q + 1) * 512], in_=ps[pb][:, :]),
                         reads=[PK[pb]], writes=[dk])
            S.op("dve", lambda e: e.tensor_copy(out=modT[:], in_=ps[2][:, 0:96]), reads=[PK[2]], writes=["modT"])
            S.op("dve", lambda e: e.tensor_scalar(out=tmpc[:], in0=modT[:, 16:32], scalar1=1.0, scalar2=None, op0=ALU.add),
                 reads=["modT"], writes=["tmpc"])
            S.op("dve", lambda e: e.tensor_tensor(out=G1[:], in0=tmpc[:], in1=ln1g[:], op=ALU.mult),
                 reads=["tmpc", "ln1g"], writes=["G1"])
            S.op("dve", lambda e: e.tensor_scalar(out=tmpc[:], in0=modT[:, 64:80], scalar1=1.0, scalar2=None, op0=ALU.add),
                 reads=["modT", "G1"], writes=["tmpc"])
            S.op("dve", lambda e: e.tensor_tensor(out=G2[:], in0=tmpc[:], in1=ln2g[:], op=ALU.mult),
                 reads=["tmpc", "ln2g"], writes=["G2"])
            S.emit()


        maskV = sb(ats, "maskV", [128, 512], BF16)
        maskS = sb(ats, "maskS", [128, 4, 512], BF16)
        maskI = sb(ats, "maskI", [128, 4, 512], BF16)
        kaT = sb(ats, "kaT", [128, SA], BF16)
        va = sb(ats, "va", [128, 32, 128], BF16)
        kiT = sb(ats, "kiT", [128, SA], BF16)
        widx = sb(ats, "widx", [128, 16, 16], F32)
        c2 = contextlib.ExitStack()
        cqT = sb(c2, "cqT", [128, 4, NOWN], BF16)

        evq = [0]

        def evac_scale(dst, src, s1, s2, reads, writes):
            evq[0] += 1
            if evq[0] % 2 == 0:
                if s2 is None:
                    S.op("dve", lambda e: e.tensor_scalar(out=dst, in0=src, scalar1=s1, scalar2=None, op0=ALU.mult),
                         reads=reads, writes=writes)
                else:
                    S.op("dve", lambda e: e.tensor_scalar(out=dst, in0=src, scalar1=s1, scalar2=s2, op0=ALU.mult,
                                                          op1=ALU.add), reads=reads, writes=writes)
            else:
                if s2 is None:
                    S.op("act", lambda e: e.activation(out=dst, in_=src, func=AF.Copy if isinstance(s1, float) else AF.Identity,
                                                       scale=s1), reads=reads, writes=writes)
                else:
                    S.op("act", lambda e: e.activation(out=dst, in_=src, func=AF.Identity, scale=s1, bias=s2),
                         reads=reads, writes=writes)

        def rstd_from(ss_ap, dim, key_in, key_out, out_ap):
            S.op("act", lambda e: e.activation(out=out_ap, in_=ss_ap, func=AF.Sqrt, scale=1.0 / dim, bias=EPS),
                 reads=[key_in], writes=[key_out])
            S.op("dve", lambda e: e.reciprocal(out=out_ap, in_=out_ap), reads=[key_out], writes=[key_out])

        psrot = [0]

        def nextps():
            i = psrot[0] % 8
            psrot[0] += 1
            return i

        if stop == 0:
            finish(S, [("modT", modT, [128, 96], F32), ("G1", G1, [128, KC], F32), ("G2", G2, [128, KC], F32),
                       ("g1_bc", g1_bc, [128, D], F32), ("g2_bc", g2_bc, [128, D], F32), ("ident", ident, [128, 128], F32),
                       ("negtri", negtri, [128, 128], BF16)])
            c2.close()
            ats.close()
            return nc
        with contextlib.ExitStack() as p1:
            hT = sb(p1, "hT", [128, KC, 1024], BF16)
            xs = sb(p1, "xs", [128, 4, D], F32)
            junk = sb(p1, "junk", [128, D], BF16)
            ssq = sb(p1, "ssq", [128, 8], F32)
            rstd = sb(p1, "rstd", [128, 8], F32)
            wbuf = [sb(p1, "wbuf%d" % i, [128, KC, 512], BF16) for i in range(2)]
            stg = [sb(p1, "stg%d" % i, [128, 512], BF16) for i in range(3)]
            stgv = [sb(p1, "stgv%d" % i, [128, 1024], BF16) for i in range(2)]
            tm32 = [sb(p1, "tm32_%d" % i, [128, 512], F32) for i in range(2)]
            sm = sb(p1, "sm", [128, 8], F32)
            onesf = sb(p1, "onesf", [128, 512], F32)
            S.op("pool", lambda e: e.memset(onesf[:], 1.0), writes=["onesf"])
            for i in range(4):
                S.op("pool", lambda e, i=i: e.affine_select(out=maskI[:, i, :], in_=onesf[:], pattern=[[1, 512]],
                                                            compare_op=ALU.is_ge, fill=0.0, base=-128 * i,
                                                            channel_multiplier=-1),
                     reads=["onesf"], writes=["maskI"])
                S.op("pool", lambda e, i=i: e.affine_select(out=maskS[:, i, :], in_=onesf[:], pattern=[[1, 512]],
                                                            compare_op=ALU.is_ge, fill=0.0, base=-128 * i - 1,
                                                            channel_multiplier=-1),
                     reads=["onesf"], writes=["maskS"])
            S.op("dve", lambda e: e.tensor_scalar(out=maskV[:], in0=onesf[:], scalar1=kvalid[:, 0:1], scalar2=None,
                                                  op0=ALU.mult), reads=["onesf", "kvalid"], writes=["maskV"])

            wload_i = [0]

            def load_w(col0, ncol, dup=False):
                i = wload_i[0] % 2
                wload_i[0] += 1
                wt, wk = wbuf[i], "wbuf%d" % i
                if dup:
                    def f(e, wt=wt):
                        a_ = e.dma_start(out=wt[:, :, 0:64], in_=w_in[:, col0:col0 + 64].rearrange("(c p) n -> p c n", p=128))
                        b_ = e.dma_start(out=wt[:, :, 64:128], in_=w_in[:, col0:col0 + 64].rearrange("(c p) n -> p c n", p=128))
                        return [a_, b_]
                    S.dma("pool", f, writes=[wk], n=2)
                else:
                    S.dma("pool", lambda e, wt=wt: e.dma_start(
                        out=wt[:, :, 0:ncol], in_=w_in[:, col0:col0 + ncol].rearrange("(c p) n -> p c n", p=128)), writes=[wk])
                return wt, wk

            def proj_tok(wt, wk, ncol, lb, pi):
                hk = "hT%d" % (lb // 4)

                def mm(e):
                    r = None
                    for kc in range(KC):
                        r = e.matmul(ps[pi][:, 0:ncol], hT[:, kc, lb * 128:(lb + 1) * 128], wt[:, kc, 0:ncol],
                                     start=(kc == 0), stop=(kc == KC - 1))
                    return r
                S.op("pe", mm, reads=[hk, wk], writes=[PK[pi]])

            def proj_feat(wt, wk, c0, hb, pi):
                def mm(e):
                    r = None
                    for kc in range(KC):
                        r = e.matmul(ps[pi][:, :], wt[:, kc, c0:c0 + 128], hT[:, kc, hb * 512:(hb + 1) * 512],
                                     start=(kc == 0), stop=(kc == KC - 1))
                    return r
                S.op("pe", mm, reads=["hT%d" % hb, wk], writes=[PK[pi]])

            def stage_xdma(at):
                S.dma("sp", lambda e: e.dma_start(
                    out=xs[:], in_=xk[at * 512:(at + 1) * 512, :].rearrange("(b p) n -> p b n", p=128)), writes=["xs"])

            def stage_xnorm():
                for b in range(4):
                    S.op("act", lambda e, b=b: e.activation(out=junk[:], in_=xs[:, b, :], func=AF.Square,
                                                            accum_out=ssq[:, b:b + 1]),
                         reads=["xs"], writes=["junk", "ssq"])
                rstd_from(ssq[:, 0:4], D, "ssq", "rstd", rstd[:, 0:4])
                for b in range(4):
                    S.op("dve", lambda e, b=b: e.tensor_scalar(
                        out=xs[:, b, :], in0=xs[:, b, :], scalar1=rstd[:, b:b + 1], scalar2=None, op0=ALU.mult),
                        reads=["rstd", "xs"], writes=["xs"])

            def stage_T(hb):
                for kc in range(KC):
                    pi = nextps()

                    def tr(e, kc=kc, pi=pi):
                        r = None
                        for b in range(4):
                            r = e.transpose(ps[pi][:, b * 128:(b + 1) * 128], xs[:, b, kc * 128:(kc + 1) * 128], ident[:])
                        return r
                    S.op("pe", tr, reads=["xs", "ident"], writes=[PK[pi]])
                    evac_scale(hT[:, kc, hb * 512:(hb + 1) * 512], ps[pi][:, :], G1[:, kc:kc + 1], modT[:, kc:kc + 1],
                               [PK[pi], "G1", "modT"], ["hT%d" % hb])

            def stage_P(at, hb, nxt):
                own = (at % 2 == 1)
                oj = at // 2
                if own:
                    wt, wk = load_w(0, 512)
                    for b in range(4):
                        lb = hb * 4 + b
                        tb = oj * 4 + b
                        pi = nextps()
                        proj_tok(wt, wk, 512, lb, pi)
                        t32, tk = tm32[tb % 2], "tm32_%d" % (tb % 2)
                        S.op("act", lambda e, pi=pi: e.activation(out=junk[:, 0:512], in_=ps[pi][:, :], func=AF.Square,
                                                                  accum_out=sm[:, 0:1]),
                             reads=[PK[pi]], writes=["junk", "sm"])
                        rstd_from(sm[:, 0:1], 512.0, "sm", "sm1", sm[:, 1:2])
                        S.op("dve", lambda e, pi=pi, t32=t32: e.tensor_scalar(out=t32[:], in0=ps[pi][:, :], scalar1=sm[:, 1:2],
                                                                             scalar2=None, op0=ALU.mult),
                             reads=[PK[pi], "sm1"], writes=[tk])
                        pj = nextps()

                        def tr(e, t32=t32, pj=pj):
                            r = None
                            for c4 in range(4):
                                r = e.transpose(ps[pj][:, c4 * 128:(c4 + 1) * 128], t32[:, c4 * 128:(c4 + 1) * 128], ident[:])
                            return r
                        S.op("pe", tr, reads=[tk, "ident"], writes=[PK[pj]])
                        for c4 in range(4):
                            evac_scale(cqT[:, c4, tb * 128:(tb + 1) * 128], ps[pj][:, c4 * 128:(c4 + 1) * 128],
                                       qng[:, c4:c4 + 1], None, [PK[pj], "qng"], ["cqT"])
                wt, wk = load_w(512, 336)
                for b in range(4):
                    lb = hb * 4 + b
                    tb = oj * 4 + b
                    ab = at * 4 + b
                    pi = nextps()
                    proj_tok(wt, wk, 336, lb, pi)
                    t32, tk = tm32[lb % 2], "tm32_%d" % (lb % 2)
                    S.op("act", lambda e, pi=pi: e.activation(out=junk[:, 0:128], in_=ps[pi][:, 0:128], func=AF.Square,
                                                              accum_out=sm[:, 2:3]),
                         reads=[PK[pi]], writes=["junk", "sm2"])
                    rstd_from(sm[:, 2:3], 128.0, "sm2", "sm3", sm[:, 3:4])
                    S.op("dve", lambda e, pi=pi, t32=t32: e.tensor_scalar(out=t32[:, 0:128], in0=ps[pi][:, 0:128], scalar1=sm[:, 3:4],
                                                                         scalar2=None, op0=ALU.mult),
                         reads=[PK[pi], "sm3"], writes=[tk])
                    S.op("act", lambda e, pi=pi, ab=ab: e.activation(out=va[:, ab, :], in_=ps[pi][:, 128:256], func=AF.Copy),
                         reads=[PK[pi]], writes=["va"])
                    if own:
                        S.op("dve", lambda e, pi=pi, tb=tb: e.tensor_scalar(out=widx[:, tb, :], in0=ps[pi][:, 320:336],
                                                                           scalar1=1.0 / 32.0, scalar2=None, op0=ALU.mult),
                             reads=[PK[pi]], writes=["widx"])
                    pj = nextps()
                    S.op("pe", lambda e, t32=t32, pj=pj: e.transpose(ps[pj][:, 0:128], t32[:, 0:128], ident[:]),
                         reads=[tk, "ident"], writes=[PK[pj]])
                    evac_scale(kaT[:, ab * 128:(ab + 1) * 128], ps[pj][:, 0:128], kgain[:, 0:1], None,
                               [PK[pj], "kgain"], ["kaT"])
                wt, wk = load_w(768, 64, dup=True)
                pi = nextps()
                proj_feat(wt, wk, 0, hb, pi)
                evac_scale(kiT[:, at * 512:(at + 1) * 512], ps[pi][:, :], 1.0, None, [PK[pi]], ["kiT"])
                if nxt is not None:
                    stage_xnorm()
                fm = ([("q", 848)] if own else []) + [("k", 1872)]
                for kind, col0 in fm:
                    for half in range(2):
                        wt, wk = load_w(col0 + half * 512, 512)
                        for hh in range(4):
                            hd = half * 4 + hh
                            pi = nextps()
                            proj_feat(wt, wk, hh * 128, hb, pi)
                            si = evq[0] % 3
                            st, sk = stg[si], "stg%d" % si
                            evac_scale(st[:], ps[pi][:, :], (float(HD) ** -0.5) if kind == "q" else 1.0, None, [PK[pi]], [sk])
                            if kind == "q":
                                S.dma("sp", lambda e, st=st, hd=hd: e.dma_start(
                                    out=qbT_s[hd, :, oj * 512:(oj + 1) * 512], in_=st[:]), reads=[sk], writes=["qbT_s"])
                            else:
                                S.dma("sp", lambda e, st=st, hd=hd: e.dma_start(
                                    out=kbT_s[hd, :, at * 512:(at + 1) * 512], in_=st[:]), reads=[sk], writes=["kbT_s"])
                for half in range(2):
                    wt, wk = load_w(2896 + half * 512, 512)
                    for b in range(4):
                        lb = hb * 4 + b
                        ab = at * 4 + b
                        pi = nextps()
                        proj_tok(wt, wk, 512, lb, pi)
                        si = evq[0] % 3
                        st, sk = stg[si], "stg%d" % si
                        evac_scale(st[:], ps[pi][:, :], 1.0, None, [PK[pi]], [sk])
                        S.dma("sp", lambda e, st=st, ab=ab, half=half: e.dma_start(
                            out=vb_s[ab * 128:(ab + 1) * 128, half * 512:(half + 1) * 512], in_=st[:]),
                            reads=[sk], writes=["vb_s"])

            tile_seq = [1, 3, 5, 7, 0, 2, 4, 6]
            stage_xdma(tile_seq[0])
            stage_xnorm()
            stage_T(0)
            for idx, at in enumerate(tile_seq):
                hb = idx % 2
                nxt = tile_seq[idx + 1] if idx + 1 < len(tile_seq) else None
                if nxt is not None:
                    stage_xdma(nxt)
                stage_P(at, hb, nxt)
                if nxt is not None:
                    stage_T(1 - hb)
            S.emit()

        if stop == 1:
            finish(S, [("kaT", kaT, [128, SA], BF16), ("va", va, [128, 32, 128], BF16), ("kiT", kiT, [128, SA], BF16),
                       ("widx", widx, [128, 16, 16], F32), ("cqT", cqT, [128, 4, NOWN], BF16), ("maskS", maskS, [128, 4, 512], BF16),
                       ("maskI", maskI, [128, 4, 512], BF16), ("maskV", maskV, [128, 512], BF16)])
            c2.close()
            ats.close()
            return nc
        with contextlib.ExitStack() as p2:
            wqu = sb(p2, "wqu", [128, 4, 2048], BF16)
            qa32 = sb(p2, "qa32", [128, 4, 1024], F32)
            sqb = [sb(p2, "sqb%d" % i, [128, 512], BF16) for i in range(2)]
            rsb = [sb(p2, "rsb%d" % i, [128, 512], F32) for i in range(2)]
            qsc = sb(p2, "qsc", [128, 1], F32)
            stg2 = [sb(p2, "stg2_%d" % i, [128, 512], BF16) for i in range(3)]
            S.op("dve", lambda e: e.tensor_scalar(out=qsc[:], in0=qgain[:], scalar1=float(HD) ** -0.5, scalar2=None,
                                                  op0=ALU.mult), reads=["qgain"], writes=["qsc"])

            def ldq(e):
                return [e.dma_start(out=wqu[:, :, hf * 1024:(hf + 1) * 1024],
                                    in_=w_qup[:, hf * 1024:(hf + 1) * 1024].rearrange("(c p) n -> p c n", p=128)) for hf in range(2)]
            S.dma("pool", ldq, writes=["wqu"], n=2)
            q2 = [0]
            for li in range(4):
                for b in range(4):
                    tb = li * 4 + b
                    for half in range(2):
                        pi = nextps()

                        def mm(e, tb=tb, half=half, pi=pi):
                            r = None
                            for kc in range(4):
                                r = e.matmul(ps[pi][:, :], cqT[:, kc, tb * 128:(tb + 1) * 128],
                                             wqu[:, kc, half * 512:(half + 1) * 512], start=(kc == 0), stop=(kc == 3))
                            return r
                        S.op("pe", mm, reads=["cqT", "wqu"], writes=[PK[pi]])
                        if (b + half) % 2 == 0:
                            S.op("dve", lambda e, pi=pi, b=b, half=half: e.tensor_copy(
                                out=qa32[:, b, half * 512:(half + 1) * 512], in_=ps[pi][:, :]), reads=[PK[pi]], writes=["qa32"])
                        else:
                            S.op("act", lambda e, pi=pi, b=b, half=half: e.activation(
                                out=qa32[:, b, half * 512:(half + 1) * 512], in_=ps[pi][:, :], func=AF.Copy), reads=[PK[pi]], writes=["qa32"])
                for hd in range(8):
                    pi = nextps()
                    k2 = q2[0] % 2
                    q2[0] += 1

                    def tr(e, hd=hd, pi=pi):
                        r = None
                        for b in range(4):
                            r = e.transpose(ps[pi][:, b * 128:(b + 1) * 128], qa32[:, b, hd * 128:(hd + 1) * 128], ident[:])
                        return r
                    S.op("pe", tr, reads=["qa32", "ident"], writes=[PK[pi]])
                    S.op("act", lambda e, pi=pi, k2=k2: e.activation(out=sqb[k2][:], in_=ps[pi][:, :], func=AF.Square),
                         reads=[PK[pi]], writes=["sqb%d" % k2])
                    pj = nextps()
                    S.op("pe", lambda e, pj=pj, k2=k2: e.matmul(ps[pj][:, :], onesb[:], sqb[k2][:], start=True, stop=True),
                         reads=["sqb%d" % k2, "onesb"], writes=[PK[pj]])
                    S.op("act", lambda e, pj=pj, k2=k2: e.activation(out=rsb[k2][:], in_=ps[pj][:, :], func=AF.Sqrt, scale=1.0 / 128.0, bias=EPS),
                         reads=[PK[pj]], writes=["rsb%d" % k2])
                    S.op("dve", lambda e, k2=k2: e.reciprocal(out=rsb[k2][:], in_=rsb[k2][:]), reads=["rsb%d" % k2], writes=["rsb%d" % k2])
                    si = q2[0] % 3
                    st, sk = stg2[si], "stg2_%d" % si
                    S.op("dve", lambda e, pi=pi, k2=k2, st=st: e.scalar_tensor_tensor(out=st[:], in0=ps[pi][:, :], scalar=qsc[:, 0:1],
                                                                                    in1=rsb[k2][:], op0=ALU.mult, op1=ALU.mult),
                         reads=[PK[pi], "qsc", "rsb%d" % k2], writes=[sk])
                    S.dma("sp", lambda e, st=st, hd=hd, li=li: e.dma_start(out=qaT_s[li, :, hd, :], in_=st[:]),
                          reads=[sk], writes=["qaT_s"])
                for pr in range(8):
                    pi = nextps()

                    def mm(e, pr=pr, li=li, pi=pi):
                        r = None
                        for kc in range(4):
                            r = e.matmul(ps[pi][:, :], wqu[:, kc, 1024 + pr * 128: 1024 + (pr + 1) * 128],
                                         cqT[:, kc, li * 512:(li + 1) * 512], start=(kc == 0), stop=(kc == 3))
                        return r
                    S.op("pe", mm, reads=["cqT", "wqu"], writes=[PK[pi]])
                    q2[0] += 1
                    si = q2[0] % 3
                    st, sk = stg2[si], "stg2_%d" % si
                    evac_scale(st[:], ps[pi][:, :], 1.0, None, [PK[pi]], [sk])
                    S.dma("sp", lambda e, st=st, pr=pr, li=li: e.dma_start(out=qiT_s[li, :, pr, :], in_=st[:]),
                          reads=[sk], writes=["qiT_s"])
            S.emit()
        c2.close()
        if stop == 2:
            finish(S, [])
            ats.close()
            return nc
        oaT = sb(ats, "oaT", [128, 8, NOWN], BF16)

        with contextlib.ExitStack() as p3:
            IT = sb(p3, "IT", [128, 32, 512], BF16)
            Wh = sb(p3, "Wh", [128, 8, 1024], BF16)
            tb32 = [sb(p3, "tb32_%d" % i, [128, 1024], F32) for i in range(1)]
            rb31 = sb(p3, "rb31_t", [128, 8], F32)
            nrb31 = sb(p3, "nrb31", [128, 8], F32)
            Wdiag = sb(p3, "Wdiag", [128, 4, 16, 128], BF16)
            qi = [sb(p3, "qi%d" % i, [128, 8, 512], BF16) for i in range(1)]
            qa = [sb(p3, "qa%d" % i, [128, 8, 512], BF16) for i in range(1)]
            rl = [sb(p3, "rl%d" % i, [128, 512], BF16) for i in range(4)]
            pt = [sb(p3, "pt%d" % i, [128, 512], BF16) for i in range(4)]
            pm = [sb(p3, "pm%d" % i, [128, 512], BF16) for i in range(4)]
            cmpb = [sb(p3, "cmp%d" % i, [128, 512], BF16) for i in range(4)]
            lo = sb(p3, "lo", [128, 512], F32)
            stp = sb(p3, "stp", [128, 512], F32)
            midb = sb(p3, "midb", [128, 512], BF16)
            rden = sb(p3, "rden", [128, 512], F32)
            penV = sb(p3, "penV", [128, 512], BF16)
            penI = sb(p3, "penI", [128, 4, 512], BF16)

            S.dma("sp", lambda e: e.dma_start(out=rb31[:], in_=rb31_d), writes=["rb31"])
            S.op("dve", lambda e: e.tensor_scalar(out=nrb31[:], in0=rb31[:], scalar1=-1.0, scalar2=None, op0=ALU.mult),
                 reads=["rb31"], writes=["nrb31"])
            S.op("dve", lambda e: e.tensor_scalar(out=penV[:], in0=maskV[:], scalar1=-1.0, scalar2=BIG, op0=ALU.add, op1=ALU.mult),
                 reads=["maskV"], writes=["penV"])
            for i in range(4):
                S.op("dve", lambda e, i=i: e.tensor_scalar(out=penI[:, i, :], in0=maskI[:, i, :], scalar1=-1.0, scalar2=BIG,
                                                           op0=ALU.add, op1=ALU.mult), reads=["maskI"], writes=["penI"])
            for h in range(8):
                tt_, tk = tb32[0], "tb32_0"
                S.dma("sp", lambda e, tt_=tt_, h=h: e.dma_start(out=tt_[:], in_=tb_d[h]), writes=[tk])
                S.op("act", lambda e, tt_=tt_, h=h: e.activation(out=Wh[:, h, :], in_=tt_[:], func=AF.Exp, bias=nrb31[:, h:h + 1]),
                     reads=[tk, "nrb31"], writes=["Wh"])
                S.op("pool", lambda e, h=h: e.affine_select(out=Wh[:, h, :], in_=Wh[:, h, :], pattern=[[1, 1024]],
                                                            compare_op=ALU.is_ge, fill=0.0, base=-384, channel_multiplier=-1),
                     reads=["Wh"], writes=["Wh"])

            W0 = 16.0
            rot = [0]
            SCB = [0, 1, 7]

            def dsa_tile(j):
                nkt = 2 * j + 2
                nkb = 4 * nkt
                qit, qik = qi[0], "qi0"
                qat, qak = qa[0], "qa0"
                S.dma("sp", lambda e: e.dma_start(out=qit[:], in_=qiT_s[j]), reads=["qiT_s"], writes=[qik])
                S.dma("sp", lambda e: e.dma_start(out=qat[:], in_=qaT_s[j]), reads=["qaT_s"], writes=[qak])
                for qb in range(4):
                    for h in range(16):
                        S.op("dve", lambda e, qb=qb, h=h: e.tensor_scalar(
                            out=Wdiag[:, qb, h, :], in0=identb[:], scalar1=widx[:, j * 4 + qb, h:h + 1], scalar2=None, op0=ALU.mult),
                            reads=["identb", "widx"], writes=["Wdiag"])
                its = [(kt, qb, h) for kt in range(nkt) for qb in range(4) for h in range(16)]
                stt = {}

                def stageSC(i):
                    kt, qb, h = its[i]
                    g = rot[0]
                    rot[0] += 1
                    scb, rli = SCB[g % 3], g % 4
                    p0_ = 64 * (h % 2)
                    stt[i] = (scb, rli)
                    S.op("pe", lambda e: e.matmul(ps[scb][:, :], qit[p0_:p0_ + 64, h // 2, qb * 128:(qb + 1) * 128],
                                                  kiT[p0_:p0_ + 64, kt * 512:(kt + 1) * 512], start=True, stop=True),
                         reads=[qik, "kiT"], writes=[PK[scb]])
                    rlt, rlk = rl[rli], "rl%d" % rli
                    if g % 3 != 2:
                        S.op("act", lambda e: e.activation(out=rlt[:], in_=ps[scb][:, :], func=AF.Relu), reads=[PK[scb]], writes=[rlk])
                    else:
                        S.op("dve", lambda e: e.tensor_scalar(out=rlt[:], in0=ps[scb][:, :], scalar1=0.0, scalar2=None, op0=ALU.max),
                             reads=[PK[scb]], writes=[rlk])

                def stageACC(i):
                    kt, qb, h = its[i]
                    scb, rli = stt[i]
                    rlt, rlk = rl[rli], "rl%d" % rli

                    def acc(e):
                        r = None
                        for sbk in range(4):
                            r = e.matmul(ps[2 + sbk][:, qb * 128:(qb + 1) * 128], rlt[:, sbk * 128:(sbk + 1) * 128],
                                         Wdiag[:, qb, h, :], start=(h == 0), stop=(h == 15))
                        return r
                    S.op("pe", acc, reads=[rlk, "Wdiag"], writes=[PK[2], PK[3], PK[4], PK[5]])
                    if qb == 3 and h == 15:
                        for sbk in range(4):
                            kb = kt * 4 + sbk
                            if kt == 0 or kt == nkt - 1:
                                mk = maskV[:] if kt == 0 else maskI[:, sbk, :]
                                pn = penV[:] if kt == 0 else penI[:, sbk, :]
                                S.op("dve", lambda e, kb=kb, sbk=sbk, mk=mk: e.tensor_tensor(out=IT[:, kb, :], in0=ps[2 + sbk][:, :], in1=mk,
                                                                                              op=ALU.mult),
                                     reads=[PK[2 + sbk], "maskV", "maskI"], writes=["IT"])
                                S.op("dve", lambda e, kb=kb, pn=pn: e.tensor_tensor(out=IT[:, kb, :], in0=IT[:, kb, :], in1=pn, op=ALU.add),
                                     reads=["IT", "penV", "penI"], writes=["IT"])
                            else:
                                S.op("act", lambda e, kb=kb, sbk=sbk: e.activation(out=IT[:, kb, :], in_=ps[2 + sbk][:, :], func=AF.Copy),
                                     reads=[PK[2 + sbk]], writes=["IT"])
                for i in range(len(its) + 1):
                    if i < len(its):
                        stageSC(i)
                    if i >= 1:
                        stageACC(i - 1)
                S.op("pool", lambda e: e.memset(lo[:], -W0), writes=["lo"])
                for it in range(NBIS + 1):
                    hk = W0 / (2.0 ** it)
                    final = (it == NBIS)
                    S.op("dve", lambda e, hk=hk, final=final: e.tensor_scalar(out=midb[:], in0=lo[:], scalar1=(0.0 if final else hk),
                                                                              scalar2=None, op0=ALU.add),
                         reads=["lo"], writes=["midb"])
                    if final:
                        for kb in range(nkb):
                            S.op("dve", lambda e, kb=kb: e.tensor_tensor(out=IT[:, kb, :], in0=IT[:, kb, :], in1=midb[:], op=ALU.is_ge),
                                 reads=["IT", "midb"], writes=["IT"])
                        break
                    for kb in range(nkb):
                        ci = kb % 4
                        S.op("dve", lambda e, kb=kb, ci=ci: e.tensor_tensor(out=cmpb[ci][:], in0=IT[:, kb, :], in1=midb[:], op=ALU.is_ge),
                             reads=["IT", "midb"], writes=["cmp%d" % ci])
                        S.op("pe", lambda e, kb=kb, ci=ci: e.matmul(ps[6][:, :], onesb[:], cmpb[ci][:], start=(kb == 0), stop=(kb == nkb - 1)),
                             reads=["cmp%d" % ci, "onesb"], writes=[PK[6]])
                    S.op("dve", lambda e, hk=hk: e.tensor_scalar(out=stp[:], in0=ps[6][:, :], scalar1=TOPK - 0.5, scalar2=hk,
                                                                 op0=ALU.is_ge, op1=ALU.mult), reads=[PK[6]], writes=["stp"])
                    S.op("dve", lambda e: e.tensor_tensor(out=lo[:], in0=lo[:], in1=stp[:], op=ALU.add), reads=["lo", "stp"], writes=["lo"])
                for h in range(8):
                    dsa_head(j, h, nkb, qat, qak)

            def dsa_head(j, h, nkb, qat, qak):
                ob, db = 2 + 2 * (h % 2), 3 + 2 * (h % 2)
                sst = {}

                def stageLG(kb):
                    g = rot[0]
                    rot[0] += 1
                    lgb, pi_ = SCB[g % 3], g % 4
                    sst[kb] = pi_
                    S.op("pe", lambda e: e.matmul(ps[lgb][:, :], kaT[:, kb * 128:(kb + 1) * 128], qat[:, h, :], start=True, stop=True),
                         reads=["kaT", qak], writes=[PK[lgb]])
                    S.op("act", lambda e: e.activation(out=pt[pi_][:], in_=ps[lgb][:, :], func=AF.Exp), reads=[PK[lgb]], writes=["pt%d" % pi_])
                    S.op("dve", lambda e: e.tensor_tensor(out=pm[pi_][:], in0=pt[pi_][:], in1=IT[:, kb, :], op=ALU.mult),
                         reads=["pt%d" % pi_, "IT"], writes=["pm%d" % pi_])
                    r_ = kb - (nkb - 4)
                    if r_ >= -1:
                        u0 = 384 - 128 * r_
                        S.op("dve", lambda e: e.tensor_tensor(out=pm[pi_][:], in0=pm[pi_][:], in1=Wh[:, h, u0:u0 + 512], op=ALU.mult),
                             reads=["pm%d" % pi_, "Wh"], writes=["pm%d" % pi_])

                def stagePV(kb):
                    pi_ = sst[kb]

                    def pv(e):
                        e.matmul(ps[ob][:, :], va[:, kb, :], pm[pi_][:], start=(kb == 0), stop=(kb == nkb - 1))
                        return e.matmul(ps[db][:, :], onesb[:], pm[pi_][:], start=(kb == 0), stop=(kb == nkb - 1))
                    S.op("pe", pv, reads=["va", "pm%d" % pi_, "onesb"], writes=[PK[ob], PK[db]])
                for step in range(nkb + 2):
                    if step < nkb:
                        stageLG(step)
                    if 0 <= step - 2 < nkb:
                        stagePV(step - 2)
                S.op("dve", lambda e: e.reciprocal(out=rden[:], in_=ps[db][:, :]), reads=[PK[db]], writes=["rden"])
                S.op("dve", lambda e: e.tensor_tensor(out=oaT[:, h, j * 512:(j + 1) * 512], in0=ps[ob][:, :], in1=rden[:], op=ALU.mult),
                     reads=[PK[ob], "rden"], writes=["oaT"])

            for j in range(4):
                dsa_tile(j)
            S.emit()

        if stop == 3:
            finish(S, [("oaT", oaT, [128, 8, NOWN], BF16)])
            ats.close()
            return nc
        obT = sb(ats, "obT", [128, 8, NOWN], BF16)

        def oTc(kc, sl):
            return (oaT if kc < 8 else obT)[:, kc % 8, sl]

        with contextlib.ExitStack() as p4:
            kbt = [sb(p4, "kbt%d" % i, [128, SA], BF16) for i in range(2)]
            vbt = [sb(p4, "vbt%d" % i, [128, 32, 128], BF16) for i in range(2)]
            qbt = [sb(p4, "qbt%d" % i, [128, NOWN], BF16) for i in range(2)]
            NZ, NL, NC_, NA = 2, 4, 3, 4
            ez = [sb(p4, "ez%d" % i, [128, 512], F32) for i in range(NZ)]
            Lp = [sb(p4, "Lp%d" % i, [128, 512], BF16) for i in range(NL)]
            Cc = [sb(p4, "Cc%d" % i, [128, 512], BF16) for i in range(NC_)]
            at_ = [sb(p4, "at%d" % i, [128, 512], BF16) for i in range(NA)]
            zero_c = sb(p4, "zero_c", [128, 512], BF16)
            S.op("pool", lambda e: e.memset(zero_c[:], 0.0), writes=["zero_c"])
            gi = [0]
            cci = [0]
            for h in range(8):
                b_ = h % 2
                S.dma("sp", lambda e, b_=b_, h=h: e.dma_start(out=kbt[b_][:], in_=kbT_s[h]), reads=["kbT_s"], writes=["kbt%d" % b_])
                S.dma("sp", lambda e, b_=b_, h=h: e.dma_start(out=qbt[b_][:], in_=qbT_s[h]), reads=["qbT_s"], writes=["qbt%d" % b_])
                S.dma("sp", lambda e, b_=b_, h=h: e.dma_start(
                    out=vbt[b_][:], in_=vb_s[:, h * 128:(h + 1) * 128].rearrange("(b p) d -> p b d", p=128)),
                    reads=["vb_s"], writes=["vbt%d" % b_])
                kk, qk, vk = "kbt%d" % b_, "qbt%d" % b_, "vbt%d" % b_
                def sb_tile(h, j, b_, kk, qk, vk):
                    nkb = 8 * (j + 1)
                    obk = 4 + (h * 4 + j) % 2
                    order = list(reversed(range(nkb)))
                    n = len(order)
                    qslc = qbt[b_][:, j * 512:(j + 1) * 512]
                    st = {}

                    def maskof(kb):
                        if kb < 4:
                            return maskV[:]
                        if kb >= nkb - 4:
                            return maskS[:, kb - (nkb - 4), :]
                        return None

                    def stageZ(i):
                        kb = order[i]
                        g = gi[0]
                        gi[0] += 1
                        zb, li_ = g % NZ, g % NL
                        kslc = kbt[b_][:, kb * 128:(kb + 1) * 128]
                        st[i] = dict(kb=kb, zb=zb, li=li_, ai=g % NA, eb=2 + g % 2, kslc=kslc, mk=maskof(kb))
                        S.op("pe", lambda e: e.matmul(ps[zb][:, :], kslc, qslc, start=True, stop=True), reads=[kk, qk], writes=[PK[zb]])
                        S.op("act", lambda e: e.activation(out=ez[zb][:], in_=ps[zb][:, :], func=AF.Exp), reads=[PK[zb]], writes=["ez%d" % zb])
                        S.op("act", lambda e: e.activation(out=Lp[li_][:], in_=ez[zb][:], func=AF.Ln, bias=1.0),
                             reads=["ez%d" % zb], writes=["Lp%d" % li_])
                        mk = st[i]["mk"]
                        if mk is not None:
                            S.op("dve", lambda e: e.tensor_tensor(out=Lp[li_][:], in0=Lp[li_][:], in1=mk, op=ALU.mult),
                                 reads=["Lp%d" % li_, "maskV", "maskS"], writes=["Lp%d" % li_])
                        if i == 0:
                            st[i]["cc"] = None
                        else:
                            prev = st[i - 1]
                            c_new = cci[0] % NC_
                            cci[0] += 1
                            pl = prev["li"]
                            if prev["cc"] is None:
                                S.op("pool", lambda e: e.tensor_copy(out=Cc[c_new][:], in_=Lp[pl][:]), reads=["Lp%d" % pl], writes=["Cc%d" % c_new])
                            else:
                                pc = prev["cc"]
                                S.op("pool", lambda e: e.tensor_tensor(out=Cc[c_new][:], in0=Cc[pc][:], in1=Lp[pl][:], op=ALU.add),
                                     reads=["Cc%d" % pc, "Lp%d" % pl], writes=["Cc%d" % c_new])
                            st[i]["cc"] = c_new

                    def stageE(i):
                        s_ = st[i]
                        eb, kslc, li_, cc, ai = s_["eb"], s_["kslc"], s_["li"], s_["cc"], s_["ai"]
                        ccap = zero_c[:] if cc is None else Cc[cc][:]
                        cck = "zero_c" if cc is None else "Cc%d" % cc

                        def em(e):
                            e.matmul(ps[eb][:, :], kslc, qslc, start=True, stop=False)
                            e.matmul(ps[eb][:, :], negtri[:], Lp[li_][:], start=False, stop=False)
                            return e.matmul(ps[eb][:, :], negones[:], ccap, start=False, stop=True)
                        S.op("pe", em, reads=[kk, qk, "negtri", "negones", "Lp%d" % li_, cck], writes=[PK[eb]])
                        S.op("act", lambda e: e.activation(out=at_[ai][:], in_=ps[eb][:, :], func=AF.Exp), reads=[PK[eb]], writes=["at%d" % ai])
                        mk = s_["mk"]
                        if mk is not None:
                            S.op("dve", lambda e: e.tensor_tensor(out=at_[ai][:], in0=at_[ai][:], in1=mk, op=ALU.mult),
                                 reads=["at%d" % ai, "maskV", "maskS"], writes=["at%d" % ai])

                    def stageV(i):
                        s_ = st[i]
                        kb, ai = s_["kb"], s_["ai"]
                        S.op("pe", lambda e: e.matmul(ps[obk][:, :], vbt[b_][:, kb, :], at_[ai][:], start=(i == 0), stop=(i == n - 1)),
                             reads=[vk, "at%d" % ai], writes=[PK[obk]])

                    for step in range(n + 2):
                        if step < n:
                            stageZ(step)
                        if 0 <= step - 1 < n:
                            stageE(step - 1)
                        if 0 <= step - 2 < n:
                            stageV(step - 2)
                    S.op("act", lambda e, obk=obk, h=h, j=j: e.activation(out=obT[:, h, j * 512:(j + 1) * 512], in_=ps[obk][:, :],
                                                                         func=AF.Copy), reads=[PK[obk]], writes=["obT"])
                for j in range(4):
                    sb_tile(h, j, b_, kk, qk, vk)
            S.emit()
        if stop == 4:
            finish(S, [("oaT", oaT, [128, 8, NOWN], BF16), ("obT", obT, [128, 8, NOWN], BF16)])
            ats.close()
            return nc
        with contextlib.ExitStack() as p5:
            wo = sb(p5, "wo", [128, KC, D], BF16)
            sq = [sb(p5, "sq%d" % i, [128, 512], BF16) for i in range(2)]
            rs = sb(p5, "rs", [128, 512], F32)
            xs5 = [sb(p5, "xs5_%d" % i, [128, D], F32) for i in range(1)]
            t5 = [sb(p5, "t5_%d" % i, [128, D], F32) for i in range(1)]
            for cg in range(4):
                S.dma("pool", lambda e, cg=cg: e.dma_start(out=wo[:, :, cg * 512:(cg + 1) * 512],
                                                           in_=w_out[:, cg * 512:(cg + 1) * 512].rearrange("(c p) n -> p c n", p=128)),
                      writes=["wo"])
            r5 = [0]
            for j in range(4):
                for g in range(2):
                    gn = gna if g == 0 else gnb
                    gk = "gna" if g == 0 else "gnb"
                    pb = r5[0] % 2
                    r5[0] += 1
                    for c_ in range(8):
                        si = c_ % 2
                        S.op("act", lambda e, si=si, g=g, c_=c_, j=j: e.activation(out=sq[si][:], in_=oTc(g * 8 + c_, slice(j * 512, (j + 1) * 512)),
                                                                                 func=AF.Square), reads=["oaT", "obT"], writes=["sq%d" % si])
                        S.op("pe", lambda e, pb=pb, si=si, c_=c_: e.matmul(ps[pb][:, :], onesb[:], sq[si][:], start=(c_ == 0), stop=(c_ == 7)),
                             reads=["sq%d" % si, "onesb"], writes=[PK[pb]])
                    S.op("act", lambda e, pb=pb: e.activation(out=rs[:], in_=ps[pb][:, :], func=AF.Sqrt, scale=1.0 / 1024.0, bias=EPS),
                         reads=[PK[pb]], writes=["rs"])
                    S.op("dve", lambda e: e.reciprocal(out=rs[:], in_=rs[:]), reads=["rs"], writes=["rs"])
                    for c_ in range(8):
                        S.op("dve", lambda e, g=g, c_=c_, j=j, gn=gn: e.scalar_tensor_tensor(
                            out=oTc(g * 8 + c_, slice(j * 512, (j + 1) * 512)), in0=oTc(g * 8 + c_, slice(j * 512, (j + 1) * 512)),
                            scalar=gn[:, c_:c_ + 1], in1=rs[:], op0=ALU.mult, op1=ALU.mult),
                            reads=["oaT", "obT", "rs", gk], writes=["oaT" if g == 0 else "obT"])
            for tb in range(16):
                at = 2 * (tb // 4) + 1
                r0 = at * 512 + (tb % 4) * 128
                xb, xbk = xs5[0], "xs5_0"
                tt_, ttk = t5[0], "t5_0"
                S.dma("sp", lambda e, xb=xb, r0=r0: e.dma_start(out=xb[:], in_=xk[r0:r0 + 128, :]), writes=[xbk])
                for cg in range(4):
                    pb = 2 + r5[0] % 4
                    r5[0] += 1

                    def mm(e, pb=pb, tb=tb, cg=cg):
                        r = None
                        for kc in range(KC):
                            r = e.matmul(ps[pb][:, :], oTc(kc, slice(tb * 128, (tb + 1) * 128)), wo[:, kc, cg * 512:(cg + 1) * 512],
                                         start=(kc == 0), stop=(kc == KC - 1))
                        return r
                    S.op("pe", mm, reads=["oaT", "obT", "wo"], writes=[PK[pb]])
                    S.op("dve", lambda e, pb=pb, tt_=tt_, cg=cg: e.tensor_tensor(out=tt_[:, cg * 512:(cg + 1) * 512], in0=ps[pb][:, :],
                                                                                in1=g1_bc[:, cg * 512:(cg + 1) * 512], op=ALU.mult),
                         reads=[PK[pb], "g1_bc"], writes=[ttk])
                S.op("pool", lambda e, tt_=tt_, xb=xb: e.tensor_tensor(out=tt_[:], in0=tt_[:], in1=xb[:], op=ALU.add),
                     reads=[ttk, xbk], writes=[ttk])
                S.dma("sp", lambda e, tt_=tt_, tb=tb: e.dma_start(out=x1_s[tb * 128:(tb + 1) * 128, :], in_=tt_[:]),
                      reads=[ttk], writes=["x1_s"])
            S.emit()
        if stop == 5:
            finish(S, [("oaT", oaT, [128, 8, NOWN], BF16), ("obT", obT, [128, 8, NOWN], BF16)])
            ats.close()
            return nc
        ats.close()

        with contextlib.ExitStack() as p6:
            x1t = sb(p6, "x1t", [128, 4, D], F32)
            h2T = sb(p6, "h2T", [128, KC, 512], BF16)
            acc = sb(p6, "acc", [128, 4, D], F32)
            h2T32 = acc[:].rearrange("p b (q n) -> p (b q) n", q=4)
            rt = sb(p6, "rt", [128, KC, 36], F32)
            wg = [sb(p6, "wg%d" % i, [128, KC, DE], BF16) for i in range(2)]
            wu = [sb(p6, "wu%d" % i, [128, KC, DE], BF16) for i in range(2)]
            wd = [sb(p6, "wd%d" % i, [128, 4, D], BF16) for i in range(2)]
            sg = [sb(p6, "sg%d" % i, [128, 512], BF16) for i in range(2)]
            uT = [sb(p6, "uT%d" % i, [128, 4, 512], BF16) for i in range(2)]
            rsm = sb(p6, "rsm", [128, 16], F32)
            lgB = sb(p6, "lgB", [128, 4, 36], F32)
            lemB = sb(p6, "lemB", [128, 4, 32], F32)
            m8B = sb(p6, "m8B", [128, 4, 32], F32)
            rsmB = sb(p6, "rsmB", [128, 4, 16], F32)
            import os
            P6CUT = int(os.environ.get("P6CUT", "9"))
            is1B = sb(p6, "is1B", [128, 4, 32], F32)
            is2B = sb(p6, "is2B", [128, 4, 32], F32)
            Gt = sb(p6, "Gt", [128, 4, 32], F32)
            S.dma("sp", lambda e: e.dma_start(out=rt[:], in_=router_d.rearrange("(c p) n -> p c n", p=128)), writes=["rt"])
            r6 = [0]
            ei = [0]
            def route_block(b):
                ops = []
                lg, rr = lgB[:, b, :], rsmB[:, b, :]
                i1, i2, lm, mm8 = is1B[:, b, :], is2B[:, b, :], lemB[:, b, :], m8B[:, b, :]

                def kx(keys):
                    return [k if k == "Gt" else "%s_%d" % (k, b) for k in keys]

                def dv(fn, reads, writes):
                    ops.append(("dve", fn, kx(reads), kx(writes)))

                def ac(fn, reads, writes):
                    ops.append(("act", fn, kx(reads), kx(writes)))
                TT = lambda o, a_, b_, op: (lambda e: e.tensor_tensor(out=o, in0=a_, in1=b_, op=op))
                dv(TT(rr[:, 0:2], lg[:, 0:2], lg[:, 2:4], ALU.max), ["lg"], ["rA"])
                dv(TT(rr[:, 8:9], rr[:, 0:1], rr[:, 1:2], ALU.max), ["rA"], ["r8"])
                dv(lambda e: e.tensor_scalar(out=rr[:, 9:10], in0=rr[:, 8:9], scalar1=-1.0, scalar2=None, op0=ALU.mult), ["r8"], ["r9"])
                ac(lambda e: e.activation(out=i1[:, 0:4], in_=lg[:, 0:4], func=AF.Exp, bias=rr[:, 9:10]), ["lg", "r9"], ["is1"])
                dv(TT(rr[:, 0:2], i1[:, 0:2], i1[:, 2:4], ALU.add), ["is1", "rA"], ["rA"])
                dv(TT(rr[:, 10:11], rr[:, 0:1], rr[:, 1:2], ALU.add), ["rA"], ["r10"])
                dv(lambda e: e.reciprocal(out=rr[:, 11:12], in_=rr[:, 10:11]), ["r10"], ["r11"])
                dv(lambda e: e.tensor_scalar(out=i2[:, 0:4], in0=lg[:, 0:4], scalar1=rr[:, 8:9], scalar2=None, op0=ALU.is_ge),
                   ["lg", "r8"], ["is2"])
                dv(lambda e: e.tensor_scalar(out=i2[:, 0:4], in0=i2[:, 0:4], scalar1=-1.0, scalar2=BIG, op0=ALU.add, op1=ALU.mult),
                   ["is2"], ["is2"])
                for g in range(4):
                    dv(lambda e, g=g: e.tensor_scalar(out=lm[:, g * 8:(g + 1) * 8], in0=lg[:, 4 + g * 8:12 + g * 8],
                                                      scalar1=i2[:, g:g + 1], scalar2=None, op0=ALU.add), ["lg", "is2"], ["lem"])

                def tree_max(outcol, okey):
                    dv(TT(mm8[:, 0:16], lm[:, 0:16], lm[:, 16:32], ALU.max), ["lem", "m8"], ["m8"])
                    dv(TT(mm8[:, 16:24], mm8[:, 0:8], mm8[:, 8:16], ALU.max), ["m8"], ["m8"])
                    dv(TT(mm8[:, 24:28], mm8[:, 16:20], mm8[:, 20:24], ALU.max), ["m8"], ["m8"])
                    dv(TT(mm8[:, 28:30], mm8[:, 24:26], mm8[:, 26:28], ALU.max), ["m8"], ["m8"])
                    dv(TT(outcol, mm8[:, 28:29], mm8[:, 29:30], ALU.max), ["m8"], [okey])
                tree_max(rr[:, 2:3], "l1")
                dv(lambda e: e.tensor_scalar(out=i1, in0=lm, scalar1=rr[:, 2:3], scalar2=None, op0=ALU.is_ge), ["lem", "l1"], ["is1"])
                dv(lambda e: e.scalar_tensor_tensor(out=lm, in0=i1, scalar=-BIG, in1=lm, op0=ALU.mult, op1=ALU.add),
                   ["is1", "lem"], ["lem"])
                tree_max(rr[:, 3:4], "l2")
                dv(lambda e: e.tensor_scalar(out=i2, in0=lm, scalar1=rr[:, 3:4], scalar2=None, op0=ALU.is_ge), ["lem", "l2"], ["is2"])
                dv(lambda e: e.tensor_scalar(out=rr[:, 12:13], in0=rr[:, 2:3], scalar1=-1.0, scalar2=None, op0=ALU.mult), ["l1"], ["r12"])
                ac(lambda e: e.activation(out=rr[:, 13:14], in_=rr[:, 3:4], func=AF.Exp, bias=rr[:, 12:13]), ["l2", "r12"], ["r13"])
                dv(lambda e: e.tensor_scalar(out=rr[:, 14:15], in0=rr[:, 13:14], scalar1=1.0, scalar2=None, op0=ALU.add), ["r13"], ["r14"])
                dv(lambda e: e.reciprocal(out=rr[:, 14:15], in_=rr[:, 14:15]), ["r14"], ["r14"])
                dv(TT(rr[:, 14:15], rr[:, 14:15], rr[:, 11:12], ALU.mult), ["r14", "r11"], ["r14"])
                dv(TT(rr[:, 15:16], rr[:, 14:15], rr[:, 13:14], ALU.mult), ["r14", "r13"], ["r15"])
                dv(lambda e: e.tensor_scalar(out=Gt[:, b, :], in0=i1, scalar1=rr[:, 14:15], scalar2=None, op0=ALU.mult), ["is1", "r14"], ["Gt"])
                dv(lambda e: e.scalar_tensor_tensor(out=Gt[:, b, :], in0=i2, scalar=rr[:, 15:16], in1=Gt[:, b, :],
                                                    op0=ALU.mult, op1=ALU.add), ["is2", "r15", "Gt"], ["Gt"])
                return ops

            for tt in range(4):
                S.dma("sp", lambda e, tt=tt: e.dma_start(out=x1t[:], in_=x1_s[tt * 512:(tt + 1) * 512, :].rearrange("(b p) n -> p b n", p=128)),
                      reads=["x1_s"], writes=["x1t"])
                P6SKIP = os.environ.get("P6SKIP", "")
                for b in range(0 if "sq" in P6SKIP else 4):
                    S.op("act", lambda e, b=b: e.activation(out=acc[:, 0, :], in_=x1t[:, b, :], func=AF.Square, accum_out=rsm[:, b:b + 1]),
                         reads=["x1t"], writes=["acc", "rsm"])
                S.op("act", lambda e: e.activation(out=rsm[:, 4:8], in_=rsm[:, 0:4], func=AF.Sqrt, scale=1.0 / D, bias=EPS),
                     reads=["rsm"], writes=["rsm4"])
                S.op("dve", lambda e: e.reciprocal(out=rsm[:, 4:8], in_=rsm[:, 4:8]), reads=["rsm4"], writes=["rsm4"])
                for b in range(4):
                    S.op("dve", lambda e, b=b: e.tensor_scalar(out=x1t[:, b, :], in0=x1t[:, b, :], scalar1=rsm[:, 4 + b:5 + b], scalar2=None,
                                                               op0=ALU.mult), reads=["x1t", "rsm4"], writes=["x1t"])
                for kc in range(0 if "tr" in P6SKIP else KC):
                    pi = 4 + r6[0] % 4
                    r6[0] += 1

                    def tr(e, kc=kc, pi=pi):
                        r = None
                        for b in range(4):
                            r = e.transpose(ps[pi][:, b * 128:(b + 1) * 128], x1t[:, b, kc * 128:(kc + 1) * 128], ident[:])
                        return r
                    S.op("pe", tr, reads=["x1t", "ident"], writes=[PK[pi]])
                    S.op("dve", lambda e, kc=kc, pi=pi: e.tensor_scalar(out=h2T[:, kc, :], in0=ps[pi][:, :], scalar1=G2[:, kc:kc + 1],
                                                                       scalar2=modT[:, 48 + kc:49 + kc], op0=ALU.mult, op1=ALU.add),
                         reads=[PK[pi], "G2", "modT"], writes=["h2T"])
                    S.op("act", lambda e, kc=kc, pi=pi: e.activation(out=h2T32[:, kc, :], in_=ps[pi][:, :], func=AF.Identity,
                                                                    scale=G2[:, kc:kc + 1], bias=modT[:, 48 + kc:49 + kc]),
                         reads=[PK[pi], "G2", "modT"], writes=["acc"])
                for b in range(4 if P6CUT >= 2 else 0):
                    pi = 4 + r6[0] % 4
                    r6[0] += 1

                    def rmm(e, b=b, pi=pi):
                        r = None
                        for kc in range(KC):
                            r = e.matmul(ps[pi][:, 0:36], h2T32[:, kc, b * 128:(b + 1) * 128], rt[:, kc, :], start=(kc == 0), stop=(kc == KC - 1))
                        return r
                    S.op("pe", rmm, reads=["acc", "rt"], writes=[PK[pi]])
                    S.op("dve", lambda e, pi=pi, b=b: e.tensor_copy(out=lgB[:, b, :], in_=ps[pi][:, 0:36]), reads=[PK[pi]], writes=["lg_%d" % b])
                pend = [route_block(b) for b in range(4)]
                for k in range(len(pend[0])):
                    for b in range(4):
                        eng_, fn_, rd_, wr_ = pend[b][k]
                        S.op(eng_, fn_, reads=rd_, writes=wr_)
                if "ms" not in P6SKIP:
                    S.op("pool", lambda e: e.memset(acc[:], 0.0), writes=["acc"])
                for ex in range(NE if P6CUT >= 5 else (2 if P6CUT >= 4 else 0)):
                    wi = ei[0] % 2
                    ei[0] += 1
                    if not (os.environ.get("P6NODMA") and (tt > 0 or ex >= 2)):
                      S.dma("pool", lambda e, wi=wi, ex=ex: e.dma_start(out=wg[wi][:], in_=w_gate[ex].rearrange("(c p) n -> p c n", p=128)),
                          writes=["wg%d" % wi])
                      S.dma("pool", lambda e, wi=wi, ex=ex: e.dma_start(out=wu[wi][:], in_=w_up[ex].rearrange("(c p) n -> p c n", p=128)),
                          writes=["wu%d" % wi])
                      S.dma("pool", lambda e, wi=wi, ex=ex: [e.dma_start(
                        out=wd[wi][:, :, hf * 1024:(hf + 1) * 1024],
                        in_=w_down[ex][:, hf * 1024:(hf + 1) * 1024].rearrange("(c p) n -> p c n", p=128)) for hf in range(2)],
                        writes=["wd%d" % wi], n=2)
                    ut, utk = uT[wi], "uT%d" % wi
                    for fc in range(4):
                        ga, ua = 2 * (fc % 2), 1 + 2 * (fc % 2)

                        def gm(e, wi=wi, fc=fc, ga=ga, ua=ua):
                            for kc in range(KC):
                                e.matmul(ps[ga][:, :], wg[wi][:, kc, fc * 128:(fc + 1) * 128], h2T[:, kc, :], start=(kc == 0), stop=(kc == KC - 1))
                            r = None
                            for kc in range(KC):
                                r = e.matmul(ps[ua][:, :], wu[wi][:, kc, fc * 128:(fc + 1) * 128], h2T[:, kc, :], start=(kc == 0), stop=(kc == KC - 1))
                            return r
                        S.op("pe", gm, reads=["wg%d" % wi, "wu%d" % wi, "h2T"], writes=[PK[ga], PK[ua]])
                        S.op("act", lambda e, fc=fc, ga=ga: e.activation(out=sg[fc % 2][:], in_=ps[ga][:, :], func=AF.Silu),
                             reads=[PK[ga]], writes=["sg%d" % (fc % 2)])
                        S.op("dve", lambda e, fc=fc, ua=ua, ut=ut: e.tensor_tensor(out=ut[:, fc, :], in0=sg[fc % 2][:], in1=ps[ua][:, :], op=ALU.mult),
                             reads=["sg%d" % (fc % 2), PK[ua]], writes=[utk])
                    for b in range(4):
                        for dg in range(4):
                            yb = 4 + r6[0] % 4
                            r6[0] += 1

                            def dm(e, yb=yb, b=b, dg=dg, wi=wi, ut=ut):
                                r = None
                                for fc in range(4):
                                    r = e.matmul(ps[yb][:, :], ut[:, fc, b * 128:(b + 1) * 128], wd[wi][:, fc, dg * 512:(dg + 1) * 512],
                                                 start=(fc == 0), stop=(fc == 3))
                                return r
                            S.op("pe", dm, reads=[utk, "wd%d" % wi], writes=[PK[yb]])
                            S.op("dve", lambda e, yb=yb, b=b, dg=dg, ex=ex: e.scalar_tensor_tensor(
                                out=acc[:, b, dg * 512:(dg + 1) * 512], in0=ps[yb][:, :], scalar=Gt[:, b, ex:ex + 1],
                                in1=acc[:, b, dg * 512:(dg + 1) * 512], op0=ALU.mult, op1=ALU.add),
                                reads=[PK[yb], "Gt", "acc"], writes=["acc"])
                S.dma("sp", lambda e, tt=tt: e.dma_start(out=x1t[:], in_=x1_s[tt * 512:(tt + 1) * 512, :].rearrange("(b p) n -> p b n", p=128)),
                      reads=["x1_s"], writes=["x1t"])
                for b in range(4):
                    S.op("dve", lambda e, b=b: e.tensor_tensor(out=acc[:, b, :], in0=acc[:, b, :], in1=g2_bc[:], op=ALU.mult),
                         reads=["acc", "g2_bc"], writes=["acc"])
                    S.op("pool", lambda e, b=b: e.tensor_tensor(out=acc[:, b, :], in0=acc[:, b, :], in1=x1t[:, b, :], op=ALU.add),
                         reads=["acc", "x1t"], writes=["acc"])
                S.dma("sp", lambda e, tt=tt: e.dma_start(out=out_d[tt * 512:(tt + 1) * 512, :].rearrange("(b p) n -> p b n", p=128), in_=acc[:]),
                      reads=["acc"], writes=["out_d"])
            S.emit()
    return nc


def _t5_bucket(dist):
    max_exact = 16
    d = np.maximum(dist, 1).astype(np.float32)
    large = max_exact + (np.log(d / max_exact) / np.log(128 / max_exact) * (32 - max_exact)).astype(np.int32)
    large = np.minimum(large, 31)
    return np.where(dist < max_exact, dist, large)


_NC_CACHE = {}


def kernel(x, c, w_mod, b_mod, ln1_g, w_in, q_norm_g, w_q_up, q_gain, k_gain, rel_bias, gn_a, gn_b, w_out, ln2_g,
           router_g, router_e, w_gate, w_up, w_down):
    f32 = np.float32
    x = np.asarray(x, f32)
    c = np.asarray(c, f32)

    def col(v, n):
        return np.ascontiguousarray(np.asarray(v, f32).reshape(n, 128).T)

    s_ = np.arange(128)[:, None]
    u_ = np.arange(1024)[None, :]
    delta = np.clip(u_ - 384 - s_, 0, 4095)
    bidx = _t5_bucket(delta)
    rb = np.asarray(rel_bias, f32)
    tbias = np.ascontiguousarray(np.transpose(rb[bidx], (2, 0, 1)))
    rb31 = np.ascontiguousarray(np.broadcast_to(rb[31][None, :], (128, 8)))
    shared = {
        "w_mod": np.ascontiguousarray(np.asarray(w_mod, f32)[0]),
        "b_mod": np.ascontiguousarray(np.asarray(b_mod, f32)[0][None, :]),
        "ln1g": col(ln1_g[0], 16), "ln2g": col(ln2_g[0], 16),
        "w_in": np.ascontiguousarray(np.asarray(w_in, f32)[0]),
        "qng": col(q_norm_g[0], 4),
        "w_qup": np.ascontiguousarray(np.asarray(w_q_up, f32)[0]),
        "qgain": col(q_gain[0], 1), "kgain": col(k_gain[0], 1),
        "gna": col(gn_a[0], 8), "gnb": col(gn_b[0], 8),
        "w_out": np.ascontiguousarray(np.asarray(w_out, f32)[0]),
        "router": np.ascontiguousarray(np.concatenate([np.asarray(router_g, f32)[0], np.asarray(router_e, f32)[0]], axis=1)),
        "w_gate": np.ascontiguousarray(np.asarray(w_gate, f32)[0]),
        "w_up": np.ascontiguousarray(np.asarray(w_up, f32)[0]),
        "w_down": np.ascontiguousarray(np.asarray(w_down, f32)[0]),
        "tbias": tbias, "rb31": rb31,
    }
    in_maps = []
    for core in range(8):
        b, h = core // 2, core % 2
        tiles = x[b].reshape(8, 512, D)
        if h == 1:
            xk = x[b]
        else:
            xk = np.concatenate([tiles[0:1], tiles[0:7]], axis=0).reshape(SA, D)
        m = dict(shared)
        m["xk"] = np.ascontiguousarray(xk)
        m["kvalid"] = np.full((128, 1), float(h), f32)
        m["c_col"] = col(c[b], 16)
        in_maps.append(m)
    if "nc" not in _NC_CACHE:
        _NC_CACHE["nc"] = build_nc()
    res = run_bass_kernel_spmd(_NC_CACHE["nc"], in_maps, core_ids=list(range(8)))
    out = np.empty((4, 4096, D), f32)
    for core in range(8):
        b, h = core // 2, core % 2
        o = np.asarray(res.results[core]["out"], f32).reshape(4, 512, D)
        for j in range(4):
            g = 2 * j + h
            out[b, g * 512:(g + 1) * 512] = o[j]
    return out
```

```python
import contextlib
import numpy as np
import concourse.bass as bass
import concourse.mybir as mybir
from concourse.bass_utils import run_bass_kernel_spmd

F32 = mybir.dt.float32
BF16 = mybir.dt.bfloat16
AF = mybir.ActivationFunctionType
ALU = mybir.AluOpType
AX = mybir.AxisListType

D = 2048
KC = 16
SA = 4096
NOWN = 2048
HD = 128
NE = 32
DE = 512
EPS = 1e-6
BIG = 1.0e30
TOPK = 256
NBIS = 14
IN_COLS = (512, 128, 128, 64, 16, 1024, 1024, 1024)
D_IN = sum(IN_COLS)
DEBUG = None


class Sched:
    ENG = ("pe", "act", "dve", "pool", "sp")
    BLK = {"pe": "tensor", "act": "scalar", "dve": "vector", "pool": "gpsimd", "sp": "sync"}

    def __init__(self, nc, stack):
        self.nc = nc
        self.stack = stack
        self.sem = {e: stack.enter_context(nc.semaphore("sem_" + e)) for e in self.ENG}
        self.cnt = {e: 0 for e in self.ENG}
        self.ops = {e: [] for e in self.ENG}
        self.waited = {e: {} for e in self.ENG}
        self.lastw = {}
        self.readers = {}
        self.touched = set()

    def _semfor(self, slot):
        name = "d_" + str(slot)
        if name not in self.sem:
            self.sem[name] = self.stack.enter_context(self.nc.semaphore(name))
            self.cnt[name] = 0
        return name

    def _deps(self, eng, reads, writes):
        need = {}

        def add(tok):
            if tok is None:
                return
            s, v = tok
            if need.get(s, 0) < v:
                need[s] = v
        for k in reads:
            add(self.lastw.get(k))
            if k.startswith("ps"):
                for t in self.readers.get(k, ()):
                    add(t)
        for k in writes:
            add(self.lastw.get(k))
            for t in self.readers.get(k, ()):
                add(t)
        out = []
        for s, v in need.items():
            if s == eng and eng == "pe":
                continue
            if self.waited[eng].get(s, 0) >= v:
                continue
            self.waited[eng][s] = v
            out.append((s, v))
        return out

    def _record(self, tok, reads, writes):
        for k in reads:
            self.readers.setdefault(k, []).append(tok)
        for k in writes:
            self.lastw[k] = tok
            self.readers[k] = []

    def op(self, eng, fn, reads=(), writes=()):
        waits = self._deps(eng, reads, writes)
        self.cnt[eng] += 1
        tok = (eng, self.cnt[eng])
        self.ops[eng].append((waits, fn, eng, 1))
        self._record(tok, reads, writes)
        return tok

    def dma(self, q, fn, reads=(), writes=(), slot=None, n=1):
        if slot is None:
            slot = writes[0]
        name = self._semfor(slot)
        waits = self._deps(q, reads, writes)
        self.cnt[name] += 16 * n
        tok = (name, self.cnt[name])
        self.ops[q].append((waits, fn, name, 16))
        self._record(tok, reads, writes)
        self.touched.add(name)
        return tok

    def drain(self):
        waits = []
        for name in sorted(self.touched):
            v = self.cnt[name]
            if self.waited["sp"].get(name, 0) < v:
                self.waited["sp"][name] = v
                waits.append((name, v))
        self.touched = set()
        if waits:
            self.ops["sp"].append((waits, None, None, 0))

    def emit(self):
        self.drain()
        with self.nc.Block() as block:
            for eng in self.ENG:
                ops = self.ops[eng]
                if not ops:
                    continue

                def body(e, ops=ops):
                    for waits, fn, semname, inc in ops:
                        for s, v in waits:
                            e.wait_ge(self.sem[s], v)
                        if fn is None:
                            continue
                        r = fn(e)
                        if isinstance(r, (list, tuple)):
                            for ins in r:
                                ins.then_inc(self.sem[semname], inc)
                        else:
                            r.then_inc(self.sem[semname], inc)
                getattr(block, self.BLK[eng])(body)
        self.ops = {e: [] for e in self.ENG}


def build_nc(stop=6):
    nc = bass.Bass("TRN2", target_bir_lowering=False)
    dbg = stop < 6

    def din(name, shape, dt=F32):
        return nc.dram_tensor(name, list(shape), dt, kind="ExternalInput").ap()

    xk = din("xk", [SA, D])
    kvalid_d = din("kvalid", [128, 1])
    c_col_d = din("c_col", [128, KC])
    w_mod = din("w_mod", [D, 6 * D])
    b_mod = din("b_mod", [1, 6 * D])
    ln1g_d = din("ln1g", [128, KC])
    ln2g_d = din("ln2g", [128, KC])
    w_in = din("w_in", [D, D_IN])
    qng_d = din("qng", [128, 4])
    w_qup = din("w_qup", [512, 2048])
    qgain_d = din("qgain", [128, 1])
    kgain_d = din("kgain", [128, 1])
    gna_d = din("gna", [128, 8])
    gnb_d = din("gnb", [128, 8])
    w_out = din("w_out", [D, D])
    router_d = din("router", [D, 36])
    if not dbg:
        w_gate = din("w_gate", [NE, D, DE])
        w_up = din("w_up", [NE, D, DE])
        w_down = din("w_down", [NE, DE, D])
    tb_d = din("tbias", [8, 128, 1024])
    rb31_d = din("rb31", [128, 8])
    out_d = nc.dram_tensor("out", [NOWN, D], F32, kind="ExternalOutput").ap()

    def dscr(name, shape, dt):
        return nc.dram_tensor(name, list(shape), dt, kind="ExternalOutput").ap()

    dbg_outs = {}

    def finish(S, items):
        for name, t, shape, dt in items:
            d_ = nc.dram_tensor("dbg_" + name, list(shape), dt, kind="ExternalOutput").ap()
            S.dma("sp", lambda e, t=t, d_=d_: e.dma_start(out=d_, in_=t[:]), reads=[name], writes=["dbg_" + name])
        if items:
            S.emit()

    kbT_s = dscr("kbT_s", [8, 128, SA], BF16)
    vb_s = dscr("vb_s", [SA, 1024], BF16)
    qbT_s = dscr("qbT_s", [8, 128, NOWN], BF16)
    qaT_s = dscr("qaT_s", [4, 128, 8, 512], BF16)
    qiT_s = dscr("qiT_s", [4, 128, 8, 512], BF16)
    x1_s = dscr("x1_s", [NOWN, D], F32)

    with contextlib.ExitStack() as gs:
        S = Sched(nc, gs)

        def sb(stack, name, shape, dt):
            return stack.enter_context(nc.sbuf_tensor(name, list(shape), dt))

        ps = [gs.enter_context(nc.psum_tensor("ps%d" % i, [128, 512], F32)) for i in range(8)]
        PK = ["ps%d" % i for i in range(8)]

        ident = sb(gs, "ident", [128, 128], F32)
        identb = sb(gs, "identb", [128, 128], BF16)
        onesb = sb(gs, "onesb", [128, 128], BF16)
        negtri = sb(gs, "negtri", [128, 128], BF16)
        negones = sb(gs, "negones", [128, 128], BF16)
        ones_row = sb(gs, "ones_row", [1, 128], F32)
        modT = sb(gs, "modT", [128, 96], F32)
        G1 = sb(gs, "G1", [128, KC], F32)
        G2 = sb(gs, "G2", [128, KC], F32)
        g2_bc = sb(gs, "g2_bc", [128, D], F32)
        kvalid = sb(gs, "kvalid_t", [128, 1], F32)
        qng = sb(gs, "qng_t", [128, 4], F32)
        qgain = sb(gs, "qgain_t", [128, 1], F32)
        kgain = sb(gs, "kgain_t", [128, 1], F32)
        gna = sb(gs, "gna_t", [128, 8], F32)
        gnb = sb(gs, "gnb_t", [128, 8], F32)
        ats = contextlib.ExitStack()
        g1_bc = sb(ats, "g1_bc", [128, D], F32)
        with contextlib.ExitStack() as p0:
            c_col = sb(p0, "c_col_t", [128, KC], F32)
            sc_col = sb(p0, "sc_col", [128, KC], F32)
            mrow = [sb(p0, "mrow%d" % i, [1, 512], F32) for i in range(2)]
            bmc = [sb(p0, "bmc%d" % i, [1, 512], F32) for i in range(2)]
            ln1g = sb(p0, "ln1g_t", [128, KC], F32)
            ln2g = sb(p0, "ln2g_t", [128, KC], F32)
            tmpc = sb(p0, "tmpc", [128, KC], F32)
            one11 = sb(p0, "one11", [1, 1], F32)
            onesf0 = sb(p0, "onesf0", [128, 128], F32)
            wm = [sb(p0, "wm%d" % i, [128, KC, 512], F32) for i in range(3)]

            small = [(c_col, c_col_d, "c_col"), (ln1g, ln1g_d, "ln1g"), (ln2g, ln2g_d, "ln2g"), (qng, qng_d, "qng"),
                     (qgain, qgain_d, "qgain"), (kgain, kgain_d, "kgain"), (gna, gna_d, "gna"), (gnb, gnb_d, "gnb"),
                     (kvalid, kvalid_d, "kvalid")]
            for t, d_, k in small:
                S.dma("sp", lambda e, t=t, d_=d_: e.dma_start(out=t[:], in_=d_), writes=[k])

            S.op("pool", lambda e: e.memset(onesf0[:], 1.0), writes=["onesf0"])
            S.op("pool", lambda e: e.memset(ones_row[:], 1.0), writes=["ones_row"])
            S.op("pool", lambda e: e.memset(one11[:], 1.0), writes=["one11"])
            S.op("pool", lambda e: e.memset(onesb[:], 1.0), writes=["onesb"])
            S.op("pool", lambda e: e.memset(negones[:], -1.0), writes=["negones"])
            S.op("pool", lambda e: e.affine_select(out=ident[:], in_=onesf0[:], pattern=[[-1, 128]],
                                                   compare_op=ALU.is_equal, fill=0.0, base=0, channel_multiplier=1),
                 reads=["onesf0"], writes=["ident"])
            S.op("pool", lambda e: e.tensor_copy(out=identb[:], in_=ident[:]), reads=["ident"], writes=["identb"])
            S.op("pool", lambda e: e.affine_select(out=negtri[:], in_=negones[:], pattern=[[-1, 128]],
                                                   compare_op=ALU.is_ge, fill=0.0, base=0, channel_multiplier=1),
                 reads=["negones"], writes=["negtri"])
            S.op("act", lambda e: e.activation(out=sc_col[:], in_=c_col[:], func=AF.Silu), reads=["c_col"],
                 writes=["sc_col"])
            for g in range(24):
                wt = wm[g % 3]
                wk = "wm%d" % (g % 3)
                mi = g % 2
                S.dma("sp", lambda e, wt=wt, g=g: e.dma_start(
                    out=wt[:], in_=w_mod[:, g * 512:(g + 1) * 512].rearrange("(c p) n -> p c n", p=128)), writes=[wk])
                S.dma("sp", lambda e, mi=mi, g=g: e.dma_start(out=bmc[mi][:], in_=b_mod[:, g * 512:(g + 1) * 512]),
                      writes=["bmc%d" % mi])
                pk = g % 2

                def mm(e, wt=wt, pk=pk):
                    r = None
                    for k in range(KC):
                        r = e.matmul(ps[pk][0:1, :], sc_col[:, k:k + 1], wt[:, k, :], start=(k == 0), stop=(k == KC - 1))
                    return r
                S.op("pe", mm, reads=[wk, "sc_col"], writes=[PK[pk]])
                S.op("dve", lambda e, mi=mi, pk=pk: e.tensor_tensor(out=mrow[mi][:], in0=ps[pk][0:1, :], in1=bmc[mi][:], op=ALU.add),
                     reads=[PK[pk], "bmc%d" % mi], writes=["mrow%d" % mi])

                def mmT(e, mi=mi, g=g):
                    r = None
                    for q in range(4):
                        j = g * 4 + q
                        r = e.matmul(ps[2][:, j:j + 1], mrow[mi][0:1, q * 128:(q + 1) * 128], one11[0:1, 0:1], start=True, stop=True)
                    return r
                S.op("pe", mmT, reads=["mrow%d" % mi, "one11"], writes=[PK[2]])
                if 8 <= g < 12 or 20 <= g < 24:
                    dst, dk, q = (g1_bc, "g1_bc", g - 8) if g < 12 else (g2_bc, "g2_bc", g - 20)
                    pb = 3 + g % 2
                    S.op("pe", lambda e, pb=pb, mi=mi: e.matmul(ps[pb][:, :], ones_row[0:1, :], mrow[mi][0:1, :], start=True, stop=True),
                         reads=["mrow%d" % mi, "ones_row"], writes=[PK[pb]])
                    S.op("dve", lambda e, dst=dst, pb=pb, q=q: e.tensor_copy(out=dst[:, q * 512:(q + 1) * 512], in_=ps[pb][:, :]),
                         reads=[PK[pb]], writes=[dk])
            S.op("dve", lambda e: e.tensor_copy(out=modT[:], in_=ps[2][:, 0:96]), reads=[PK[2]], writes=["modT"])
            S.op("dve", lambda e: e.tensor_scalar(out=tmpc[:], in0=modT[:, 16:32], scalar1=1.0, scalar2=None, op0=ALU.add),
                 reads=["modT"], writes=["tmpc"])
            S.op("dve", lambda e: e.tensor_tensor(out=G1[:], in0=tmpc[:], in1=ln1g[:], op=ALU.mult),
                 reads=["tmpc", "ln1g"], writes=["G1"])
            S.op("dve", lambda e: e.tensor_scalar(out=tmpc[:], in0=modT[:, 64:80], scalar1=1.0, scalar2=None, op0=ALU.add),
                 reads=["modT", "G1"], writes=["tmpc"])
            S.op("dve", lambda e: e.tensor_tensor(out=G2[:], in0=tmpc[:], in1=ln2g[:], op=ALU.mult),
                 reads=["tmpc", "ln2g"], writes=["G2"])
            S.emit()


        maskV = sb(ats, "maskV", [128, 512], BF16)
        maskS = sb(ats, "maskS", [128, 4, 512], BF16)
        maskI = sb(ats, "maskI", [128, 4, 512], BF16)
        kaT = sb(ats, "kaT", [128, SA], BF16)
        va = sb(ats, "va", [128, 32, 128], BF16)
        kiT = sb(ats, "kiT", [128, SA], BF16)
        widx = sb(ats, "widx", [128, 16, 16], F32)
        c2 = contextlib.ExitStack()
        cqT = sb(c2, "cqT", [128, 4, NOWN], BF16)

        evq = [0]

        def evac_scale(dst, src, s1, s2, reads, writes):
            evq[0] += 1
            if evq[0] % 2 == 0:
                if s2 is None:
                    S.op("dve", lambda e: e.tensor_scalar(out=dst, in0=src, scalar1=s1, scalar2=None, op0=ALU.mult),
                         reads=reads, writes=writes)
                else:
                    S.op("dve", lambda e: e.tensor_scalar(out=dst, in0=src, scalar1=s1, scalar2=s2, op0=ALU.mult,
                                                          op1=ALU.add), reads=reads, writes=writes)
            else:
                if s2 is None:
                    S.op("act", lambda e: e.activation(out=dst, in_=src, func=AF.Copy if isinstance(s1, float) else AF.Identity,
                                                       scale=s1), reads=reads, writes=writes)
                else:
                    S.op("act", lambda e: e.activation(out=dst, in_=src, func=AF.Identity, scale=s1, bias=s2),
                         reads=reads, writes=writes)

        def rstd_from(ss_ap, dim, key_in, key_out, out_ap):
            S.op("act", lambda e: e.activation(out=out_ap, in_=ss_ap, func=AF.Sqrt, scale=1.0 / dim, bias=EPS),
                 reads=[key_in], writes=[key_out])
            S.op("dve", lambda e: e.reciprocal(out=out_ap, in_=out_ap), reads=[key_out], writes=[key_out])

        psrot = [0]

        def nextps():
            i = psrot[0] % 8
            psrot[0] += 1
            return i

        if stop == 0:
            finish(S, [("modT", modT, [128, 96], F32), ("G1", G1, [128, KC], F32), ("G2", G2, [128, KC], F32),
                       ("g1_bc", g1_bc, [128, D], F32), ("g2_bc", g2_bc, [128, D], F32), ("ident", ident, [128, 128], F32),
                       ("negtri", negtri, [128, 128], BF16)])
            c2.close()
            ats.close()
            return nc
        with contextlib.ExitStack() as p1:
            hT = sb(p1, "hT", [128, KC, 1024], BF16)
            xs = sb(p1, "xs", [128, 4, D], F32)
            junk = sb(p1, "junk", [128, D], BF16)
            ssq = sb(p1, "ssq", [128, 8], F32)
            rstd = sb(p1, "rstd", [128, 8], F32)
            wbuf = [sb(p1, "wbuf%d" % i, [128, KC, 512], BF16) for i in range(2)]
            stg = [sb(p1, "stg%d" % i, [128, 512], BF16) for i in range(3)]
            stgv = [sb(p1, "stgv%d" % i, [128, 1024], BF16) for i in range(2)]
            tm32 = [sb(p1, "tm32_%d" % i, [128, 512], F32) for i in range(2)]
            sm = sb(p1, "sm", [128, 8], F32)
            onesf = sb(p1, "onesf", [128, 512], F32)
            S.op("pool", lambda e: e.memset(onesf[:], 1.0), writes=["onesf"])
            for i in range(4):
                S.op("pool", lambda e, i=i: e.affine_select(out=maskI[:, i, :], in_=onesf[:], pattern=[[1, 512]],
                                                            compare_op=ALU.is_ge, fill=0.0, base=-128 * i,
                                                            channel_multiplier=-1),
                     reads=["onesf"], writes=["maskI"])
                S.op("pool", lambda e, i=i: e.affine_select(out=maskS[:, i, :], in_=onesf[:], pattern=[[1, 512]],
                                                            compare_op=ALU.is_ge, fill=0.0, base=-128 * i - 1,
                                                            channel_multiplier=-1),
                     reads=["onesf"], writes=["maskS"])
            S.op("dve", lambda e: e.tensor_scalar(out=maskV[:], in0=onesf[:], scalar1=kvalid[:, 0:1], scalar2=None,
                                                  op0=ALU.mult), reads=["onesf", "kvalid"], writes=["maskV"])

            wload_i = [0]

            def load_w(col0, ncol, dup=False):
                i = wload_i[0] % 2
                wload_i[0] += 1
                wt, wk = wbuf[i], "wbuf%d" % i
                if dup:
                    def f(e, wt=wt):
                        a_ = e.dma_start(out=wt[:, :, 0:64], in_=w_in[:, col0:col0 + 64].rearrange("(c p) n -> p c n", p=128))
                        b_ = e.dma_start(out=wt[:, :, 64:128], in_=w_in[:, col0:col0 + 64].rearrange("(c p) n -> p c n", p=128))
                        return [a_, b_]
                    S.dma("pool", f, writes=[wk], n=2)
                else:
                    S.dma("pool", lambda e, wt=wt: e.dma_start(
                        out=wt[:, :, 0:ncol], in_=w_in[:, col0:col0 + ncol].rearrange("(c p) n -> p c n", p=128)), writes=[wk])
                return wt, wk

            def proj_tok(wt, wk, ncol, lb, pi):
                hk = "hT%d" % (lb // 4)

                def mm(e):
                    r = None
                    for kc in range(KC):
                        r = e.matmul(ps[pi][:, 0:ncol], hT[:, kc, lb * 128:(lb + 1) * 128], wt[:, kc, 0:ncol],
                                     start=(kc == 0), stop=(kc == KC - 1))
                    return r
                S.op("pe", mm, reads=[hk, wk], writes=[PK[pi]])

            def proj_feat(wt, wk, c0, hb, pi):
                def mm(e):
                    r = None
                    for kc in range(KC):
                        r = e.matmul(ps[pi][:, :], wt[:, kc, c0:c0 + 128], hT[:, kc, hb * 512:(hb + 1) * 512],
                                     start=(kc == 0), stop=(kc == KC - 1))
                    return r
                S.op("pe", mm, reads=["hT%d" % hb, wk], writes=[PK[pi]])

            def stage_xdma(at):
                S.dma("sp", lambda e: e.dma_start(
                    out=xs[:], in_=xk[at * 512:(at + 1) * 512, :].rearrange("(b p) n -> p b n", p=128)), writes=["xs"])

            def stage_xnorm():
                for b in range(4):
                    S.op("act", lambda e, b=b: e.activation(out=junk[:], in_=xs[:, b, :], func=AF.Square,
                                                            accum_out=ssq[:, b:b + 1]),
                         reads=["xs"], writes=["junk", "ssq"])
                rstd_from(ssq[:, 0:4], D, "ssq", "rstd", rstd[:, 0:4])
                for b in range(4):
                    S.op("dve", lambda e, b=b: e.tensor_scalar(
                        out=xs[:, b, :], in0=xs[:, b, :], scalar1=rstd[:, b:b + 1], scalar2=None, op0=ALU.mult),
                        reads=["rstd", "xs"], writes=["xs"])

            def stage_T(hb):
                for kc in range(KC):
                    pi = nextps()

                    def tr(e, kc=kc, pi=pi):
                        r = None
                        for b in range(4):
                            r = e.transpose(ps[pi][:, b * 128:(b + 1) * 128], xs[:, b, kc * 128:(kc + 1) * 128], ident[:])
                        return r
                    S.op("pe", tr, reads=["xs", "ident"], writes=[PK[pi]])
                    evac_scale(hT[:, kc, hb * 512:(hb + 1) * 512], ps[pi][:, :], G1[:, kc:kc + 1], modT[:, kc:kc + 1],
                               [PK[pi], "G1", "modT"], ["hT%d" % hb])

            def stage_P(at, hb, nxt):
                own = (at % 2 == 1)
                oj = at // 2
                if own:
                    wt, wk = load_w(0, 512)
                    for b in range(4):
                        lb = hb * 4 + b
                        tb = oj * 4 + b
                        pi = nextps()
                        proj_tok(wt, wk, 512, lb, pi)
                        t32, tk = tm32[tb % 2], "tm32_%d" % (tb % 2)
                        S.op("act", lambda e, pi=pi: e.activation(out=junk[:, 0:512], in_=ps[pi][:, :], func=AF.Square,
                                                                  accum_out=sm[:, 0:1]),
                             reads=[PK[pi]], writes=["junk", "sm"])
                        rstd_from(sm[:, 0:1], 512.0, "sm", "sm1", sm[:, 1:2])
                        S.op("dve", lambda e, pi=pi, t32=t32: e.tensor_scalar(out=t32[:], in0=ps[pi][:, :], scalar1=sm[:, 1:2],
                                                                             scalar2=None, op0=ALU.mult),
                             reads=[PK[pi], "sm1"], writes=[tk])
                        pj = nextps()

                        def tr(e, t32=t32, pj=pj):
                            r = None
                            for c4 in range(4):
                                r = e.transpose(ps[pj][:, c4 * 128:(c4 + 1) * 128], t32[:, c4 * 128:(c4 + 1) * 128], ident[:])
                            return r
                        S.op("pe", tr, reads=[tk, "ident"], writes=[PK[pj]])
                        for c4 in range(4):
                            evac_scale(cqT[:, c4, tb * 128:(tb + 1) * 128], ps[pj][:, c4 * 128:(c4 + 1) * 128],
                                       qng[:, c4:c4 + 1], None, [PK[pj], "qng"], ["cqT"])
                wt, wk = load_w(512, 336)
                for b in range(4):
                    lb = hb * 4 + b
                    tb = oj * 4 + b
                    ab = at * 4 + b
                    pi = nextps()
                    proj_tok(wt, wk, 336, lb, pi)
                    t32, tk = tm32[lb % 2], "tm32_%d" % (lb % 2)
                    S.op("act", lambda e, pi=pi: e.activation(out=junk[:, 0:128], in_=ps[pi][:, 0:128], func=AF.Square,
                                                              accum_out=sm[:, 2:3]),
                         reads=[PK[pi]], writes=["junk", "sm2"])
                    rstd_from(sm[:, 2:3], 128.0, "sm2", "sm3", sm[:, 3:4])
                    S.op("dve", lambda e, pi=pi, t32=t32: e.tensor_scalar(out=t32[:, 0:128], in0=ps[pi][:, 0:128], scalar1=sm[:, 3:4],
                                                                         scalar2=None, op0=ALU.mult),
                         reads=[PK[pi], "sm3"], writes=[tk])
                    S.op("act", lambda e, pi=pi, ab=ab: e.activation(out=va[:, ab, :], in_=ps[pi][:, 128:256], func=AF.Copy),
                         reads=[PK[pi]], writes=["va"])
                    if own:
                        S.op("dve", lambda e, pi=pi, tb=tb: e.tensor_scalar(out=widx[:, tb, :], in0=ps[pi][:, 320:336],
                                                                           scalar1=1.0 / 32.0, scalar2=None, op0=ALU.mult),
                             reads=[PK[pi]], writes=["widx"])
                    pj = nextps()
                    S.op("pe", lambda e, t32=t32, pj=pj: e.transpose(ps[pj][:, 0:128], t32[:, 0:128], ident[:]),
                         reads=[tk, "ident"], writes=[PK[pj]])
                    evac_scale(kaT[:, ab * 128:(ab + 1) * 128], ps[pj][:, 0:128], kgain[:, 0:1], None,
                               [PK[pj], "kgain"], ["kaT"])
                wt, wk = load_w(768, 64, dup=True)
                pi = nextps()
                proj_feat(wt, wk, 0, hb, pi)
                evac_scale(kiT[:, at * 512:(at + 1) * 512], ps[pi][:, :], 1.0, None, [PK[pi]], ["kiT"])
                if nxt is not None:
                    stage_xnorm()
                fm = ([("q", 848)] if own else []) + [("k", 1872)]
                for kind, col0 in fm:
                    for half in range(2):
                        wt, wk = load_w(col0 + half * 512, 512)
                        for hh in range(4):
                            hd = half * 4 + hh
                            pi = nextps()
                            proj_feat(wt, wk, hh * 128, hb, pi)
                            si = evq[0] % 3
                            st, sk = stg[si], "stg%d" % si
                            evac_scale(st[:], ps[pi][:, :], (float(HD) ** -0.5) if kind == "q" else 1.0, None, [PK[pi]], [sk])
                            if kind == "q":
                                S.dma("sp", lambda e, st=st, hd=hd: e.dma_start(
                                    out=qbT_s[hd, :, oj * 512:(oj + 1) * 512], in_=st[:]), reads=[sk], writes=["qbT_s"])
                            else:
                                S.dma("sp", lambda e, st=st, hd=hd: e.dma_start(
                                    out=kbT_s[hd, :, at * 512:(at + 1) * 512], in_=st[:]), reads=[sk], writes=["kbT_s"])
                for half in range(2):
                    wt, wk = load_w(2896 + half * 512, 512)
                    for b in range(4):
                        lb = hb * 4 + b
                        ab = at * 4 + b
                        pi = nextps()
                        proj_tok(wt, wk, 512, lb, pi)
                        si = evq[0] % 3
                        st, sk = stg[si], "stg%d" % si
                        evac_scale(st[:], ps[pi][:, :], 1.0, None, [PK[pi]], [sk])
                        S.dma("sp", lambda e, st=st, ab=ab, half=half: e.dma_start(
                            out=vb_s[ab * 128:(ab + 1) * 128, half * 512:(half + 1) * 512], in_=st[:]),
                            reads=[sk], writes=["vb_s"])

            tile_seq = [1, 3, 5, 7, 0, 2, 4, 6]
            stage_xdma(tile_seq[0])
            stage_xnorm()
            stage_T(0)
            for idx, at in enumerate(tile_seq):
                hb = idx % 2
                nxt = tile_seq[idx + 1] if idx + 1 < len(tile_seq) else None
                if nxt is not None:
                    stage_xdma(nxt)
                stage_P(at, hb, nxt)
                if nxt is not None:
                    stage_T(1 - hb)
            S.emit()

        if stop == 1:
            finish(S, [("kaT", kaT, [128, SA], BF16), ("va", va, [128, 32, 128], BF16), ("kiT", kiT, [128, SA], BF16),
                       ("widx", widx, [128, 16, 16], F32), ("cqT", cqT, [128, 4, NOWN], BF16), ("maskS", maskS, [128, 4, 512], BF16),
                       ("maskI", maskI, [128, 4, 512], BF16), ("maskV", maskV, [128, 512], BF16)])
            c2.close()
            ats.close()
            return nc
        with contextlib.ExitStack() as p2:
            wqu = sb(p2, "wqu", [128, 4, 2048], BF16)
            qa32 = sb(p2, "qa32", [128, 4, 1024], F32)
            sqb = [sb(p2, "sqb%d" % i, [128, 512], BF16) for i in range(2)]
            rsb = [sb(p2, "rsb%d" % i, [128, 512], F32) for i in range(2)]
            qsc = sb(p2, "qsc", [128, 1], F32)
            stg2 = [sb(p2, "stg2_%d" % i, [128, 512], BF16) for i in range(3)]
            S.op("dve", lambda e: e.tensor_scalar(out=qsc[:], in0=qgain[:], scalar1=float(HD) ** -0.5, scalar2=None,
                                                  op0=ALU.mult), reads=["qgain"], writes=["qsc"])

            def ldq(e):
                return [e.dma_start(out=wqu[:, :, hf * 1024:(hf + 1) * 1024],
                                    in_=w_qup[:, hf * 1024:(hf + 1) * 1024].rearrange("(c p) n -> p c n", p=128)) for hf in range(2)]
            S.dma("pool", ldq, writes=["wqu"], n=2)
            q2 = [0]
            for li in range(4):
                for b in range(4):
                    tb = li * 4 + b
                    for half in range(2):
                        pi = nextps()

                        def mm(e, tb=tb, half=half, pi=pi):
                            r = None
                            for kc in range(4):
                                r = e.matmul(ps[pi][:, :], cqT[:, kc, tb * 128:(tb + 1) * 128],
                                             wqu[:, kc, half * 512:(half + 1) * 512], start=(kc == 0), stop=(kc == 3))
                            return r
                        S.op("pe", mm, reads=["cqT", "wqu"], writes=[PK[pi]])
                        if (b + half) % 2 == 0:
                            S.op("dve", lambda e, pi=pi, b=b, half=half: e.tensor_copy(
                                out=qa32[:, b, half * 512:(half + 1) * 512], in_=ps[pi][:, :]), reads=[PK[pi]], writes=["qa32"])
                        else:
                            S.op("act", lambda e, pi=pi, b=b, half=half: e.activation(
                                out=qa32[:, b, half * 512:(half + 1) * 512], in_=ps[pi][:, :], func=AF.Copy), reads=[PK[pi]], writes=["qa32"])
                for hd in range(8):
                    pi = nextps()
                    k2 = q2[0] % 2
                    q2[0] += 1

                    def tr(e, hd=hd, pi=pi):
                        r = None
                        for b in range(4):
                            r = e.transpose(ps[pi][:, b * 128:(b + 1) * 128], qa32[:, b, hd * 128:(hd + 1) * 128], ident[:])
                        return r
                    S.op("pe", tr, reads=["qa32", "ident"], writes=[PK[pi]])
                    S.op("act", lambda e, pi=pi, k2=k2: e.activation(out=sqb[k2][:], in_=ps[pi][:, :], func=AF.Square),
                         reads=[PK[pi]], writes=["sqb%d" % k2])
                    pj = nextps()
                    S.op("pe", lambda e, pj=pj, k2=k2: e.matmul(ps[pj][:, :], onesb[:], sqb[k2][:], start=True, stop=True),
                         reads=["sqb%d" % k2, "onesb"], writes=[PK[pj]])
                    S.op("act", lambda e, pj=pj, k2=k2: e.activation(out=rsb[k2][:], in_=ps[pj][:, :], func=AF.Sqrt, scale=1.0 / 128.0, bias=EPS),
                         reads=[PK[pj]], writes=["rsb%d" % k2])
                    S.op("dve", lambda e, k2=k2: e.reciprocal(out=rsb[k2][:], in_=rsb[k2][:]), reads=["rsb%d" % k2], writes=["rsb%d" % k2])
                    si = q2[0] % 3
                    st, sk = stg2[si], "stg2_%d" % si
                    S.op("dve", lambda e, pi=pi, k2=k2, st=st: e.scalar_tensor_tensor(out=st[:], in0=ps[pi][:, :], scalar=qsc[:, 0:1],
                                                                                    in1=rsb[k2][:], op0=ALU.mult, op1=ALU.mult),
                         reads=[PK[pi], "qsc", "rsb%d" % k2], writes=[sk])
                    S.dma("sp", lambda e, st=st, hd=hd, li=li: e.dma_start(out=qaT_s[li, :, hd, :], in_=st[:]),
                          reads=[sk], writes=["qaT_s"])
                for pr in range(8):
                    pi = nextps()

                    def mm(e, pr=pr, li=li, pi=pi):
                        r = None
                        for kc in range(4):
                            r = e.matmul(ps[pi][:, :], wqu[:, kc, 1024 + pr * 128: 1024 + (pr + 1) * 128],
                                         cqT[:, kc, li * 512:(li + 1) * 512], start=(kc == 0), stop=(kc == 3))
                        return r
                    S.op("pe", mm, reads=["cqT", "wqu"], writes=[PK[pi]])
                    q2[0] += 1
                    si = q2[0] % 3
                    st, sk = stg2[si], "stg2_%d" % si
                    evac_scale(st[:], ps[pi][:, :], 1.0, None, [PK[pi]], [sk])
                    S.dma("sp", lambda e, st=st, pr=pr, li=li: e.dma_start(out=qiT_s[li, :, pr, :], in_=st[:]),
                          reads=[sk], writes=["qiT_s"])
            S.emit()
        c2.close()
        if stop == 2:
            finish(S, [])
            ats.close()
            return nc
        oaT = sb(ats, "oaT", [128, 8, NOWN], BF16)

        with contextlib.ExitStack() as p3:
            IT = sb(p3, "IT", [128, 32, 512], BF16)
            Wh = sb(p3, "Wh", [128, 8, 1024], BF16)
            tb32 = [sb(p3, "tb32_%d" % i, [128, 1024], F32) for i in range(1)]
            rb31 = sb(p3, "rb31_t", [128, 8], F32)
            nrb31 = sb(p3, "nrb31", [128, 8], F32)
            Wdiag = sb(p3, "Wdiag", [128, 4, 16, 128], BF16)
            qi = [sb(p3, "qi%d" % i, [128, 8, 512], BF16) for i in range(1)]
            qa = [sb(p3, "qa%d" % i, [128, 8, 512], BF16) for i in range(1)]
            rl = [sb(p3, "rl%d" % i, [128, 512], BF16) for i in range(4)]
            pt = [sb(p3, "pt%d" % i, [128, 512], BF16) for i in range(4)]
            pm = [sb(p3, "pm%d" % i, [128, 512], BF16) for i in range(4)]
            cmpb = [sb(p3, "cmp%d" % i, [128, 512], BF16) for i in range(4)]
            lo = sb(p3, "lo", [128, 512], F32)
            stp = sb(p3, "stp", [128, 512], F32)
            midb = sb(p3, "midb", [128, 512], BF16)
            rden = sb(p3, "rden", [128, 512], F32)
            penV = sb(p3, "penV", [128, 512], BF16)
            penI = sb(p3, "penI", [128, 4, 512], BF16)

            S.dma("sp", lambda e: e.dma_start(out=rb31[:], in_=rb31_d), writes=["rb31"])
            S.op("dve", lambda e: e.tensor_scalar(out=nrb31[:], in0=rb31[:], scalar1=-1.0, scalar2=None, op0=ALU.mult),
                 reads=["rb31"], writes=["nrb31"])
            S.op("dve", lambda e: e.tensor_scalar(out=penV[:], in0=maskV[:], scalar1=-1.0, scalar2=BIG, op0=ALU.add, op1=ALU.mult),
                 reads=["maskV"], writes=["penV"])
            for i in range(4):
                S.op("dve", lambda e, i=i: e.tensor_scalar(out=penI[:, i, :], in0=maskI[:, i, :], scalar1=-1.0, scalar2=BIG,
                                                           op0=ALU.add, op1=ALU.mult), reads=["maskI"], writes=["penI"])
            for h in range(8):
                tt_, tk = tb32[0], "tb32_0"
                S.dma("sp", lambda e, tt_=tt_, h=h: e.dma_start(out=tt_[:], in_=tb_d[h]), writes=[tk])
                S.op("act", lambda e, tt_=tt_, h=h: e.activation(out=Wh[:, h, :], in_=tt_[:], func=AF.Exp, bias=nrb31[:, h:h + 1]),
                     reads=[tk, "nrb31"], writes=["Wh"])
                S.op("pool", lambda e, h=h: e.affine_select(out=Wh[:, h, :], in_=Wh[:, h, :], pattern=[[1, 1024]],
                                                            compare_op=ALU.is_ge, fill=0.0, base=-384, channel_multiplier=-1),
                     reads=["Wh"], writes=["Wh"])

            W0 = 16.0
            rot = [0]
            SCB = [0, 1, 7]

            def dsa_tile(j):
                nkt = 2 * j + 2
                nkb = 4 * nkt
                qit, qik = qi[0], "qi0"
                qat, qak = qa[0], "qa0"
                S.dma("sp", lambda e: e.dma_start(out=qit[:], in_=qiT_s[j]), reads=["qiT_s"], writes=[qik])
                S.dma("sp", lambda e: e.dma_start(out=qat[:], in_=qaT_s[j]), reads=["qaT_s"], writes=[qak])
                for qb in range(4):
                    for h in range(16):
                        S.op("dve", lambda e, qb=qb, h=h: e.tensor_scalar(
                            out=Wdiag[:, qb, h, :], in0=identb[:], scalar1=widx[:, j * 4 + qb, h:h + 1], scalar2=None, op0=ALU.mult),
                            reads=["identb", "widx"], writes=["Wdiag"])
                its = [(kt, qb, h) for kt in range(nkt) for qb in range(4) for h in range(16)]
                stt = {}

                def stageSC(i):
                    kt, qb, h = its[i]
                    g = rot[0]
                    rot[0] += 1
                    scb, rli = SCB[g % 3], g % 4
                    p0_ = 64 * (h % 2)
                    stt[i] = (scb, rli)
                    S.op("pe", lambda e: e.matmul(ps[scb][:, :], qit[p0_:p0_ + 64, h // 2, qb * 128:(qb + 1) * 128],
                                                  kiT[p0_:p0_ + 64, kt * 512:(kt + 1) * 512], start=True, stop=True),
                         reads=[qik, "kiT"], writes=[PK[scb]])
                    rlt, rlk = rl[rli], "rl%d" % rli
                    if g % 3 != 2:
                        S.op("act", lambda e: e.activation(out=rlt[:], in_=ps[scb][:, :], func=AF.Relu), reads=[PK[scb]], writes=[rlk])
                    else:
                        S.op("dve", lambda e: e.tensor_scalar(out=rlt[:], in0=ps[scb][:, :], scalar1=0.0, scalar2=None, op0=ALU.max),
                             reads=[PK[scb]], writes=[rlk])

                def stageACC(i):
                    kt, qb, h = its[i]
                    scb, rli = stt[i]
                    rlt, rlk = rl[rli], "rl%d" % rli

                    def acc(e):
                        r = None
                        for sbk in range(4):
                            r = e.matmul(ps[2 + sbk][:, qb * 128:(qb + 1) * 128], rlt[:, sbk * 128:(sbk + 1) * 128],
                                         Wdiag[:, qb, h, :], start=(h == 0), stop=(h == 15))
                        return r
                    S.op("pe", acc, reads=[rlk, "Wdiag"], writes=[PK[2], PK[3], PK[4], PK[5]])
                    if qb == 3 and h == 15:
                        for sbk in range(4):
                            kb = kt * 4 + sbk
                            if kt == 0 or kt == nkt - 1:
                                mk = maskV[:] if kt == 0 else maskI[:, sbk, :]
                                pn = penV[:] if kt == 0 else penI[:, sbk, :]
                                S.op("dve", lambda e, kb=kb, sbk=sbk, mk=mk: e.tensor_tensor(out=IT[:, kb, :], in0=ps[2 + sbk][:, :], in1=mk,
                                                                                              op=ALU.mult),
                                     reads=[PK[2 + sbk], "maskV", "maskI"], writes=["IT"])
                                S.op("dve", lambda e, kb=kb, pn=pn: e.tensor_tensor(out=IT[:, kb, :], in0=IT[:, kb, :], in1=pn, op=ALU.add),
                                     reads=["IT", "penV", "penI"], writes=["IT"])
                            else:
                                S.op("act", lambda e, kb=kb, sbk=sbk: e.activation(out=IT[:, kb, :], in_=ps[2 + sbk][:, :], func=AF.Copy),
                                     reads=[PK[2 + sbk]], writes=["IT"])
                for i in range(len(its) + 1):
                    if i < len(its):
                        stageSC(i)
                    if i >= 1:
                        stageACC(i - 1)
                S.op("pool", lambda e: e.memset(lo[:], -W0), writes=["lo"])
                for it in range(NBIS + 1):
                    hk = W0 / (2.0 ** it)
                    final = (it == NBIS)
                    S.op("dve", lambda e, hk=hk, final=final: e.tensor_scalar(out=midb[:], in0=lo[:], scalar1=(0.0 if final else hk),
                                                                              scalar2=None, op0=ALU.add),
                         reads=["lo"], writes=["midb"])
                    if final:
                        for kb in range(nkb):
                            S.op("dve", lambda e, kb=kb: e.tensor_tensor(out=IT[:, kb, :], in0=IT[:, kb, :], in1=midb[:], op=ALU.is_ge),
                                 reads=["IT", "midb"], writes=["IT"])
                        break
                    for kb in range(nkb):
                        ci = kb % 4
                        S.op("dve", lambda e, kb=kb, ci=ci: e.tensor_tensor(out=cmpb[ci][:], in0=IT[:, kb, :], in1=midb[:], op=ALU.is_ge),
                             reads=["IT", "midb"], writes=["cmp%d" % ci])
                        S.op("pe", lambda e, kb=kb, ci=ci: e.matmul(ps[6][:, :], onesb[:], cmpb[ci][:], start=(kb == 0), stop=(kb == nkb - 1)),
                             reads=["cmp%d" % ci, "onesb"], writes=[PK[6]])
                    S.op("dve", lambda e, hk=hk: e.tensor_scalar(out=stp[:], in0=ps[6][:, :], scalar1=TOPK - 0.5, scalar2=hk,
                                                                 op0=ALU.is_ge, op1=ALU.mult), reads=[PK[6]], writes=["stp"])
                    S.op("dve", lambda e: e.tensor_tensor(out=lo[:], in0=lo[:], in1=stp[:], op=ALU.add), reads=["lo", "stp"], writes=["lo"])
                for h in range(8):
                    dsa_head(j, h, nkb, qat, qak)

            def dsa_head(j, h, nkb, qat, qak):
                ob, db = 2 + 2 * (h % 2), 3 + 2 * (h % 2)
                sst = {}

                def stageLG(kb):
                    g = rot[0]
                    rot[0] += 1
                    lgb, pi_ = SCB[g % 3], g % 4
                    sst[kb] = pi_
                    S.op("pe", lambda e: e.matmul(ps[lgb][:, :], kaT[:, kb * 128:(kb + 1) * 128], qat[:, h, :], start=True, stop=True),
                         reads=["kaT", qak], writes=[PK[lgb]])
                    S.op("act", lambda e: e.activation(out=pt[pi_][:], in_=ps[lgb][:, :], func=AF.Exp), reads=[PK[lgb]], writes=["pt%d" % pi_])
                    S.op("dve", lambda e: e.tensor_tensor(out=pm[pi_][:], in0=pt[pi_][:], in1=IT[:, kb, :], op=ALU.mult),
                         reads=["pt%d" % pi_, "IT"], writes=["pm%d" % pi_])
                    r_ = kb - (nkb - 4)
                    if r_ >= -1:
                        u0 = 384 - 128 * r_
                        S.op("dve", lambda e: e.tensor_tensor(out=pm[pi_][:], in0=pm[pi_][:], in1=Wh[:, h, u0:u0 + 512], op=ALU.mult),
                             reads=["pm%d" % pi_, "Wh"], writes=["pm%d" % pi_])

                def stagePV(kb):
                    pi_ = sst[kb]

                    def pv(e):
                        e.matmul(ps[ob][:, :], va[:, kb, :], pm[pi_][:], start=(kb == 0), stop=(kb == nkb - 1))
                        return e.matmul(ps[db][:, :], onesb[:], pm[pi_][:], start=(kb == 0), stop=(kb == nkb - 1))
                    S.op("pe", pv, reads=["va", "pm%d" % pi_, "onesb"], writes=[PK[ob], PK[db]])
                for step in range(nkb + 2):
                    if step < nkb:
                        stageLG(step)
                    if 0 <= step - 2 < nkb:
                        stagePV(step - 2)
                S.op("dve", lambda e: e.reciprocal(out=rden[:], in_=ps[db][:, :]), reads=[PK[db]], writes=["rden"])
                S.op("dve", lambda e: e.tensor_tensor(out=oaT[:, h, j * 512:(j + 1) * 512], in0=ps[ob][:, :], in1=rden[:], op=ALU.mult),
                     reads=[PK[ob], "rden"], writes=["oaT"])

            for j in range(4):
                dsa_tile(j)
            S.emit()

        if stop == 3:
            finish(S, [("oaT", oaT, [128, 8, NOWN], BF16)])
            ats.close()
            return nc
        obT = sb(ats, "obT", [128, 8, NOWN], BF16)

        def oTc(kc, sl):
            return (oaT if kc < 8 else obT)[:, kc % 8, sl]

        with contextlib.ExitStack() as p4:
            kbt = [sb(p4, "kbt%d" % i, [128, SA], BF16) for i in range(2)]
            vbt = [sb(p4, "vbt%d" % i, [128, 32, 128], BF16) for i in range(2)]
            qbt = [sb(p4, "qbt%d" % i, [128, NOWN], BF16) for i in range(2)]
            NZ, NL, NC_, NA = 2, 4, 3, 4
            ez = [sb(p4, "ez%d" % i, [128, 512], F32) for i in range(NZ)]
            Lp = [sb(p4, "Lp%d" % i, [128, 512], BF16) for i in range(NL)]
            Cc = [sb(p4, "Cc%d" % i, [128, 512], BF16) for i in range(NC_)]
            at_ = [sb(p4, "at%d" % i, [128, 512], BF16) for i in range(NA)]
            zero_c = sb(p4, "zero_c", [128, 512], BF16)
            S.op("pool", lambda e: e.memset(zero_c[:], 0.0), writes=["zero_c"])
            gi = [0]
            cci = [0]
            for h in range(8):
                b_ = h % 2
                S.dma("sp", lambda e, b_=b_, h=h: e.dma_start(out=kbt[b_][:], in_=kbT_s[h]), reads=["kbT_s"], writes=["kbt%d" % b_])
                S.dma("sp", lambda e, b_=b_, h=h: e.dma_start(out=qbt[b_][:], in_=qbT_s[h]), reads=["qbT_s"], writes=["qbt%d" % b_])
                S.dma("sp", lambda e, b_=b_, h=h: e.dma_start(
                    out=vbt[b_][:], in_=vb_s[:, h * 128:(h + 1) * 128].rearrange("(b p) d -> p b d", p=128)),
                    reads=["vb_s"], writes=["vbt%d" % b_])
                kk, qk, vk = "kbt%d" % b_, "qbt%d" % b_, "vbt%d" % b_
                def sb_tile(h, j, b_, kk, qk, vk):
                    nkb = 8 * (j + 1)
                    obk = 4 + (h * 4 + j) % 2
                    order = list(reversed(range(nkb)))
                    n = len(order)
                    qslc = qbt[b_][:, j * 512:(j + 1) * 512]
                    st = {}

                    def maskof(kb):
                        if kb < 4:
                            return maskV[:]
                        if kb >= nkb - 4:
                            return maskS[:, kb - (nkb - 4), :]
                        return None

                    def stageZ(i):
                        kb = order[i]
                        g = gi[0]
                        gi[0] += 1
                        zb, li_ = g % NZ, g % NL
                        kslc = kbt[b_][:, kb * 128:(kb + 1) * 128]
                        st[i] = dict(kb=kb, zb=zb, li=li_, ai=g % NA, eb=2 + g % 2, kslc=kslc, mk=maskof(kb))
                        S.op("pe", lambda e: e.matmul(ps[zb][:, :], kslc, qslc, start=True, stop=True), reads=[kk, qk], writes=[PK[zb]])
                        S.op("act", lambda e: e.activation(out=ez[zb][:], in_=ps[zb][:, :], func=AF.Exp), reads=[PK[zb]], writes=["ez%d" % zb])
                        S.op("act", lambda e: e.activation(out=Lp[li_][:], in_=ez[zb][:], func=AF.Ln, bias=1.0),
                             reads=["ez%d" % zb], writes=["Lp%d" % li_])
                        mk = st[i]["mk"]
                        if mk is not None:
                            S.op("dve", lambda e: e.tensor_tensor(out=Lp[li_][:], in0=Lp[li_][:], in1=mk, op=ALU.mult),
                                 reads=["Lp%d" % li_, "maskV", "maskS"], writes=["Lp%d" % li_])
                        if i == 0:
                            st[i]["cc"] = None
                        else:
                            prev = st[i - 1]
                            c_new = cci[0] % NC_
                            cci[0] += 1
                            pl = prev["li"]
                            if prev["cc"] is None:
                                S.op("dve", lambda e: e.tensor_copy(out=Cc[c_new][:], in_=Lp[pl][:]), reads=["Lp%d" % pl], writes=["Cc%d" % c_new])
                            else:
                                pc = prev["cc"]
                                S.op("dve", lambda e: e.tensor_tensor(out=Cc[c_new][:], in0=Cc[pc][:], in1=Lp[pl][:], op=ALU.add),
                                     reads=["Cc%d" % pc, "Lp%d" % pl], writes=["Cc%d" % c_new])
                            st[i]["cc"] = c_new

                    def stageE(i):
                        s_ = st[i]
                        eb, kslc, li_, cc, ai = s_["eb"], s_["kslc"], s_["li"], s_["cc"], s_["ai"]
                        ccap = zero_c[:] if cc is None else Cc[cc][:]
                        cck = "zero_c" if cc is None else "Cc%d" % cc

                        def em(e):
                            e.matmul(ps[eb][:, :], kslc, qslc, start=True, stop=False)
                            e.matmul(ps[eb][:, :], negtri[:], Lp[li_][:], start=False, stop=False)
                            return e.matmul(ps[eb][:, :], negones[:], ccap, start=False, stop=True)
                        S.op("pe", em, reads=[kk, qk, "negtri", "negones", "Lp%d" % li_, cck], writes=[PK[eb]])
                        S.op("act", lambda e: e.activation(out=at_[ai][:], in_=ps[eb][:, :], func=AF.Exp), reads=[PK[eb]], writes=["at%d" % ai])
                        mk = s_["mk"]
                        if mk is not None:
                            S.op("dve", lambda e: e.tensor_tensor(out=at_[ai][:], in0=at_[ai][:], in1=mk, op=ALU.mult),
                                 reads=["at%d" % ai, "maskV", "maskS"], writes=["at%d" % ai])

                    def stageV(i):
                        s_ = st[i]
                        kb, ai = s_["kb"], s_["ai"]
                        S.op("pe", lambda e: e.matmul(ps[obk][:, :], vbt[b_][:, kb, :], at_[ai][:], start=(i == 0), stop=(i == n - 1)),
                             reads=[vk, "at%d" % ai], writes=[PK[obk]])

                    for step in range(n + 2):
                        if step < n:
                            stageZ(step)
                        if 0 <= step - 1 < n:
                            stageE(step - 1)
                        if 0 <= step - 2 < n:
                            stageV(step - 2)
                    S.op("act", lambda e, obk=obk, h=h, j=j: e.activation(out=obT[:, h, j * 512:(j + 1) * 512], in_=ps[obk][:, :],
                                                                         func=AF.Copy), reads=[PK[obk]], writes=["obT"])
                for j in range(4):
                    sb_tile(h, j, b_, kk, qk, vk)
            S.emit()
        if stop == 4:
            finish(S, [("oaT", oaT, [128, 8, NOWN], BF16), ("obT", obT, [128, 8, NOWN], BF16)])
            ats.close()
            return nc
        with contextlib.ExitStack() as p5:
            wo = sb(p5, "wo", [128, KC, D], BF16)
            sq = [sb(p5, "sq%d" % i, [128, 512], BF16) for i in range(2)]
            rs = sb(p5, "rs", [128, 512], F32)
            xs5 = [sb(p5, "xs5_%d" % i, [128, D], F32) for i in range(1)]
            t5 = [sb(p5, "t5_%d" % i, [128, D], F32) for i in range(1)]
            for cg in range(4):
                S.dma("pool", lambda e, cg=cg: e.dma_start(out=wo[:, :, cg * 512:(cg + 1) * 512],
                                                           in_=w_out[:, cg * 512:(cg + 1) * 512].rearrange("(c p) n -> p c n", p=128)),
                      writes=["wo"])
            r5 = [0]
            for j in range(4):
                for g in range(2):
                    gn = gna if g == 0 else gnb
                    gk = "gna" if g == 0 else "gnb"
                    pb = r5[0] % 2
                    r5[0] += 1
                    for c_ in range(8):
                        si = c_ % 2
                        S.op("act", lambda e, si=si, g=g, c_=c_, j=j: e.activation(out=sq[si][:], in_=oTc(g * 8 + c_, slice(j * 512, (j + 1) * 512)),
                                                                                 func=AF.Square), reads=["oaT", "obT"], writes=["sq%d" % si])
                        S.op("pe", lambda e, pb=pb, si=si, c_=c_: e.matmul(ps[pb][:, :], onesb[:], sq[si][:], start=(c_ == 0), stop=(c_ == 7)),
                             reads=["sq%d" % si, "onesb"], writes=[PK[pb]])
                    S.op("act", lambda e, pb=pb: e.activation(out=rs[:], in_=ps[pb][:, :], func=AF.Sqrt, scale=1.0 / 1024.0, bias=EPS),
                         reads=[PK[pb]], writes=["rs"])
                    S.op("dve", lambda e: e.reciprocal(out=rs[:], in_=rs[:]), reads=["rs"], writes=["rs"])
                    for c_ in range(8):
                        S.op("dve", lambda e, g=g, c_=c_, j=j, gn=gn: e.scalar_tensor_tensor(
                            out=oTc(g * 8 + c_, slice(j * 512, (j + 1) * 512)), in0=oTc(g * 8 + c_, slice(j * 512, (j + 1) * 512)),
                            scalar=gn[:, c_:c_ + 1], in1=rs[:], op0=ALU.mult, op1=ALU.mult),
                            reads=["oaT", "obT", "rs", gk], writes=["oaT" if g == 0 else "obT"])
            for tb in range(16):
                at = 2 * (tb // 4) + 1
                r0 = at * 512 + (tb % 4) * 128
                xb, xbk = xs5[0], "xs5_0"
                tt_, ttk = t5[0], "t5_0"
                S.dma("sp", lambda e, xb=xb, r0=r0: e.dma_start(out=xb[:], in_=xk[r0:r0 + 128, :]), writes=[xbk])
                for cg in range(4):
                    pb = 2 + r5[0] % 4
                    r5[0] += 1

                    def mm(e, pb=pb, tb=tb, cg=cg):
                        r = None
                        for kc in range(KC):
                            r = e.matmul(ps[pb][:, :], oTc(kc, slice(tb * 128, (tb + 1) * 128)), wo[:, kc, cg * 512:(cg + 1) * 512],
                                         start=(kc == 0), stop=(kc == KC - 1))
                        return r
                    S.op("pe", mm, reads=["oaT", "obT", "wo"], writes=[PK[pb]])
                    S.op("dve", lambda e, pb=pb, tt_=tt_, cg=cg: e.tensor_tensor(out=tt_[:, cg * 512:(cg + 1) * 512], in0=ps[pb][:, :],
                                                                                in1=g1_bc[:, cg * 512:(cg + 1) * 512], op=ALU.mult),
                         reads=[PK[pb], "g1_bc"], writes=[ttk])
                S.op("pool", lambda e, tt_=tt_, xb=xb: e.tensor_tensor(out=tt_[:], in0=tt_[:], in1=xb[:], op=ALU.add),
                     reads=[ttk, xbk], writes=[ttk])
                S.dma("sp", lambda e, tt_=tt_, tb=tb: e.dma_start(out=x1_s[tb * 128:(tb + 1) * 128, :], in_=tt_[:]),
                      reads=[ttk], writes=["x1_s"])
            S.emit()
        if stop == 5:
            finish(S, [("oaT", oaT, [128, 8, NOWN], BF16), ("obT", obT, [128, 8, NOWN], BF16)])
            ats.close()
            return nc
        ats.close()

        with contextlib.ExitStack() as p6:
            x1t = sb(p6, "x1t", [128, 4, D], F32)
            h2T = sb(p6, "h2T", [128, KC, 512], BF16)
            acc = sb(p6, "acc", [128, 4, D], F32)
            h2T32 = acc[:].rearrange("p b (q n) -> p (b q) n", q=4)
            rt = sb(p6, "rt", [128, KC, 36], F32)
            wg = [sb(p6, "wg%d" % i, [128, KC, DE], BF16) for i in range(2)]
            wu = [sb(p6, "wu%d" % i, [128, KC, DE], BF16) for i in range(2)]
            wd = [sb(p6, "wd%d" % i, [128, 4, D], BF16) for i in range(2)]
            sg = [sb(p6, "sg%d" % i, [128, 512], BF16) for i in range(2)]
            uT = [sb(p6, "uT%d" % i, [128, 4, 512], BF16) for i in range(2)]
            rsm = sb(p6, "rsm", [128, 16], F32)
            lgB = sb(p6, "lgB", [128, 4, 36], F32)
            lemB = sb(p6, "lemB", [128, 4, 32], F32)
            m8B = sb(p6, "m8B", [128, 4, 32], F32)
            rsmB = sb(p6, "rsmB", [128, 4, 16], F32)
            import os
            P6CUT = int(os.environ.get("P6CUT", "9"))
            is1B = sb(p6, "is1B", [128, 4, 32], F32)
            is2B = sb(p6, "is2B", [128, 4, 32], F32)
            Gt = sb(p6, "Gt", [128, 4, 32], F32)
            S.dma("sp", lambda e: e.dma_start(out=rt[:], in_=router_d.rearrange("(c p) n -> p c n", p=128)), writes=["rt"])
            r6 = [0]
            ei = [0]
            def route_block(b):
                ops = []
                lg, rr = lgB[:, b, :], rsmB[:, b, :]
                i1, i2, lm, mm8 = is1B[:, b, :], is2B[:, b, :], lemB[:, b, :], m8B[:, b, :]

                def kx(keys):
                    return [k if k == "Gt" else "%s_%d" % (k, b) for k in keys]

                def dv(fn, reads, writes):
                    ops.append(("dve", fn, kx(reads), kx(writes)))

                def ac(fn, reads, writes):
                    ops.append(("act", fn, kx(reads), kx(writes)))
                TT = lambda o, a_, b_, op: (lambda e: e.tensor_tensor(out=o, in0=a_, in1=b_, op=op))
                dv(TT(rr[:, 0:2], lg[:, 0:2], lg[:, 2:4], ALU.max), ["lg"], ["rA"])
                dv(TT(rr[:, 8:9], rr[:, 0:1], rr[:, 1:2], ALU.max), ["rA"], ["r8"])
                dv(lambda e: e.tensor_scalar(out=rr[:, 9:10], in0=rr[:, 8:9], scalar1=-1.0, scalar2=None, op0=ALU.mult), ["r8"], ["r9"])
                ac(lambda e: e.activation(out=i1[:, 0:4], in_=lg[:, 0:4], func=AF.Exp, bias=rr[:, 9:10]), ["lg", "r9"], ["is1"])
                dv(TT(rr[:, 0:2], i1[:, 0:2], i1[:, 2:4], ALU.add), ["is1", "rA"], ["rA"])
                dv(TT(rr[:, 10:11], rr[:, 0:1], rr[:, 1:2], ALU.add), ["rA"], ["r10"])
                dv(lambda e: e.reciprocal(out=rr[:, 11:12], in_=rr[:, 10:11]), ["r10"], ["r11"])
                dv(lambda e: e.tensor_scalar(out=i2[:, 0:4], in0=lg[:, 0:4], scalar1=rr[:, 8:9], scalar2=None, op0=ALU.is_ge),
                   ["lg", "r8"], ["is2"])
                dv(lambda e: e.tensor_scalar(out=i2[:, 0:4], in0=i2[:, 0:4], scalar1=-1.0, scalar2=BIG, op0=ALU.add, op1=ALU.mult),
                   ["is2"], ["is2"])
                for g in range(4):
                    dv(lambda e, g=g: e.tensor_scalar(out=lm[:, g * 8:(g + 1) * 8], in0=lg[:, 4 + g * 8:12 + g * 8],
                                                      scalar1=i2[:, g:g + 1], scalar2=None, op0=ALU.add), ["lg", "is2"], ["lem"])

                def tree_max(outcol, okey):
                    dv(TT(mm8[:, 0:16], lm[:, 0:16], lm[:, 16:32], ALU.max), ["lem", "m8"], ["m8"])
                    dv(TT(mm8[:, 16:24], mm8[:, 0:8], mm8[:, 8:16], ALU.max), ["m8"], ["m8"])
                    dv(TT(mm8[:, 24:28], mm8[:, 16:20], mm8[:, 20:24], ALU.max), ["m8"], ["m8"])
                    dv(TT(mm8[:, 28:30], mm8[:, 24:26], mm8[:, 26:28], ALU.max), ["m8"], ["m8"])
                    dv(TT(outcol, mm8[:, 28:29], mm8[:, 29:30], ALU.max), ["m8"], [okey])
                tree_max(rr[:, 2:3], "l1")
                dv(lambda e: e.tensor_scalar(out=i1, in0=lm, scalar1=rr[:, 2:3], scalar2=None, op0=ALU.is_ge), ["lem", "l1"], ["is1"])
                dv(lambda e: e.scalar_tensor_tensor(out=lm, in0=i1, scalar=-BIG, in1=lm, op0=ALU.mult, op1=ALU.add),
                   ["is1", "lem"], ["lem"])
                tree_max(rr[:, 3:4], "l2")
                dv(lambda e: e.tensor_scalar(out=i2, in0=lm, scalar1=rr[:, 3:4], scalar2=None, op0=ALU.is_ge), ["lem", "l2"], ["is2"])
                dv(lambda e: e.tensor_scalar(out=rr[:, 12:13], in0=rr[:, 2:3], scalar1=-1.0, scalar2=None, op0=ALU.mult), ["l1"], ["r12"])
                ac(lambda e: e.activation(out=rr[:, 13:14], in_=rr[:, 3:4], func=AF.Exp, bias=rr[:, 12:13]), ["l2", "r12"], ["r13"])
                dv(lambda e: e.tensor_scalar(out=rr[:, 14:15], in0=rr[:, 13:14], scalar1=1.0, scalar2=None, op0=ALU.add), ["r13"], ["r14"])
                dv(lambda e: e.reciprocal(out=rr[:, 14:15], in_=rr[:, 14:15]), ["r14"], ["r14"])
                dv(TT(rr[:, 14:15], rr[:, 14:15], rr[:, 11:12], ALU.mult), ["r14", "r11"], ["r14"])
                dv(TT(rr[:, 15:16], rr[:, 14:15], rr[:, 13:14], ALU.mult), ["r14", "r13"], ["r15"])
                dv(lambda e: e.tensor_scalar(out=Gt[:, b, :], in0=i1, scalar1=rr[:, 14:15], scalar2=None, op0=ALU.mult), ["is1", "r14"], ["Gt"])
                dv(lambda e: e.scalar_tensor_tensor(out=Gt[:, b, :], in0=i2, scalar=rr[:, 15:16], in1=Gt[:, b, :],
                                                    op0=ALU.mult, op1=ALU.add), ["is2", "r15", "Gt"], ["Gt"])
                return ops

            for tt in range(4):
                S.dma("sp", lambda e, tt=tt: e.dma_start(out=x1t[:], in_=x1_s[tt * 512:(tt + 1) * 512, :].rearrange("(b p) n -> p b n", p=128)),
                      reads=["x1_s"], writes=["x1t"])
                P6SKIP = os.environ.get("P6SKIP", "")
                for b in range(0 if "sq" in P6SKIP else 4):
                    S.op("act", lambda e, b=b: e.activation(out=acc[:, 0, :], in_=x1t[:, b, :], func=AF.Square, accum_out=rsm[:, b:b + 1]),
                         reads=["x1t"], writes=["acc", "rsm"])
                S.op("act", lambda e: e.activation(out=rsm[:, 4:8], in_=rsm[:, 0:4], func=AF.Sqrt, scale=1.0 / D, bias=EPS),
                     reads=["rsm"], writes=["rsm4"])
                S.op("dve", lambda e: e.reciprocal(out=rsm[:, 4:8], in_=rsm[:, 4:8]), reads=["rsm4"], writes=["rsm4"])
                for b in range(4):
                    S.op("dve", lambda e, b=b: e.tensor_scalar(out=x1t[:, b, :], in0=x1t[:, b, :], scalar1=rsm[:, 4 + b:5 + b], scalar2=None,
                                                               op0=ALU.mult), reads=["x1t", "rsm4"], writes=["x1t"])
                for kc in range(0 if "tr" in P6SKIP else KC):
                    pi = 4 + r6[0] % 4
                    r6[0] += 1

                    def tr(e, kc=kc, pi=pi):
                        r = None
                        for b in range(4):
                            r = e.transpose(ps[pi][:, b * 128:(b + 1) * 128], x1t[:, b, kc * 128:(kc + 1) * 128], ident[:])
                        return r
                    S.op("pe", tr, reads=["x1t", "ident"], writes=[PK[pi]])
                    S.op("dve", lambda e, kc=kc, pi=pi: e.tensor_scalar(out=h2T[:, kc, :], in0=ps[pi][:, :], scalar1=G2[:, kc:kc + 1],
                                                                       scalar2=modT[:, 48 + kc:49 + kc], op0=ALU.mult, op1=ALU.add),
                         reads=[PK[pi], "G2", "modT"], writes=["h2T"])
                    S.op("act", lambda e, kc=kc, pi=pi: e.activation(out=h2T32[:, kc, :], in_=ps[pi][:, :], func=AF.Identity,
                                                                    scale=G2[:, kc:kc + 1], bias=modT[:, 48 + kc:49 + kc]),
                         reads=[PK[pi], "G2", "modT"], writes=["acc"])
                for b in range(4 if P6CUT >= 2 else 0):
                    pi = 4 + r6[0] % 4
                    r6[0] += 1

                    def rmm(e, b=b, pi=pi):
                        r = None
                        for kc in range(KC):
                            r = e.matmul(ps[pi][:, 0:36], h2T32[:, kc, b * 128:(b + 1) * 128], rt[:, kc, :], start=(kc == 0), stop=(kc == KC - 1))
                        return r
                    S.op("pe", rmm, reads=["acc", "rt"], writes=[PK[pi]])
                    S.op("dve", lambda e, pi=pi, b=b: e.tensor_copy(out=lgB[:, b, :], in_=ps[pi][:, 0:36]), reads=[PK[pi]], writes=["lg_%d" % b])
                pend = [route_block(b) for b in range(4)]
                for k in range(len(pend[0])):
                    for b in range(4):
                        eng_, fn_, rd_, wr_ = pend[b][k]
                        S.op(eng_, fn_, reads=rd_, writes=wr_)
                if "ms" not in P6SKIP:
                    S.op("pool", lambda e: e.memset(acc[:], 0.0), writes=["acc"])
                for ex in range(NE if P6CUT >= 5 else (2 if P6CUT >= 4 else 0)):
                    wi = ei[0] % 2
                    ei[0] += 1
                    if not (os.environ.get("P6NODMA") and (tt > 0 or ex >= 2)):
                      S.dma("pool", lambda e, wi=wi, ex=ex: e.dma_start(out=wg[wi][:], in_=w_gate[ex].rearrange("(c p) n -> p c n", p=128)),
                          writes=["wg%d" % wi])
                      S.dma("pool", lambda e, wi=wi, ex=ex: e.dma_start(out=wu[wi][:], in_=w_up[ex].rearrange("(c p) n -> p c n", p=128)),
                          writes=["wu%d" % wi])
                      S.dma("pool", lambda e, wi=wi, ex=ex: [e.dma_start(
                        out=wd[wi][:, :, hf * 1024:(hf + 1) * 1024],
                        in_=w_down[ex][:, hf * 1024:(hf + 1) * 1024].rearrange("(c p) n -> p c n", p=128)) for hf in range(2)],
                        writes=["wd%d" % wi], n=2)
                    ut, utk = uT[wi], "uT%d" % wi
                    for fc in range(4):
                        ga, ua = 2 * (fc % 2), 1 + 2 * (fc % 2)

                        def gm(e, wi=wi, fc=fc, ga=ga, ua=ua):
                            for kc in range(KC):
                                e.matmul(ps[ga][:, :], wg[wi][:, kc, fc * 128:(fc + 1) * 128], h2T[:, kc, :], start=(kc == 0), stop=(kc == KC - 1))
                            r = None
                            for kc in range(KC):
                                r = e.matmul(ps[ua][:, :], wu[wi][:, kc, fc * 128:(fc + 1) * 128], h2T[:, kc, :], start=(kc == 0), stop=(kc == KC - 1))
                            return r
                        S.op("pe", gm, reads=["wg%d" % wi, "wu%d" % wi, "h2T"], writes=[PK[ga], PK[ua]])
                        S.op("act", lambda e, fc=fc, ga=ga: e.activation(out=sg[fc % 2][:], in_=ps[ga][:, :], func=AF.Silu),
                             reads=[PK[ga]], writes=["sg%d" % (fc % 2)])
                        S.op("dve", lambda e, fc=fc, ua=ua, ut=ut: e.tensor_tensor(out=ut[:, fc, :], in0=sg[fc % 2][:], in1=ps[ua][:, :], op=ALU.mult),
                             reads=["sg%d" % (fc % 2), PK[ua]], writes=[utk])
                    for b in range(4):
                        for dg in range(4):
                            yb = 4 + r6[0] % 4
                            r6[0] += 1

                            def dm(e, yb=yb, b=b, dg=dg, wi=wi, ut=ut):
                                r = None
                                for fc in range(4):
                                    r = e.matmul(ps[yb][:, :], ut[:, fc, b * 128:(b + 1) * 128], wd[wi][:, fc, dg * 512:(dg + 1) * 512],
                                                 start=(fc == 0), stop=(fc == 3))
                                return r
                            S.op("pe", dm, reads=[utk, "wd%d" % wi], writes=[PK[yb]])
                            S.op("dve", lambda e, yb=yb, b=b, dg=dg, ex=ex: e.scalar_tensor_tensor(
                                out=acc[:, b, dg * 512:(dg + 1) * 512], in0=ps[yb][:, :], scalar=Gt[:, b, ex:ex + 1],
                                in1=acc[:, b, dg * 512:(dg + 1) * 512], op0=ALU.mult, op1=ALU.add),
                                reads=[PK[yb], "Gt", "acc"], writes=["acc"])
                S.dma("sp", lambda e, tt=tt: e.dma_start(out=x1t[:], in_=x1_s[tt * 512:(tt + 1) * 512, :].rearrange("(b p) n -> p b n", p=128)),
                      reads=["x1_s"], writes=["x1t"])
                for b in range(4):
                    S.op("dve", lambda e, b=b: e.tensor_tensor(out=acc[:, b, :], in0=acc[:, b, :], in1=g2_bc[:], op=ALU.mult),
                         reads=["acc", "g2_bc"], writes=["acc"])
                    S.op("pool", lambda e, b=b: e.tensor_tensor(out=acc[:, b, :], in0=acc[:, b, :], in1=x1t[:, b, :], op=ALU.add),
                         reads=["acc", "x1t"], writes=["acc"])
                S.dma("sp", lambda e, tt=tt: e.dma_start(out=out_d[tt * 512:(tt + 1) * 512, :].rearrange("(b p) n -> p b n", p=128), in_=acc[:]),
                      reads=["acc"], writes=["out_d"])
            S.emit()
    return nc


def _t5_bucket(dist):
    max_exact = 16
    d = np.maximum(dist, 1).astype(np.float32)
    large = max_exact + (np.log(d / max_exact) / np.log(128 / max_exact) * (32 - max_exact)).astype(np.int32)
    large = np.minimum(large, 31)
    return np.where(dist < max_exact, dist, large)


_NC_CACHE = {}


def kernel(x, c, w_mod, b_mod, ln1_g, w_in, q_norm_g, w_q_up, q_gain, k_gain, rel_bias, gn_a, gn_b, w_out, ln2_g,
           router_g, router_e, w_gate, w_up, w_down):
    f32 = np.float32
    x = np.asarray(x, f32)
    c = np.asarray(c, f32)

    def col(v, n):
        return np.ascontiguousarray(np.asarray(v, f32).reshape(n, 128).T)

    s_ = np.arange(128)[:, None]
    u_ = np.arange(1024)[None, :]
    delta = np.clip(u_ - 384 - s_, 0, 4095)
    bidx = _t5_bucket(delta)
    rb = np.asarray(rel_bias, f32)
    tbias = np.ascontiguousarray(np.transpose(rb[bidx], (2, 0, 1)))
    rb31 = np.ascontiguousarray(np.broadcast_to(rb[31][None, :], (128, 8)))
    shared = {
        "w_mod": np.ascontiguousarray(np.asarray(w_mod, f32)[0]),
        "b_mod": np.ascontiguousarray(np.asarray(b_mod, f32)[0][None, :]),
        "ln1g": col(ln1_g[0], 16), "ln2g": col(ln2_g[0], 16),
        "w_in": np.ascontiguousarray(np.asarray(w_in, f32)[0]),
        "qng": col(q_norm_g[0], 4),
        "w_qup": np.ascontiguousarray(np.asarray(w_q_up, f32)[0]),
        "qgain": col(q_gain[0], 1), "kgain": col(k_gain[0], 1),
        "gna": col(gn_a[0], 8), "gnb": col(gn_b[0], 8),
        "w_out": np.ascontiguousarray(np.asarray(w_out, f32)[0]),
        "router": np.ascontiguousarray(np.concatenate([np.asarray(router_g, f32)[0], np.asarray(router_e, f32)[0]], axis=1)),
        "w_gate": np.ascontiguousarray(np.asarray(w_gate, f32)[0]),
        "w_up": np.ascontiguousarray(np.asarray(w_up, f32)[0]),
        "w_down": np.ascontiguousarray(np.asarray(w_down, f32)[0]),
        "tbias": tbias, "rb31": rb31,
    }
    in_maps = []
    for core in range(8):
        b, h = core // 2, core % 2
        tiles = x[b].reshape(8, 512, D)
        if h == 1:
            xk = x[b]
        else:
            xk = np.concatenate([tiles[0:1], tiles[0:7]], axis=0).reshape(SA, D)
        m = dict(shared)
        m["xk"] = np.ascontiguousarray(xk)
        m["kvalid"] = np.full((128, 1), float(h), f32)
        m["c_col"] = col(c[b], 16)
        in_maps.append(m)
    if "nc" not in _NC_CACHE:
        _NC_CACHE["nc"] = build_nc()
    res = run_bass_kernel_spmd(_NC_CACHE["nc"], in_maps, core_ids=list(range(8)))
    out = np.empty((4, 4096, D), f32)
    for core in range(8):
        b, h = core // 2, core % 2
        o = np.asarray(res.results[core]["out"], f32).reshape(4, 512, D)
        for j in range(4):
            g = 2 * j + h
            out[b, g * 512:(g + 1) * 512] = o[j]
    return out
```
